# Optimizing a Trainium2 kernel written in Bass

```python
import jax, jax.numpy as jnp
from jax import lax
import numpy as np

D_MODEL = 1024
BATCH = 8
SEQ = 4096
DEPTH = 1

N_META = 16
EPS = 1e-6
NEG_INF = -1e30
MLA_HEADS = 16
Q_RANK = 256
KV_RANK = 128
NOPE_DIM = 64
ROPE_DIM = 32
V_DIM = 64
ROPE_THETA = 10000.0
Q_BLOCK = 128
SSM_HEADS = 16
SSM_HEAD_DIM = 64
D_INNER = SSM_HEADS * SSM_HEAD_DIM
SSM_GROUPS = 2
HEADS_PER_GROUP = SSM_HEADS // SSM_GROUPS
D_STATE = 64
CONV_WIDTH = 4
CONV_DIM = D_INNER + 2 * SSM_GROUPS * D_STATE
CHUNK = 128
N_EXPERT_GROUPS = 4
EXPERTS_PER_GROUP = 8
N_EXPERTS = N_EXPERT_GROUPS * EXPERTS_PER_GROUP
TOP_K = 2
D_EXPERT = 256
PROJ_SIZES = (Q_RANK, KV_RANK, ROPE_DIM, D_INNER, CONV_DIM, SSM_HEADS, D_MODEL, D_MODEL)
D_IN_PROJ = Q_RANK + KV_RANK + ROPE_DIM + D_INNER + CONV_DIM + SSM_HEADS + 2 * D_MODEL

kernel_name = "hybrid_mla_ssd_gated_hmoe"


def rmsnorm(x, g):
    xf = x.astype(jnp.float32)
    y = xf * lax.rsqrt(jnp.mean(xf * xf, axis=-1, keepdims=True) + EPS)
    return (y * g.astype(jnp.float32)).astype(x.dtype)


def pad_left(a, n):
    return jnp.pad(a, [(0, 0), (n, 0)] + [(0, 0)] * (a.ndim - 2))


def apply_rope(x, cos, sin):
    x1, x2 = jnp.split(x, 2, axis=-1)
    return jnp.concatenate([x1 * cos - x2 * sin, x2 * cos + x1 * sin], axis=-1).astype(x.dtype)


def mla_branch(cq, ckv, kr, q_norm, w_uq, kv_norm, w_ukv):
    b, L, _ = cq.shape
    pad = CHUNK - N_META
    Lp = L + pad
    nb = Lp // Q_BLOCK
    q = (rmsnorm(cq, q_norm) @ w_uq).reshape(b, L, MLA_HEADS, NOPE_DIM + ROPE_DIM)
    kv = (rmsnorm(ckv, kv_norm) @ w_ukv).reshape(b, L, MLA_HEADS, NOPE_DIM + V_DIM)
    q, kv, kr = pad_left(q, pad), pad_left(kv, pad), pad_left(kr, pad)
    key_pos = jnp.arange(Lp)
    pos = jnp.maximum(key_pos - pad, 0).astype(jnp.float32)
    inv = ROPE_THETA ** (-jnp.arange(0, ROPE_DIM, 2, dtype=jnp.float32) / ROPE_DIM)
    ang = pos[:, None] * inv[None, :]
    cos, sin = jnp.cos(ang), jnp.sin(ang)
    q_nope, q_rope = q[..., :NOPE_DIM], q[..., NOPE_DIM:]
    q_rope = apply_rope(q_rope, cos[None, :, None, :], sin[None, :, None, :])
    k_nope, v = kv[..., :NOPE_DIM], kv[..., NOPE_DIM:]
    k_rope = apply_rope(kr, cos[None], sin[None])
    scale = (NOPE_DIM + ROPE_DIM) ** -0.5
    qn_b = jnp.moveaxis(q_nope.reshape(b, nb, Q_BLOCK, MLA_HEADS, NOPE_DIM), 1, 0)
    qr_b = jnp.moveaxis(q_rope.reshape(b, nb, Q_BLOCK, MLA_HEADS, ROPE_DIM), 1, 0)

    def attend(args):
        qn, qr, blk = args
        qpos = blk * Q_BLOCK + jnp.arange(Q_BLOCK)
        s = jnp.einsum('bqhd,bkhd->bhqk', qn, k_nope) + jnp.einsum('bqhr,bkr->bhqk', qr, k_rope)
        s = s.astype(jnp.float32) * scale
        mask = (key_pos[None, :] <= qpos[:, None]) & (key_pos[None, :] >= pad)
        s = jnp.where(mask, s, NEG_INF)
        p = jax.nn.softmax(s, axis=-1).astype(v.dtype)
        return jnp.einsum('bhqk,bkhd->bqhd', p, v)

    o = lax.map(attend, (qn_b, qr_b, jnp.arange(nb)))
    o = jnp.moveaxis(o, 0, 1).reshape(b, Lp, MLA_HEADS * V_DIM)
    return o[:, pad:]


def ssd_branch(z, xbc, dt_raw, conv_w, conv_b, dt_bias, a_log, d_skip, norm_g):
    b, L, _ = xbc.shape
    pad = CHUNK - N_META
    Lp = L + pad
    nc = Lp // CHUNK
    xbc = lax.conv_general_dilated(xbc, conv_w[:, None, :], window_strides=(1,),
                                   padding=[(CONV_WIDTH - 1, 0)],
                                   dimension_numbers=('NWC', 'WIO', 'NWC'),
                                   feature_group_count=CONV_DIM) + conv_b
    xbc = jax.nn.silu(xbc)
    xs = xbc[..., :D_INNER]
    bm = xbc[..., D_INNER:D_INNER + SSM_GROUPS * D_STATE]
    cm = xbc[..., D_INNER + SSM_GROUPS * D_STATE:]
    dt = jax.nn.softplus((dt_raw + dt_bias).astype(jnp.float32))
    A = -jnp.exp(a_log.astype(jnp.float32)).reshape(SSM_GROUPS, HEADS_PER_GROUP)
    xs, bm, cm, dt = pad_left(xs, pad), pad_left(bm, pad), pad_left(cm, pad), pad_left(dt, pad)
    x = xs.reshape(b, nc, CHUNK, SSM_GROUPS, HEADS_PER_GROUP, SSM_HEAD_DIM)
    bm = bm.reshape(b, nc, CHUNK, SSM_GROUPS, D_STATE)
    cm = cm.reshape(b, nc, CHUNK, SSM_GROUPS, D_STATE)
    dt = dt.reshape(b, nc, CHUNK, SSM_GROUPS, HEADS_PER_GROUP)
    a = dt * A
    xdt = x * dt[..., None]
    a_cs = jnp.cumsum(a, axis=2)
    seg = a_cs[:, :, :, None] - a_cs[:, :, None, :]
    tril = jnp.tril(jnp.ones((CHUNK, CHUNK), dtype=bool))[:, :, None, None]
    lmat = jnp.exp(jnp.where(tril, seg, -jnp.inf))
    cb = jnp.einsum('bclgn,bcsgn->bclsg', cm, bm)
    y_diag = jnp.einsum('bclsg,bclsge,bcsgep->bclgep', cb, lmat, xdt)
    decay = jnp.exp(a_cs[:, :, -1:] - a_cs)
    states = jnp.einsum('bclgn,bclge,bclgep->bcgepn', bm, decay, xdt)
    chunk_decay = jnp.exp(a_cs[:, :, -1])

    def step(carry, inp):
        st, dec = inp
        return carry * dec[..., None, None] + st, carry

    init = jnp.zeros((b, SSM_GROUPS, HEADS_PER_GROUP, SSM_HEAD_DIM, D_STATE), states.dtype)
    _, prev = lax.scan(step, init, (jnp.moveaxis(states, 1, 0), jnp.moveaxis(chunk_decay, 1, 0)))
    prev = jnp.moveaxis(prev, 0, 1)
    y_off = jnp.einsum('bclgn,bcgepn,bclge->bclgep', cm, prev, jnp.exp(a_cs))
    y = y_diag + y_off + x * d_skip.reshape(SSM_GROUPS, HEADS_PER_GROUP)[:, :, None]
    y = y.reshape(b, Lp, D_INNER)[:, pad:]
    yz = (y * jax.nn.silu(z)).astype(jnp.float32).reshape(b, L, SSM_GROUPS, D_INNER // SSM_GROUPS)
    yz = yz * lax.rsqrt(jnp.mean(yz * yz, axis=-1, keepdims=True) + EPS)
    return (yz.reshape(b, L, D_INNER) * norm_g.astype(jnp.float32)).astype(z.dtype)


def hier_moe(v, w_group, b_group, w_expert, b_expert, w_gate, w_up, w_down):
    shp = v.shape
    vt = v.reshape(-1, D_MODEL)
    g_logits = (vt @ w_group + b_group).astype(jnp.float32)
    p_group = jax.nn.softmax(g_logits, axis=-1)
    g_idx = jnp.argmax(g_logits, axis=-1)
    p_g = jnp.take_along_axis(p_group, g_idx[:, None], axis=-1)
    e_logits = (vt @ w_expert + b_expert).astype(jnp.float32).reshape(-1, N_EXPERT_GROUPS, EXPERTS_PER_GROUP)
    e_logits = jnp.take_along_axis(e_logits, g_idx[:, None, None], axis=1)[:, 0]
    top_vals, top_idx = lax.top_k(e_logits, TOP_K)
    w = jax.nn.softmax(top_vals, axis=-1) * p_g
    ids = g_idx[:, None] * EXPERTS_PER_GROUP + top_idx
    comb = jnp.sum(jax.nn.one_hot(ids, N_EXPERTS, dtype=jnp.float32) * w[..., None], axis=1).astype(vt.dtype)
    out = jnp.zeros_like(vt)
    for gi in range(N_EXPERT_GROUPS):
        sl = slice(gi * EXPERTS_PER_GROUP, (gi + 1) * EXPERTS_PER_GROUP)
        h = jax.nn.silu(jnp.einsum('td,edf->tef', vt, w_gate[sl])) * jnp.einsum('td,edf->tef', vt, w_up[sl])
        out = out + jnp.einsum('tef,te,efd->td', h, comb[:, sl], w_down[sl])
    return out.reshape(shp)


def setup_inputs(seed: int = 0) -> dict:
    key = jax.random.key(seed)
    ks = jax.random.split(key, 32)

    def nrm(k, shape, scale):
        return jax.random.normal(k, shape, jnp.float32) * scale

    def gain(k, shape):
        return 1.0 + 0.01 * jax.random.normal(k, shape, jnp.float32)

    dt0 = jnp.exp(jax.random.uniform(ks[9], (DEPTH, SSM_HEADS), jnp.float32, np.log(1e-3), np.log(1e-1)))
    return {
        "x": nrm(ks[0], (BATCH, SEQ, D_MODEL), 1.0),
        "meta_tokens": nrm(ks[1], (N_META, D_MODEL), 1.0),
        "norm_mix": gain(ks[2], (DEPTH, D_MODEL)),
        "w_in": nrm(ks[3], (DEPTH, D_MODEL, D_IN_PROJ), D_MODEL ** -0.5),
        "mla_q_norm": gain(ks[4], (DEPTH, Q_RANK)),
        "mla_w_uq": nrm(ks[5], (DEPTH, Q_RANK, MLA_HEADS * (NOPE_DIM + ROPE_DIM)), Q_RANK ** -0.5),
        "mla_kv_norm": gain(ks[6], (DEPTH, KV_RANK)),
        "mla_w_ukv": nrm(ks[7], (DEPTH, KV_RANK, MLA_HEADS * (NOPE_DIM + V_DIM)), KV_RANK ** -0.5),
        "ssm_conv_w": nrm(ks[8], (DEPTH, CONV_WIDTH, CONV_DIM), CONV_WIDTH ** -0.5),
        "ssm_conv_b": nrm(ks[10], (DEPTH, CONV_DIM), 0.01),
        "ssm_dt_bias": dt0 + jnp.log(-jnp.expm1(-dt0)),
        "ssm_a_log": jnp.log(jax.random.uniform(ks[11], (DEPTH, SSM_HEADS), jnp.float32, 1.0, 16.0)),
        "ssm_d_skip": gain(ks[12], (DEPTH, SSM_HEADS)),
        "ssm_norm": gain(ks[13], (DEPTH, D_INNER)),
        "w_branch_attn": nrm(ks[14], (DEPTH, MLA_HEADS * V_DIM, D_MODEL), (MLA_HEADS * V_DIM) ** -0.5),
        "w_branch_ssm": nrm(ks[15], (DEPTH, D_INNER, D_MODEL), D_INNER ** -0.5),
        "w_out": nrm(ks[16], (DEPTH, D_MODEL, D_MODEL), D_MODEL ** -0.5),
        "norm_ffn": gain(ks[17], (DEPTH, D_MODEL)),
        "moe_w_group": nrm(ks[18], (DEPTH, D_MODEL, N_EXPERT_GROUPS), D_MODEL ** -0.5),
        "moe_b_group": nrm(ks[19], (DEPTH, N_EXPERT_GROUPS), 0.01),
        "moe_w_expert": nrm(ks[20], (DEPTH, D_MODEL, N_EXPERTS), D_MODEL ** -0.5),
        "moe_b_expert": nrm(ks[21], (DEPTH, N_EXPERTS), 0.01),
        "moe_w_gate": nrm(ks[22], (DEPTH, N_EXPERTS, D_MODEL, D_EXPERT), D_MODEL ** -0.5),
        "moe_w_up": nrm(ks[23], (DEPTH, N_EXPERTS, D_MODEL, D_EXPERT), D_MODEL ** -0.5),
        "moe_w_down": nrm(ks[24], (DEPTH, N_EXPERTS, D_EXPERT, D_MODEL), D_EXPERT ** -0.5),
        "norm_final": gain(ks[25], (D_MODEL,)),
    }


def reference(x, meta_tokens, norm_mix, w_in, mla_q_norm, mla_w_uq, mla_kv_norm, mla_w_ukv,
              ssm_conv_w, ssm_conv_b, ssm_dt_bias, ssm_a_log, ssm_d_skip, ssm_norm,
              w_branch_attn, w_branch_ssm, w_out, norm_ffn, moe_w_group, moe_b_group,
              moe_w_expert, moe_b_expert, moe_w_gate, moe_w_up, moe_w_down, norm_final):
    b = x.shape[0]
    meta = jnp.broadcast_to(meta_tokens.astype(x.dtype)[None], (b, N_META, D_MODEL))
    h = jnp.concatenate([meta, x], axis=1)
    split_idx = list(np.cumsum(PROJ_SIZES)[:-1])
    for l in range(DEPTH):
        u = rmsnorm(h, norm_mix[l])
        proj = u @ w_in[l]
        cq, ckv, kr, z, xbc, dt_raw, g_attn, g_ssm = jnp.split(proj, split_idx, axis=-1)
        o_attn = mla_branch(cq, ckv, kr, mla_q_norm[l], mla_w_uq[l], mla_kv_norm[l], mla_w_ukv[l])
        o_ssm = ssd_branch(z, xbc, dt_raw, ssm_conv_w[l], ssm_conv_b[l], ssm_dt_bias[l],
                           ssm_a_log[l], ssm_d_skip[l], ssm_norm[l])
        merged = (jax.nn.sigmoid(g_attn) * (o_attn @ w_branch_attn[l])
                  + jax.nn.sigmoid(g_ssm) * (o_ssm @ w_branch_ssm[l]))
        h = h + (merged @ w_out[l]).astype(h.dtype)
        h = h + hier_moe(rmsnorm(h, norm_ffn[l]), moe_w_group[l], moe_b_group[l], moe_w_expert[l],
                         moe_b_expert[l], moe_w_gate[l], moe_w_up[l], moe_w_down[l]).astype(h.dtype)
    return rmsnorm(h, norm_final)[:, N_META:]
```

```python
import os
import numpy as np
from contextlib import ExitStack
import concourse.bass as bass
import concourse.mybir as mybir
from concourse.bass_utils import run_bass_kernel_spmd

F32 = mybir.dt.float32
BF16 = mybir.dt.bfloat16
I32 = mybir.dt.int32
ALU = mybir.AluOpType
AF = mybir.ActivationFunctionType
AX = mybir.AxisListType

N_DMA_SEMS = 3
D = 1024
KC = 8
NEXP = 32
EPS = 1e-6
A_W = 464
OFF_Z, OFF_X, OFF_GA, OFF_GS = 464, 1488, 2768, 3792
P1_COLS = 2768
W_IN_COLS = 4816


class Prog:
    def __init__(self, nc, es):
        self.nc = nc
        self.es = es
        self.eng = {"pe": nc.tensor, "act": nc.scalar, "dve": nc.vector,
                    "pool": nc.gpsimd, "sp": nc.sync}
        self.ops = []
        self.sem = {k: es.enter_context(nc.semaphore("s_" + k)) for k in self.eng}
        self.dsem = {}
        for q in ("sp", "pool", "act"):
            self.dsem[q] = [es.enter_context(nc.semaphore(f"d_{q}{i}")) for i in range(N_DMA_SEMS)]
        self.cnt = {k: 0 for k in self.eng}
        self.known = {}
        self.dcount = {}
        self.drr = {q: 0 for q in self.dsem}
        self.n = 0
        self.total_ops = 0

    def sb(self, es, shape, dtype, name=None):
        self.n += 1
        return es.enter_context(self.nc.sbuf_tensor(name or f"sb{self.n}", list(shape), dtype))

    def ps(self, es, shape, dtype, name=None):
        self.n += 1
        return es.enter_context(self.nc.psum_tensor(name or f"ps{self.n}", list(shape), dtype))

    def op(self, eng, fn, r=(), w=(), dma=False):
        self.nrec = getattr(self, "nrec", 0) + 1
        if self.nrec > int(os.environ.get("KOPS", "100000000")):
            return
        self.ops.append((eng, fn, tuple(r), tuple(w), dma))

    def dma(self, q, fn, r=(), w=()):
        self.op(q, fn, r, w, dma=True)

    def _wait(self, eng, s, v):
        kk = (eng, id(s))
        if self.known.get(kk, 0) >= v:
            return
        self.known[kk] = v
        self.eng[eng].wait_ge(s, v)

    def flush(self):
        ops = self.ops
        self.ops = []
        n = len(ops)
        self.total_ops += n
        last_w, readers = {}, {}
        deps = [None] * n
        for i, (eng, fn, r, w, dma) in enumerate(ops):
            d = set()
            for k in r:
                if k in last_w:
                    d.add(last_w[k])
            for k in w:
                if k in last_w:
                    d.add(last_w[k])
                d.update(readers.get(k, ()))
            d.discard(i)
            deps[i] = d
            for k in r:
                readers.setdefault(k, []).append(i)
            for k in w:
                last_w[k] = i
                readers[k] = []
        signals = [False] * n
        last_of = {}
        for i in range(n):
            eng_i, dma_i = ops[i][0], ops[i][4]
            if not dma_i:
                last_of[eng_i] = i
            keep = {}
            for j in deps[i]:
                eng_j, dma_j = ops[j][0], ops[j][4]
                if dma_j:
                    keep[("dma", j)] = j
                    continue
                if eng_j == "pe" and eng_i == "pe" and not dma_i:
                    continue
                key = ("eng", eng_j)
                if key not in keep or keep[key] < j:
                    keep[key] = j
            deps[i] = sorted(keep.values())
            for j in deps[i]:
                signals[j] = True
        for e, i in last_of.items():
            signals[i] = True
        tok = [None] * n
        for i, (eng, fn, r, w, dma) in enumerate(ops):
            waits = [tok[j] for j in deps[i]]
            s = None
            if dma:
                pool = self.dsem[eng]
                s = pool[self.drr[eng] % len(pool)]
                self.drr[eng] += 1
                c = self.dcount.get(id(s), 0)
                if c > 0:
                    waits.append((s, c))
            for (ws, wv) in waits:
                self._wait(eng, ws, wv)
            res = fn()
            if dma:
                lst = res if isinstance(res, (list, tuple)) else [res]
                c = self.dcount.get(id(s), 0)
                for ins in lst:
                    ins.then_inc(s, 16)
                    c += 16
                self.dcount[id(s)] = c
                tok[i] = (s, c)
            elif signals[i]:
                self.cnt[eng] += 1
                res.then_inc(self.sem[eng], 1)
                tok[i] = (self.sem[eng], self.cnt[eng])
        for wname in self.eng:
            for e2 in self.eng:
                if e2 != wname and self.cnt[e2] > 0:
                    self._wait(wname, self.sem[e2], self.cnt[e2])
            for q, pool in self.dsem.items():
                for s in pool:
                    c = self.dcount.get(id(s), 0)
                    if c > 0:
                        self._wait(wname, s, c)


def build(NT):
    STOP = int(os.environ.get('KSTOP', '99'))
    NR = NT - 1
    TOK = NT * 128
    NSLOT = 2 * NR + NEXP
    nc = bass.Bass("TRN2", target_bir_lowering=False)

    def din(name, shape, dt=F32):
        return nc.dram_tensor(name, list(shape), dt, kind="ExternalInput").ap()

    x = din("x", [NR * 128, D])
    meta = din("meta", [16, D])
    w_in = din("w_in", [128, KC, W_IN_COLS])
    g_mix = din("g_mix", [128, D])
    g_q = din("g_q", [128, 256])
    g_kv = din("g_kv", [128, 128])
    g_ssm = din("g_ssm", [128, D])
    g_ffn = din("g_ffn", [128, D])
    g_fin = din("g_fin", [128, D])
    w_uqa = din("w_uqa", [128, 2, 16, 128])
    w_uqs = din("w_uqs", [128, 2, 16, 32])
    w_uk = din("w_uk", [128, 16, 64])
    w_uv = din("w_uv", [128, 16, 64])
    c2tok = din("c2tok", [128, NT, 32])
    s2tok = din("s2tok", [128, NT, 32])
    c2T = din("c2T", [32, TOK])
    s2T = din("s2T", [32, TOK])
    conv_w = din("conv_w", [128, 10, 4])
    conv_b = din("conv_b", [128, 10])
    dt_bias = din("dt_bias", [128, 16])
    a_log = din("a_log", [128, 16])
    d_skip = din("d_skip", [128, 16])
    w_ba = din("w_ba", [128, KC, D])
    w_bs = din("w_bs", [128, KC, D])
    w_out = din("w_out", [128, KC, D])
    w_r = din("w_r", [128, KC, 36])
    b_r = din("b_r", [128, 36])
    w_gu = din("w_gu", [NEXP * 128, KC * 512])
    w_dn = din("w_dn", [NEXP * 128, 2 * D])
    y_out = nc.dram_tensor("y", [NR * 128, D], F32, kind="ExternalOutput").ap()

    def scratch(name, shape, dt):
        return nc.dram_tensor(name, list(shape), dt, kind="Internal").ap()

    sc_osT = scratch("sc_osT", [TOK, D], BF16)
    sc_cq = scratch("sc_cq", [128, 2, TOK], BF16)
    sc_ckv = scratch("sc_ckv", [128, TOK], BF16)
    sc_kr = scratch("sc_kr", [32, TOK], BF16)
    sc_h1 = scratch("sc_h1", [TOK, D], F32)
    sc_vh = scratch("sc_vh", [TOK, D], BF16)
    sc_xs = scratch("sc_xs", [NSLOT * 128, D], BF16)
    sc_ys = scratch("sc_ys", [NSLOT * 128, D], F32)
    WGU_C, WC = KC * 512, KC * 512 + 2 * D
    sc_wc = scratch("sc_wc", [NEXP * 128, WC], BF16)

    with ExitStack() as es:
        P = Prog(nc, es)
        sb = lambda shape, dt, e=es: P.sb(e, shape, dt)
        bank = [P.ps(es, [128, 512], F32, name=f"bank{i}") for i in range(8)]

        def bfv(i):
            return bank[i][:].bitcast(BF16)

        ident = sb([128, 128], BF16)
        identf = sb([128, 128], F32)
        uincl = sb([128, 128], F32)
        mask01 = sb([128, 128], F32)
        negm = sb([128, 8, 128], BF16)
        ones_f = sb([128, 128], F32)
        ones_b = sb([128, 128], BF16)
        epst = sb([128, 1], F32)
        onet = sb([128, 1], F32)
        mask0 = sb([128, 1], F32)
        A_bc = sb([128, 16], F32)
        dtb_bc = sb([128, 16], F32)
        dsk_bc = sb([128, 16], F32)
        oh1 = sb([128, NR, 32], F32)
        oh2 = sb([128, NR, 32], F32)
        rank = sb([128, NR, 32], F32)
        wts = sb([128, NR, 2], F32)
        Macc = sb([128, 32], F32)

        P.op("pool", lambda: nc.gpsimd.memset(identf[:], 1.0), w=["identf"])
        P.op("pool", lambda: nc.gpsimd.affine_select(out=identf[:], in_=identf[:], pattern=[[-1, 128]],
                                                      compare_op=ALU.is_equal, fill=0.0, base=0, channel_multiplier=1),
             r=["identf"], w=["identf"])
        P.op("dve", lambda: nc.vector.tensor_copy(out=ident[:], in_=identf[:]), r=["identf"], w=["ident"])
        P.op("pool", lambda: nc.gpsimd.memset(ones_f[:], 1.0), w=["ones_f"])
        P.op("pool", lambda: nc.gpsimd.memset(ones_b[:], 1.0), w=["ones_b"])
        P.op("pool", lambda: nc.gpsimd.affine_select(out=uincl[:], in_=ones_f[:], pattern=[[1, 128]],
                                                      compare_op=ALU.is_ge, fill=0.0, base=0, channel_multiplier=-1),
             r=["ones_f"], w=["uincl"])
        P.op("dve", lambda: nc.vector.tensor_copy(out=mask01[:], in_=uincl[:]), r=["uincl"], w=["mask01"])
        P.op("dve", lambda: nc.vector.tensor_scalar(out=negm[:], in0=uincl[:].unsqueeze(1).to_broadcast([128, 8, 128]),
                                                    scalar1=-1.0, scalar2=30000.0, op0=ALU.add, op1=ALU.mult),
             r=["uincl"], w=["negm"])
        P.op("dve", lambda: nc.vector.memset(epst[:], EPS), w=["eps"])
        P.op("dve", lambda: nc.vector.memset(onet[:], 1.0), w=["onet"])
        P.op("dve", lambda: nc.vector.memset(mask0[:], 1.0), w=["mask0"])
        P.op("dve", lambda: nc.vector.memset(mask0[0:112, :], 0.0), r=["mask0"], w=["mask0"])
        P.dma("sp", lambda: nc.sync.dma_start(out=A_bc[:], in_=a_log), w=["A_bc"])
        P.dma("sp", lambda: nc.sync.dma_start(out=dtb_bc[:], in_=dt_bias), w=["dtb"])
        P.dma("sp", lambda: nc.sync.dma_start(out=dsk_bc[:], in_=d_skip), w=["dsk"])
        P.op("act", lambda: nc.scalar.activation(out=A_bc[:], in_=A_bc[:], func=AF.Exp), r=["A_bc"], w=["A_bc"])
        P.op("dve", lambda: nc.vector.tensor_scalar(out=A_bc[:], in0=A_bc[:], scalar1=-1.0, scalar2=None, op0=ALU.mult),
             r=["A_bc"], w=["A_bc"])
        P.flush()

        def rstd_ops(ss, out, n, key_in, key_out, extra_scale=None):
            P.op("act", lambda: nc.scalar.activation(out=out, in_=ss, func=AF.Ln, scale=1.0 / n, bias=epst[:, 0:1]),
                 r=[key_in, "eps"], w=[key_out])
            P.op("act", lambda: nc.scalar.activation(out=out, in_=out, func=AF.Exp, scale=-0.5), r=[key_out], w=[key_out])
            if extra_scale is not None:
                P.op("dve", lambda: nc.vector.tensor_scalar(out=out, in0=out, scalar1=float(extra_scale), scalar2=None,
                                                            op0=ALU.mult), r=[key_out], w=[key_out])

        ldc = [0]

        def load_w(dst, src, ncols, src_off, stg, key, nk=KC):
            for c in range(nk):
                P.dma("pool", lambda c=c: nc.gpsimd.dma_start(out=dst[:, c, 0:ncols], in_=src[:, c, src_off:src_off + ncols]), w=[key])

        with ExitStack() as e1:
            s1 = lambda shape, dt: P.sb(e1, shape, dt)
            w_inb = s1([128, KC, P1_COLS], BF16)
            stg = [s1([128, 512], F32) for _ in range(2)]
            load_w(w_inb, w_in, P1_COLS, 0, stg, "w_inb")
            gmix = s1([128, D], F32)
            gq = s1([128, 256], F32)
            gkv = s1([128, 128], F32)
            gss = s1([128, D], F32)
            c2t = s1([128, NT, 32], F32)
            s2t = s1([128, NT, 32], F32)
            cw = s1([128, 10, 4], F32)
            cb = s1([128, 10], F32)
            for dst, src, k in ((gmix, g_mix, "gmix"), (gq, g_q, "gq"), (gkv, g_kv, "gkv"), (gss, g_ssm, "gss"),
                                (c2t, c2tok, "c2t"), (s2t, s2tok, "s2t"), (cw, conv_w, "cw"), (cb, conv_b, "cb")):
                P.dma("sp", lambda dst=dst, src=src: nc.sync.dma_start(out=dst[:], in_=src), w=[k])

            def mb(n, shape, dt):
                return [s1(shape, dt) for _ in range(n)]
            xt = mb(2, [128, D], F32)
            xt0 = s1([128, D], F32)
            junk = s1([128, D], BF16)
            u = s1([128, D], BF16)
            uT = mb(2, [128, KC, 128], BF16)
            st8 = s1([128, 8], F32)
            tA = s1([128, 512], BF16)
            tAT = s1([128, 512], BF16)
            kr1 = s1([128, 32], F32)
            kr2 = s1([128, 32], F32)
            dtt = mb(2, [128, 16], F32)
            at = mb(2, [128, 16], F32)
            zs = mb(3, [128, D], BF16)
            xh = s1([128, 10, 132], F32)
            cacc = s1([128, 10, 128], F32)
            xbcs = mb(3, [128, 10, 128], BF16)
            xtok = mb(2, [128, D], BF16)
            btok = mb(2, [128, 128], BF16)
            xdt = mb(2, [128, D], BF16)
            xdtd = mb(2, [128, D], BF16)
            acs = s1([128, 16], F32)
            eacs = mb(2, [128, 16], F32)
            Rg = s1([128, 8, 128], F32)
            Dg = s1([128, 8, 128], F32)
            LT = s1([128, 8, 128], F32)
            dec = s1([128, 16], F32)
            cbtm = s1([128, 2, 128], F32)
            MT = mb(2, [128, 16, 128], BF16)
            cdsel = mb(2, [128, 8], F32)
            Sst = s1([128, 512], F32)
            prevb = s1([128, 512], BF16)
            yd = s1([128, D], F32)
            yy = s1([128, D], F32)
            ssg = s1([128, 4], F32)
            osm = s1([128, D], BF16)
            osmT = s1([128, KC, 128], BF16)

            P.op("dve", lambda: nc.vector.memset(xt0[:], 0.0), w=["xt0"])
            P.dma("sp", lambda: nc.sync.dma_start(out=xt0[112:128, :], in_=meta), r=["xt0"], w=["xt0"])
            P.op("dve", lambda: nc.vector.memset(xh[:], 0.0), w=["xh", ("xh", 0), ("xh", 1), ("xh", 2)])
            P.op("dve", lambda: nc.vector.memset(Sst[:], 0.0), w=["Sst"])
            P.op("pool", lambda: nc.gpsimd.memset(tA[:], 0.0), w=["tA"])

            def stageA(t):
                i2, i3 = t % 2, t % 3
                xcur = xt0 if t == 0 else xt[i2]
                kx = "xt0" if t == 0 else ("xt", i2)
                uTc, kuT = uT[i2], ("uT", i2)
                dttc, kdt = dtt[i2], ("dtt", i2)
                atc, kat = at[i2], ("at", i2)
                zsc, kzs = zs[i3], ("zs", i3)
                xb, kxb = xbcs[i3], ("xbcs", i3)
                if t > 0:
                    P.dma("sp", lambda: nc.sync.dma_start(out=xcur[:], in_=x[(t - 1) * 128:t * 128, :]), w=[kx])
                P.op("act", lambda: nc.scalar.activation(out=junk[:], in_=xcur[:], func=AF.Square, accum_out=st8[:, 0:1]),
                     r=[kx], w=["junk", "ssx"])
                rstd_ops(st8[:, 0:1], st8[:, 1:2], D, "ssx", "rsx")
                P.op("dve", lambda: nc.vector.scalar_tensor_tensor(out=u[:], in0=xcur[:], scalar=st8[:, 1:2], in1=gmix[:],
                                                                   op0=ALU.mult, op1=ALU.mult), r=[kx, "rsx", "gmix"], w=["u"])

                def tr_u():
                    for c in range(KC):
                        ins = nc.tensor.transpose(out=bfv(0)[:, c * 128:(c + 1) * 128], in_=u[:, c * 128:(c + 1) * 128], identity=ident[:])
                    return ins
                P.op("pe", tr_u, r=["u", "ident"], w=["b0"])
                P.op("act", lambda: nc.scalar.copy(out=uTc[:].rearrange("p c t -> p (c t)"), in_=bfv(0)), r=["b0"], w=[kuT])
                yield

                def mmA():
                    for c in range(KC):
                        ins = nc.tensor.matmul(bank[1][:, 0:A_W], lhsT=uTc[:, c, :], rhs=w_inb[:, c, 0:A_W], start=(c == 0), stop=(c == KC - 1))
                    return ins
                P.op("pe", mmA, r=[kuT, "w_inb"], w=["b1"])
                P.op("act", lambda: nc.scalar.activation(out=junk[:, 0:256], in_=bank[1][:, 0:256], func=AF.Square, accum_out=st8[:, 2:3]),
                     r=["b1"], w=["junk", "ssq"])
                P.op("act", lambda: nc.scalar.activation(out=junk[:, 256:384], in_=bank[1][:, 256:384], func=AF.Square, accum_out=st8[:, 4:5]),
                     r=["b1"], w=["junk", "sskv"])
                rstd_ops(st8[:, 2:3], st8[:, 3:4], 256, "ssq", "rsq", extra_scale=96 ** -0.5)
                rstd_ops(st8[:, 4:5], st8[:, 5:6], 128, "sskv", "rskv")
                P.op("dve", lambda: nc.vector.scalar_tensor_tensor(out=tA[:, 0:256], in0=bank[1][:, 0:256], scalar=st8[:, 3:4], in1=gq[:],
                                                                   op0=ALU.mult, op1=ALU.mult), r=["b1", "rsq", "gq"], w=["tA"])
                P.op("dve", lambda: nc.vector.scalar_tensor_tensor(out=tA[:, 256:384], in0=bank[1][:, 256:384], scalar=st8[:, 5:6], in1=gkv[:],
                                                                   op0=ALU.mult, op1=ALU.mult), r=["b1", "rskv", "gkv"], w=["tA"])
                P.op("dve", lambda: nc.vector.tensor_tensor(out=kr1[:], in0=bank[1][:, 384:416], in1=c2t[:, t, :], op=ALU.mult),
                     r=["b1", "c2t"], w=["kr1"])
                P.op("dve", lambda: nc.vector.tensor_tensor(out=kr2[:], in0=bank[1][:, 416:448], in1=s2t[:, t, :], op=ALU.mult),
                     r=["b1", "s2t"], w=["kr2"])
                P.op("dve", lambda: nc.vector.tensor_tensor(out=tA[:, 384:416], in0=kr1[:], in1=kr2[:], op=ALU.add),
                     r=["kr1", "kr2"], w=["tA"])
                P.op("dve", lambda: nc.vector.tensor_tensor(out=dttc[:], in0=bank[1][:, 448:464], in1=dtb_bc[:], op=ALU.add),
                     r=["b1", "dtb"], w=[kdt])
                P.op("act", lambda: nc.scalar.activation(out=dttc[:], in_=dttc[:], func=AF.Exp), r=[kdt], w=[kdt])
                P.op("act", lambda: nc.scalar.activation(out=dttc[:], in_=dttc[:], func=AF.Ln, bias=onet[:, 0:1]), r=[kdt, "onet"], w=[kdt])
                if t == 0:
                    P.op("dve", lambda: nc.vector.tensor_scalar(out=dttc[:], in0=dttc[:], scalar1=mask0[:, 0:1], scalar2=None, op0=ALU.mult),
                         r=[kdt, "mask0"], w=[kdt])
                P.op("dve", lambda: nc.vector.tensor_tensor(out=atc[:], in0=dttc[:], in1=A_bc[:], op=ALU.mult), r=[kdt, "A_bc"], w=[kat])
                yield

                def mm_z():
                    for hf in range(2):
                        for c in range(KC):
                            ins = nc.tensor.matmul(bank[2 + hf][:], lhsT=uTc[:, c, :], rhs=w_inb[:, c, OFF_Z + hf * 512: OFF_Z + (hf + 1) * 512],
                                                   start=(c == 0), stop=(c == KC - 1))
                    return ins
                P.op("pe", mm_z, r=[kuT, "w_inb"], w=["b2", "b3"])
                for hf in range(2):
                    P.op("act", lambda hf=hf: nc.scalar.activation(out=zsc[:, hf * 512:(hf + 1) * 512], in_=bank[2 + hf][:], func=AF.Silu),
                         r=["b%d" % (2 + hf)], w=[kzs])
                yield

                def tr_A():
                    for c in range(3):
                        nc.tensor.transpose(out=bfv(0)[:, c * 128:(c + 1) * 128], in_=tA[:, c * 128:(c + 1) * 128], identity=ident[:])
                    return nc.tensor.transpose(out=bfv(0)[0:32, 384:512], in_=tA[:, 384:416], identity=ident[:])
                P.op("pe", tr_A, r=["tA", "ident"], w=["b0"])
                P.op("act", lambda: nc.scalar.copy(out=tAT[:, 0:384], in_=bfv(0)[:, 0:384]), r=["b0"], w=["tATa"])
                P.op("act", lambda: nc.scalar.copy(out=tAT[0:32, 384:512], in_=bfv(0)[0:32, 384:512]), r=["b0"], w=["tATb"])
                P.dma("sp", lambda: nc.sync.dma_start(out=sc_cq[:, :, t * 128:(t + 1) * 128],
                                                      in_=tAT[:, 0:256].rearrange("p (c t) -> p c t", c=2)), r=["tATa"], w=[("sc_cq", t)])
                P.dma("sp", lambda: nc.sync.dma_start(out=sc_ckv[:, t * 128:(t + 1) * 128], in_=tAT[:, 256:384]), r=["tATa"], w=[("sc_ckv", t)])
                P.dma("sp", lambda: nc.sync.dma_start(out=sc_kr[:, t * 128:(t + 1) * 128], in_=tAT[0:32, 384:512]), r=["tATb"], w=[("sc_kr", t)])
                yield
                xhk = [("xh", 0), ("xh", 1), ("xh", 2)]
                for grp in range(3):
                    chunks = list(range(grp * 4, min(10, grp * 4 + 4)))

                    def mmX(chunks=chunks):
                        for i, j in enumerate(chunks):
                            for c in range(KC):
                                ins = nc.tensor.matmul(bank[4][:, i * 128:(i + 1) * 128], lhsT=w_inb[:, c, OFF_X + j * 128: OFF_X + (j + 1) * 128],
                                                       rhs=uTc[:, c, :], start=(c == 0), stop=(c == KC - 1))
                        return ins
                    P.op("pe", mmX, r=[kuT, "w_inb"], w=["b4"])
                    nchk = len(chunks)
                    P.op("act", lambda grp=grp, nchk=nchk: nc.scalar.copy(
                        out=xh[:, grp * 4: grp * 4 + nchk, 3:131],
                        in_=bank[4][:, 0:nchk * 128].rearrange("p (c t) -> p c t", c=nchk)), r=["b4", "xh"], w=[("xh", grp)])
                    for j in chunks:
                        P.op("dve", lambda j=j: nc.vector.tensor_scalar(out=cacc[:, j, :], in0=xh[:, j, 0:128], scalar1=cw[:, j, 0:1], scalar2=cb[:, j:j + 1],
                                                                       op0=ALU.mult, op1=ALU.add), r=["xh", ("xh", grp), "cw", "cb"], w=[("cacc", j)])
                        for k in range(1, 4):
                            P.op("dve", lambda j=j, k=k: nc.vector.scalar_tensor_tensor(out=cacc[:, j, :], in0=xh[:, j, k:k + 128], scalar=cw[:, j, k:k + 1],
                                                                                       in1=cacc[:, j, :], op0=ALU.mult, op1=ALU.add),
                                 r=["xh", ("xh", grp), "cw", ("cacc", j)], w=[("cacc", j)])
                    yield
                ck = [("cacc", j) for j in range(10)]
                P.op("act", lambda: nc.scalar.activation(out=xb[:].rearrange("p c t -> p (c t)"), in_=cacc[:].rearrange("p c t -> p (c t)"), func=AF.Silu),
                     r=ck, w=[kxb])
                P.op("pool", lambda: nc.gpsimd.tensor_copy(out=xh[:, :, 0:3], in_=xh[:, :, 128:131]), r=xhk + ["xh"], w=["xh"] + xhk)
                if t == 0:
                    P.op("dve", lambda: nc.vector.memset(xb[:, :, 0:112], 0.0), r=[kxb], w=[kxb])
                yield

            def stageB(t):
                i2, i3 = t % 2, t % 3
                dttc, kdt = dtt[i2], ("dtt", i2)
                atc, kat = at[i2], ("at", i2)
                xb, kxb = xbcs[i3], ("xbcs", i3)
                xtk, kxt = xtok[i2], ("xtok", i2)
                btk, kbt = btok[i2], ("btok", i2)
                xd, kxd = xdt[i2], ("xdt", i2)
                xdd, kxdd = xdtd[i2], ("xdtd", i2)
                ea, kea = eacs[i2], ("eacs", i2)
                MTc = MT[i2]
                cds = cdsel[i2]

                def tr_x():
                    for c in range(8):
                        ins = nc.tensor.transpose(out=bfv(5)[:, c * 128:(c + 1) * 128], in_=xb[:, c, :], identity=ident[:])
                    return nc.tensor.transpose(out=bfv(6)[:, 0:128], in_=xb[:, 8, :], identity=ident[:])
                P.op("pe", tr_x, r=[kxb, "ident"], w=["b5", "b6"])
                P.op("act", lambda: nc.scalar.copy(out=xtk[:], in_=bfv(5)), r=["b5"], w=[kxt])
                P.op("act", lambda: nc.scalar.copy(out=btk[:], in_=bfv(6)[:, 0:128]), r=["b6"], w=[kbt])
                P.op("pe", lambda: nc.tensor.matmul(bank[7][:, 0:16], lhsT=uincl[:], rhs=atc[:], start=True, stop=True), r=["uincl", kat], w=["b7"])
                P.op("dve", lambda: nc.vector.tensor_copy(out=acs[:], in_=bank[7][:, 0:16]), r=["b7"], w=["acs"])
                P.op("act", lambda: nc.scalar.activation(out=ea[:], in_=bank[7][:, 0:16], func=AF.Exp), r=["b7"], w=[kea])

                P.op("pe", lambda: nc.tensor.matmul(bank[7][:, 128:256], lhsT=xb[0:64, 8, :], rhs=xb[0:64, 9, :], start=True, stop=True),
                     r=[kxb], w=["b7"])
                P.op("pe", lambda: nc.tensor.matmul(bank[4][:, 0:128], lhsT=xb[64:128, 8, :], rhs=xb[64:128, 9, :], start=True, stop=True),
                     r=[kxb], w=["b4"])
                P.op("dve", lambda: nc.vector.tensor_tensor(out=cbtm[:, 0, :], in0=bank[7][:, 128:256], in1=mask01[:], op=ALU.mult),
                     r=["b7", "mask01"], w=["cbtm0"])
                P.op("dve", lambda: nc.vector.tensor_tensor(out=cbtm[:, 1, :], in0=bank[4][:, 0:128], in1=mask01[:], op=ALU.mult),
                     r=["b4", "mask01"], w=["cbtm1"])
                P.op("dve", lambda: nc.vector.tensor_tensor(out=xd[:].rearrange("p (h d) -> p h d", h=16),
                                                            in0=xtk[:].rearrange("p (h d) -> p h d", h=16),
                                                            in1=dttc[:].unsqueeze(2).to_broadcast([128, 16, 64]), op=ALU.mult),
                     r=[kxt, kdt], w=[kxd])
                yield
                for g in range(2):
                    P.op("pool", lambda g=g: nc.gpsimd.tensor_tensor(out=Rg[:], in0=uincl[:].unsqueeze(1).to_broadcast([128, 8, 128]),
                                                                    in1=atc[:, g * 8:(g + 1) * 8].unsqueeze(2).to_broadcast([128, 8, 128]), op=ALU.mult),
                         r=["uincl", kat], w=["Rg"])

                    def mmbc(g=g):
                        for q4 in range(2):
                            nc.tensor.matmul(bank[5 + q4][:], lhsT=ones_f[:], rhs=Rg[:, q4 * 4:(q4 + 1) * 4, :].rearrange("p e l -> p (e l)"),
                                             start=True, stop=False)
                            ins = nc.tensor.matmul(bank[5 + q4][:], lhsT=ident[:], rhs=negm[:, q4 * 4:(q4 + 1) * 4, :].rearrange("p e l -> p (e l)"),
                                                   start=False, stop=True)
                        return ins
                    P.op("pe", mmbc, r=["ones_f", "Rg", "ident", "negm"], w=["b5", "b6"])
                    for q4 in range(2):
                        P.op("dve", lambda g=g, q4=q4: nc.vector.tensor_tensor(
                            out=Dg[:, q4 * 4:(q4 + 1) * 4, :], in0=bank[5 + q4][:].rearrange("p (e l) -> p e l", e=4),
                            in1=acs[:, g * 8 + q4 * 4: g * 8 + q4 * 4 + 4].unsqueeze(2).to_broadcast([128, 4, 128]), op=ALU.subtract),
                            r=["b%d" % (5 + q4), "acs"], w=[("Dg", q4)])
                    P.op("act", lambda: nc.scalar.activation(out=LT[:], in_=Dg[:], func=AF.Exp),
                         r=[("Dg", 0), ("Dg", 1)], w=["LT"])
                    P.op("dve", lambda g=g: nc.vector.tensor_copy(out=dec[:, g * 8:(g + 1) * 8], in_=LT[:, :, 127]), r=["LT"], w=[("dec", g)])
                    P.op("act", lambda g=g: nc.scalar.activation(
                        out=cds[g * 64:(g + 1) * 64, 0:4],
                        in_=bank[5][g * 64:(g + 1) * 64, :].rearrange("p (e l) -> p e l", e=4)[:, :, 127], func=AF.Exp),
                        r=["b5"], w=[("cdselA", i2, g)])
                    P.op("act", lambda g=g: nc.scalar.activation(
                        out=cds[g * 64:(g + 1) * 64, 4:8],
                        in_=bank[6][g * 64:(g + 1) * 64, :].rearrange("p (e l) -> p e l", e=4)[:, :, 127], func=AF.Exp),
                        r=["b6", ("cdselA", i2, g)], w=[("cdsel", i2, g)])
                    P.op("pool", lambda g=g: nc.gpsimd.tensor_tensor(
                        out=MTc[:, g * 8:(g + 1) * 8, :], in0=LT[:],
                        in1=cbtm[:, g, :].unsqueeze(1).to_broadcast([128, 8, 128]), op=ALU.mult), r=["LT", "cbtm0", "cbtm1"], w=[("MT", i2, g)])
                    yield
                P.op("dve", lambda: nc.vector.tensor_tensor(out=xdd[:].rearrange("p (h d) -> p h d", h=16),
                                                            in0=xd[:].rearrange("p (h d) -> p h d", h=16),
                                                            in1=dec[:].unsqueeze(2).to_broadcast([128, 16, 64]), op=ALU.mult),
                     r=[kxd, ("dec", 0), ("dec", 1)], w=[kxdd])
                yield

            def stageC(t):
                i2, i3 = t % 2, t % 3
                zsc, kzs = zs[i3], ("zs", i3)
                xb, kxb = xbcs[i3], ("xbcs", i3)
                xtk, kxt = xtok[i2], ("xtok", i2)
                btk, kbt = btok[i2], ("btok", i2)
                xd, kxd = xdt[i2], ("xdt", i2)
                xdd, kxdd = xdtd[i2], ("xdtd", i2)
                ea, kea = eacs[i2], ("eacs", i2)
                MTc = MT[i2]
                cds = cdsel[i2]
                P.op("pool", lambda: nc.gpsimd.tensor_copy(out=prevb[:], in_=Sst[:]), r=["Sst"], w=["prevb"])
                for g in range(2):
                    P.op("pe", lambda g=g: nc.tensor.matmul(bank[4][g * 64:(g + 1) * 64, :], lhsT=btk[:, g * 64:(g + 1) * 64], rhs=xdd[:, g * 512:(g + 1) * 512],
                                                            start=True, stop=True), r=[kbt, kxdd], w=["b4", "b4s%d" % g])
                P.op("dve", lambda: nc.vector.tensor_tensor(out=Sst[:].rearrange("p (e d) -> p e d", e=8), in0=Sst[:].rearrange("p (e d) -> p e d", e=8),
                                                            in1=cds[:].unsqueeze(2).to_broadcast([128, 8, 64]), op=ALU.mult),
                     r=["Sst", ("cdsel", i2, 0), ("cdsel", i2, 1), "prevb"], w=["Sst"])
                P.op("dve", lambda: nc.vector.tensor_tensor(out=Sst[:], in0=Sst[:], in1=bank[4][:], op=ALU.add), r=["Sst", "b4", "b4s0", "b4s1"], w=["Sst"])
                yield
                if t > 0:
                    def mmY():
                        for h in range(16):
                            ins = nc.tensor.matmul(bank[2 + h // 8][:, (h % 8) * 64:(h % 8 + 1) * 64], lhsT=MTc[:, h, :], rhs=xd[:, h * 64:(h + 1) * 64],
                                                   start=True, stop=True)
                        return ins
                    P.op("pe", mmY, r=[("MT", i2, 0), ("MT", i2, 1), kxd], w=["b2", "b3"])

                    for g in range(2):
                        P.op("pe", lambda g=g: nc.tensor.matmul(bank[5 + g][:], lhsT=xb[g * 64:(g + 1) * 64, 9, :], rhs=prevb[g * 64:(g + 1) * 64, :],
                                                                start=True, stop=True), r=[kxb, "prevb"], w=["b%d" % (5 + g)])
                    for g in range(2):
                        P.op("act", lambda g=g: nc.scalar.copy(out=yd[:, g * 512:(g + 1) * 512], in_=bank[2 + g][:]), r=["b%d" % (2 + g)], w=[("yd", g)])
                        P.op("dve", lambda g=g: nc.vector.tensor_tensor(
                            out=yy[:, g * 512:(g + 1) * 512].rearrange("p (e d) -> p e d", e=8),
                            in0=bank[5 + g][:].rearrange("p (e d) -> p e d", e=8),
                            in1=ea[:, g * 8:(g + 1) * 8].unsqueeze(2).to_broadcast([128, 8, 64]), op=ALU.mult),
                            r=["b%d" % (5 + g), kea], w=[("yy", g)])
                    yield
                    for g in range(2):
                        P.op("pool", lambda g=g: nc.gpsimd.tensor_tensor(out=yy[:, g * 512:(g + 1) * 512], in0=yy[:, g * 512:(g + 1) * 512],
                                                                        in1=yd[:, g * 512:(g + 1) * 512], op=ALU.add),
                             r=[("yy", g), ("yd", g)], w=[("yy", g)])
                        P.op("dve", lambda g=g: nc.vector.tensor_tensor(
                            out=yd[:, g * 512:(g + 1) * 512].rearrange("p (e d) -> p e d", e=8),
                            in0=xtk[:, g * 512:(g + 1) * 512].rearrange("p (e d) -> p e d", e=8),
                            in1=dsk_bc[:, g * 8:(g + 1) * 8].unsqueeze(2).to_broadcast([128, 8, 64]), op=ALU.mult),
                            r=[kxt, "dsk", ("yy", g)], w=[("yd", g)])
                        P.op("pool", lambda g=g: nc.gpsimd.tensor_tensor(out=yy[:, g * 512:(g + 1) * 512], in0=yy[:, g * 512:(g + 1) * 512],
                                                                        in1=yd[:, g * 512:(g + 1) * 512], op=ALU.add),
                             r=[("yy", g), ("yd", g)], w=[("yy", g)])
                        P.op("dve", lambda g=g: nc.vector.tensor_tensor(out=yy[:, g * 512:(g + 1) * 512], in0=yy[:, g * 512:(g + 1) * 512],
                                                                       in1=zsc[:, g * 512:(g + 1) * 512], op=ALU.mult),
                             r=[("yy", g), kzs], w=[("yy", g)])
                        P.op("act", lambda g=g: nc.scalar.activation(out=junk[:, g * 512:(g + 1) * 512], in_=yy[:, g * 512:(g + 1) * 512],
                                                                     func=AF.Square, accum_out=ssg[:, g:g + 1]), r=[("yy", g)], w=["junk", ("ssg", g)])
                        rstd_ops(ssg[:, g:g + 1], ssg[:, 2 + g:3 + g], 512, ("ssg", g), ("rsg", g))
                        P.op("dve", lambda g=g: nc.vector.scalar_tensor_tensor(out=osm[:, g * 512:(g + 1) * 512], in0=yy[:, g * 512:(g + 1) * 512],
                                                                              scalar=ssg[:, 2 + g:3 + g], in1=gss[:, g * 512:(g + 1) * 512],
                                                                              op0=ALU.mult, op1=ALU.mult), r=[("yy", g), ("rsg", g), "gss"], w=[("osm", g)])
                        yield
                    def tr_o():
                        for c in range(KC):
                            ins = nc.tensor.transpose(out=bfv(0)[:, c * 128:(c + 1) * 128], in_=osm[:, c * 128:(c + 1) * 128], identity=ident[:])
                        return ins
                    P.op("pe", tr_o, r=[("osm", 0), ("osm", 1), "ident"], w=["b0"])
                    P.op("act", lambda: nc.scalar.copy(out=osmT[:].rearrange("p c t -> p (c t)"), in_=bfv(0)), r=["b0"], w=["osmT"])
                    P.dma("sp", lambda: nc.sync.dma_start(out=sc_osT[t * 128:(t + 1) * 128, :], in_=osmT[:].rearrange("p c t -> p (c t)")),
                          r=["osmT"], w=[("sc_osT", t)])
                yield

            for n in range(NT + 2):
                gens = []
                if n - 2 >= 0:
                    gens.append(stageC(n - 2))
                if 0 <= n - 1 < NT:
                    gens.append(stageB(n - 1))
                if n < NT:
                    gens.append(stageA(n))
                while gens:
                    for g_ in list(gens):
                        try:
                            next(g_)
                        except StopIteration:
                            gens.remove(g_)
            P.flush()
            if STOP <= 1:
                return nc

        def load_flat(dst2, src2, ncols, stg, key):
            P.dma("pool", lambda: nc.gpsimd.dma_start(out=dst2[:, 0:ncols], in_=src2[:, 0:ncols]), w=[key])

        groups = [[0]] + [list(range(g0, min(g0 + 4, NT))) for g0 in range(1, NT, 4)]

        with ExitStack() as e23:
            o_attnT = P.sb(e23, [128, KC, TOK], BF16)
            with ExitStack() as e2:
                s2 = lambda shape, dt: P.sb(e2, shape, dt)
                stg = [s2([128, 512], F32) for _ in range(2)]
                wqa = s2([128, 2, 16, 128], BF16)
                wqs = s2([128, 2, 16, 32], BF16)
                wkb = s2([128, 16, 64], BF16)
                wvb = s2([128, 16, 64], BF16)
                load_flat(wqa[:].rearrange("p c h m -> p (c h m)"), w_uqa.rearrange("p c h m -> p (c h m)"), 4096, stg, "wqa")
                load_flat(wqs[:].rearrange("p c h m -> p (c h m)"), w_uqs.rearrange("p c h m -> p (c h m)"), 1024, stg, "wqs")
                load_flat(wkb[:].rearrange("p h m -> p (h m)"), w_uk.rearrange("p h m -> p (h m)"), 1024, stg, "wkb")
                load_flat(wvb[:].rearrange("p h m -> p (h m)"), w_uv.rearrange("p h m -> p (h m)"), 1024, stg, "wvb")
                c2b = s2([32, TOK], BF16)
                s2b = s2([32, TOK], BF16)
                for o in range(0, TOK, 512):
                    wd = min(512, TOK - o)
                    for dst, src, k in ((c2b, c2T, "c2b"), (s2b, s2T, "s2b")):
                        i = ldc[0] % 2
                        ldc[0] += 1
                        st = stg[i]
                        P.dma("sp", lambda o=o, wd=wd, st=st, src=src: nc.sync.dma_start(out=st[0:32, 0:wd], in_=src[:, o:o + wd]), w=[("stg", i)])
                        P.op("dve", lambda o=o, wd=wd, st=st, dst=dst: nc.vector.tensor_copy(out=dst[:, o:o + wd], in_=st[0:32, 0:wd]),
                             r=[("stg", i)], w=[k])
                cqnT = s2([128, 2, TOK], BF16)
                ckvnT = s2([128, TOK], BF16)
                P.dma("sp", lambda: nc.sync.dma_start(out=cqnT[:, 0, :], in_=sc_cq[:, 0, :]), w=["cqnT0"])
                P.dma("sp", lambda: nc.sync.dma_start(out=cqnT[:, 1, :], in_=sc_cq[:, 1, :]), w=["cqnT1"])
                P.dma("sp", lambda: nc.sync.dma_start(out=ckvnT[:], in_=sc_ckv), w=["ckvnT"])
                KT = [s2([128, TOK], BF16) for _ in range(2)]
                QT = [s2([128, TOK], BF16) for _ in range(2)]
                Vaug = [s2([128, NT, 128], BF16) for _ in range(2)]
                PT = [s2([128, 512], BF16) for _ in range(3)]
                qr1 = s2([32, 512], F32)
                qr2 = s2([32, 512], F32)
                rrow = s2([128, 512], F32)
                bcs = s2([128, 512], F32)
                for par in range(2):
                    oc = 64 if par == 0 else 0
                    P.op("pool", lambda par=par: nc.gpsimd.memset(KT[par][:], 0.0), w=[("KT", par)])
                    P.op("pool", lambda par=par: nc.gpsimd.memset(QT[par][:], 0.0), w=[("QT", par)])
                    P.dma("sp", lambda par=par: nc.sync.dma_start(out=KT[par][0:32, :], in_=sc_kr), r=[("KT", par)], w=[("KT", par)])
                    P.op("pool", lambda par=par: nc.gpsimd.memset(Vaug[par][:], 0.0), w=[("Vaug", par)])
                    P.op("pool", lambda par=par, oc=oc: nc.gpsimd.memset(Vaug[par][:, :, oc:oc + 1], 1.0), r=[("Vaug", par)], w=[("Vaug", par)])
                    P.op("pool", lambda par=par, oc=oc: nc.gpsimd.memset(Vaug[par][0:112, 0, oc:oc + 1], 0.0), r=[("Vaug", par)], w=[("Vaug", par)])
                P.op("dve", lambda: nc.vector.memset(rrow[:], 1.0), w=["rrow"])
                NG = len(groups)

                def proj_items(h):
                    par = h % 2
                    voff = 0 if par == 0 else 64
                    items = []
                    for gi, grp in enumerate(groups):
                        c0, c1 = grp[0] * 128, (grp[-1] + 1) * 128
                        n = c1 - c0

                        def itemKQ(h=h, par=par, gi=gi, c0=c0, c1=c1, n=n):
                            P.op("pe", lambda: nc.tensor.matmul(bank[5][64:128, 0:n], lhsT=wkb[:, h, :], rhs=ckvnT[:, c0:c1], start=True, stop=True),
                                 r=["wkb", "ckvnT"], w=["b5"])
                            P.op("dve", lambda: nc.vector.tensor_copy(out=KT[par][64:128, c0:c1], in_=bank[5][64:128, 0:n]),
                                 r=["b5", ("KT", par)], w=[("KTg", par, gi)])

                            def mmQ():
                                for c in range(2):
                                    nc.tensor.matmul(bank[6][:, 0:n], lhsT=wqa[:, c, h, :], rhs=cqnT[:, c, c0:c1], start=(c == 0), stop=(c == 1))
                                for c in range(2):
                                    ins = nc.tensor.matmul(bank[7][0:32, 0:n], lhsT=wqs[:, c, h, :], rhs=cqnT[:, c, c0:c1], start=(c == 0), stop=(c == 1))
                                return ins
                            P.op("pe", mmQ, r=["wqa", "wqs", "cqnT0", "cqnT1"], w=["b6", "b7"])
                            P.op("dve", lambda: nc.vector.tensor_copy(out=QT[par][64:128, c0:c1], in_=bank[6][64:128, 0:n]),
                                 r=["b6", ("QT", par)], w=[("QTa", par, gi)])
                            P.op("dve", lambda: nc.vector.tensor_tensor(out=qr1[:, 0:n], in0=bank[6][0:32, 0:n], in1=c2b[:, c0:c1], op=ALU.mult),
                                 r=["b6", "c2b"], w=["qr1"])
                            P.op("dve", lambda: nc.vector.tensor_tensor(out=qr2[:, 0:n], in0=bank[7][0:32, 0:n], in1=s2b[:, c0:c1], op=ALU.mult),
                                 r=["b7", "s2b"], w=["qr2"])
                            P.op("pool", lambda: nc.gpsimd.tensor_tensor(out=QT[par][0:32, c0:c1], in0=qr1[:, 0:n], in1=qr2[:, 0:n], op=ALU.add),
                                 r=["qr1", "qr2", ("QT", par)], w=[("QTb", par, gi)])
                        items.append(itemKQ)
                    for t0 in range(0, NT, 8):
                        tn = min(8, NT - t0)

                        def itemV(h=h, par=par, voff=voff, t0=t0, tn=tn):
                            def mmV():
                                for j in range(tn):
                                    ins = nc.tensor.matmul(bank[5][:, j * 64:(j + 1) * 64], lhsT=ckvnT[:, (t0 + j) * 128:(t0 + j + 1) * 128], rhs=wvb[:, h, :],
                                                           start=True, stop=True)
                                return ins
                            P.op("pe", mmV, r=["wvb", "ckvnT"], w=["b5"])
                            P.op("dve", lambda: nc.vector.tensor_copy(
                                out=Vaug[par][:, t0:t0 + tn, voff:voff + 64], in_=bank[5][:, 0:tn * 64].rearrange("p (t d) -> p t d", t=tn)),
                                r=["b5", ("Vaug", par)], w=[("Vaug", par)])
                        items.append(itemV)
                    return items

                def kq_keys(par):
                    return [("KT", par), ("QT", par)] + [("KTg", par, gi) for gi in range(NG)] + [("QTa", par, gi) for gi in range(NG)] + \
                           [("QTb", par, gi) for gi in range(NG)]

                its = []
                octr = 0
                for h in range(16):
                    for gi, grp in enumerate(groups):
                        ob = 3 + (octr % 2)
                        octr += 1
                        for kt in range(grp[-1] + 1):
                            its.append((h, gi, kt, ob))
                NI = len(its)
                LOOK = 2
                pend = {h: proj_items(h) for h in range(16)}
                for it_ in pend[0]:
                    it_()
                pend[0] = []
                deferred = []

                def emit_S(idx):
                    h, gi, kt, ob = its[idx]
                    par = h % 2
                    if pend[h]:
                        for it_ in pend[h]:
                            it_()
                        pend[h] = []
                    grp = groups[gi]
                    q0, q1 = grp[0] * 128, (grp[-1] + 1) * 128
                    j = max(0, kt - grp[0])
                    qs = q0 + j * 128
                    n = q1 - qs
                    sbk = idx % 3
                    P.op("pe", lambda: nc.tensor.matmul(bank[sbk][:, 0:n], lhsT=KT[par][:, kt * 128:(kt + 1) * 128], rhs=QT[par][:, qs:q1],
                                                        start=True, stop=True), r=kq_keys(par), w=["b%d" % sbk])

                def emit_rest(idx):
                    h, gi, kt, ob = its[idx]
                    par = h % 2
                    grp = groups[gi]
                    q0, q1 = grp[0] * 128, (grp[-1] + 1) * 128
                    nq = q1 - q0
                    j = max(0, kt - grp[0])
                    n = q1 - (q0 + j * 128)
                    sbk = idx % 3
                    M = 65 if par == 0 else 128
                    last_kt = grp[-1]
                    P.op("act", lambda: nc.scalar.activation(out=PT[sbk][:, 0:n], in_=bank[sbk][:, 0:n], func=AF.Exp),
                         r=["b%d" % sbk], w=[("PT", sbk)])
                    if kt >= grp[0]:
                        P.op("dve", lambda: nc.vector.tensor_tensor(out=PT[sbk][:, 0:128], in0=PT[sbk][:, 0:128], in1=mask01[:], op=ALU.mult),
                             r=[("PT", sbk), "mask01"], w=[("PT", sbk)])
                    assert all(d[2] != ob for d in deferred), "pending normalisation on the accumulator bank"
                    P.op("pe", lambda: nc.tensor.matmul(bank[ob][0:M, j * 128:nq], lhsT=Vaug[par][:, kt, 0:M], rhs=PT[sbk][:, 0:n],
                                                        start=(kt == 0), stop=(kt == last_kt)),
                         r=[("PT", sbk), ("Vaug", par)], w=["b%d" % ob])
                    if kt == last_kt:
                        dr = 64 if par == 0 else 0
                        r0, r1 = (0, 64) if par == 0 else (64, 128)
                        P.op("dve", lambda: nc.vector.tensor_scalar(out=rrow[dr:dr + 1, 0:nq], in0=bank[ob][dr:dr + 1, 0:nq],
                                                                    scalar1=1e-30, scalar2=None, op0=ALU.max), r=["b%d" % ob], w=["rrow"])
                        P.op("dve", lambda: nc.vector.reciprocal(out=rrow[dr:dr + 1, 0:nq], in_=rrow[dr:dr + 1, 0:nq]), r=["rrow"], w=["rrow"])

                        def fin():
                            P.op("pe", lambda: nc.tensor.matmul(bank[7][0:r1, 0:nq], lhsT=ones_f[dr:dr + 1, 0:r1], rhs=rrow[dr:dr + 1, 0:nq],
                                                                start=True, stop=True), r=["rrow", "ones_f"], w=["b7"])
                            P.op("dve", lambda: nc.vector.tensor_copy(out=bcs[r0:r1, 0:nq], in_=bank[7][r0:r1, 0:nq]), r=["b7"], w=["bcs"])
                            P.op("dve", lambda: nc.vector.tensor_tensor(out=o_attnT[r0:r1, h // 2, q0:q1], in0=bank[ob][r0:r1, 0:nq], in1=bcs[r0:r1, 0:nq],
                                                                        op=ALU.mult), r=["b%d" % ob, "bcs"], w=[("oT", h, gi)])
                        deferred.append((idx + 1, fin, ob))

                cast_items = []
                CR = 256
                for r0_ in range(0, NEXP * 128, CR):
                    cast_items.append(lambda r0_=r0_: P.dma("pool", lambda: nc.gpsimd.dma_start(out=sc_wc[r0_:r0_ + CR, 0:WGU_C], in_=w_gu[r0_:r0_ + CR, :]),
                                                            w=[("sc_wgu", r0_)]))
                    cast_items.append(lambda r0_=r0_: P.dma("pool", lambda: nc.gpsimd.dma_start(out=sc_wc[r0_:r0_ + CR, WGU_C:WC], in_=w_dn[r0_:r0_ + CR, :]),
                                                            w=[("sc_wdn", r0_)]))
                cast_every = max(1, (NI - 8) // len(cast_items))
                head_start = {}
                for idx, (h, gi, kt, ob) in enumerate(its):
                    head_start.setdefault(h, idx)
                for idx in range(NI + LOOK):
                    if idx < NI:
                        emit_S(idx)
                    jdx = idx - LOOK
                    if jdx >= 0:
                        emit_rest(jdx)
                        while deferred and deferred[0][0] <= jdx:
                            deferred.pop(0)[1]()
                        h = its[jdx][0]
                        loc = jdx - head_start[h]
                        if h + 1 < 16 and loc >= 4 and (loc - 4) % 16 == 0 and pend[h + 1]:
                            pend[h + 1].pop(0)()
                        if cast_items and jdx % cast_every == 0:
                            cast_items.pop(0)()
                for d in deferred:
                    d[1]()
                for ci in cast_items:
                    ci()
                P.flush()
                if STOP <= 2:
                    return nc

            with ExitStack() as e3:
                s3 = lambda shape, dt: P.sb(e3, shape, dt)
                wg = s3([128, KC, 2048], BF16)
                wba = s3([128, KC, D], BF16)
                wbs = s3([128, KC, D], BF16)
                wo = s3([128, KC, D], BF16)
                wrb = s3([128, KC, 36], BF16)
                with ExitStack() as eL:
                    stg = [P.sb(eL, [128, 512], F32) for _ in range(2)]
                    load_w(wg, w_in, 2048, OFF_GA, stg, "wg")
                    load_w(wba, w_ba, D, 0, stg, "wba")
                    load_w(wbs, w_bs, D, 0, stg, "wbs")
                    load_w(wo, w_out, D, 0, stg, "wo")
                    load_w(wrb, w_r, 36, 0, stg, "wrb")
                    P.flush()
                gmix = s3([128, D], F32)
                gffn = s3([128, D], F32)
                brt = s3([128, 36], F32)
                ustr = s3([128, 128], BF16)
                for dst, src, k in ((gmix, g_mix, "gmix"), (gffn, g_ffn, "gffn"), (brt, b_r, "brt")):
                    P.dma("sp", lambda dst=dst, src=src: nc.sync.dma_start(out=dst[:], in_=src), w=[k])
                P.op("pool", lambda: nc.gpsimd.affine_select(out=ustr[:], in_=ones_b[:], pattern=[[1, 128]], compare_op=ALU.is_gt, fill=0.0,
                                                              base=0, channel_multiplier=-1), r=["ones_b"], w=["ustr"])
                P.op("dve", lambda: nc.vector.memset(Macc[:], 0.0), w=["Macc"])
                xt = [s3([128, D], F32)] * 2
                u = s3([128, D], BF16)
                uT = s3([128, KC, 128], BF16)
                osT = s3([128, KC, 128], BF16)
                st8 = s3([128, 16], F32)
                sga = s3([128, D], F32)
                sgs = s3([128, D], F32)
                mrb = s3([128, D], BF16)
                mT = s3([128, KC, 128], BF16)
                h1 = s3([128, D], F32)
                vh = s3([128, D], BF16)
                vT = s3([128, KC, 128], BF16)
                lg = s3([128, 36], F32)
                sm = s3([128, 16], F32)
                pen = s3([128, 4], F32)
                goh = s3([128, 4], F32)
                elm = s3([128, 32], F32)
                elm2 = s3([128, 32], F32)
                Mt = s3([128, 32], F32)
                Mtb = s3([128, 32], BF16)
                Maccb = s3([128, 32], BF16)

                for t in range(1, NT):
                    r_ = t - 1
                    xcur = xt[t % 2]
                    kx = "xt3"
                    P.dma("sp", lambda t=t, xcur=xcur: nc.sync.dma_start(out=xcur[:], in_=x[(t - 1) * 128:t * 128, :]), w=[kx])
                    P.dma("sp", lambda t=t: nc.sync.dma_start(out=osT[:].rearrange("p c t -> p (c t)"), in_=sc_osT[t * 128:(t + 1) * 128, :]), w=["osT"])
                    P.op("act", lambda xcur=xcur: nc.scalar.activation(out=mrb[:], in_=xcur[:], func=AF.Square, accum_out=st8[:, 0:1]),
                         r=[kx], w=[("mrb", 0), ("mrb", 1), "ssx"])
                    rstd_ops(st8[:, 0:1], st8[:, 1:2], D, "ssx", "rsx")
                    P.op("dve", lambda xcur=xcur: nc.vector.scalar_tensor_tensor(out=u[:], in0=xcur[:], scalar=st8[:, 1:2], in1=gmix[:],
                                                                                op0=ALU.mult, op1=ALU.mult), r=[kx, "rsx", "gmix"], w=["u"])

                    def tr8(src, bnk):
                        def f():
                            for c in range(KC):
                                ins = nc.tensor.transpose(out=bfv(bnk)[:, c * 128:(c + 1) * 128], in_=src[:, c * 128:(c + 1) * 128], identity=ident[:])
                            return ins
                        return f
                    P.op("pe", tr8(u, 0), r=["u", "ident"], w=["b0"])
                    P.op("act", lambda: nc.scalar.copy(out=uT[:].rearrange("p c t -> p (c t)"), in_=bfv(0)), r=["b0"], w=["uT"])

                    def mmG():
                        for q4 in range(4):
                            for c in range(KC):
                                ins = nc.tensor.matmul(bank[1 + q4][:], lhsT=uT[:, c, :], rhs=wg[:, c, q4 * 512:(q4 + 1) * 512], start=(c == 0), stop=(c == KC - 1))
                        return ins
                    P.op("pe", mmG, r=["uT", "wg"], w=["b1", "b2", "b3", "b4"])
                    for q4 in range(4):
                        dst = sga if q4 < 2 else sgs
                        P.op("act", lambda q4=q4, dst=dst: nc.scalar.activation(out=dst[:, (q4 % 2) * 512:(q4 % 2 + 1) * 512], in_=bank[1 + q4][:], func=AF.Sigmoid),
                             r=["b%d" % (1 + q4)], w=[("sg", q4)])

                    def mmBr(t=t):
                        for hf in range(2):
                            for c in range(KC):
                                nc.tensor.matmul(bank[5 + hf][:], lhsT=o_attnT[:, c, t * 128:(t + 1) * 128], rhs=wba[:, c, hf * 512:(hf + 1) * 512],
                                                 start=(c == 0), stop=(c == KC - 1))
                        for hf in range(2):
                            for c in range(KC):
                                ins = nc.tensor.matmul(bank[1 + hf][:], lhsT=osT[:, c, :], rhs=wbs[:, c, hf * 512:(hf + 1) * 512],
                                                       start=(c == 0), stop=(c == KC - 1))
                        return ins
                    P.op("pe", mmBr, r=["wba", "wbs", "osT"], w=["b5", "b6", "b1", "b2"])
                    for hf in range(2):
                        sl = slice(hf * 512, (hf + 1) * 512)
                        P.op("dve", lambda hf=hf, sl=sl: nc.vector.tensor_tensor(out=sga[:, sl], in0=bank[5 + hf][:], in1=sga[:, sl], op=ALU.mult),
                             r=["b%d" % (5 + hf), ("sg", hf)], w=[("sg", hf)])
                        P.op("dve", lambda hf=hf, sl=sl: nc.vector.tensor_tensor(out=sgs[:, sl], in0=bank[1 + hf][:], in1=sgs[:, sl], op=ALU.mult),
                             r=["b%d" % (1 + hf), ("sg", 2 + hf)], w=[("sg", 2 + hf)])
                        P.op("pool", lambda hf=hf, sl=sl: nc.gpsimd.tensor_tensor(out=mrb[:, sl], in0=sga[:, sl], in1=sgs[:, sl], op=ALU.add),
                             r=[("sg", hf), ("sg", 2 + hf)], w=[("mrb", hf)])
                    P.op("pe", tr8(mrb, 0), r=[("mrb", 0), ("mrb", 1), "ident"], w=["b0"])
                    P.op("act", lambda: nc.scalar.copy(out=mT[:].rearrange("p c t -> p (c t)"), in_=bfv(0)), r=["b0"], w=["mT"])

                    def mmO():
                        for hf in range(2):
                            for c in range(KC):
                                ins = nc.tensor.matmul(bank[3 + hf][:], lhsT=mT[:, c, :], rhs=wo[:, c, hf * 512:(hf + 1) * 512], start=(c == 0), stop=(c == KC - 1))
                        return ins
                    P.op("pe", mmO, r=["mT", "wo"], w=["b3", "b4"])
                    for hf in range(2):
                        sl = slice(hf * 512, (hf + 1) * 512)
                        P.op("dve", lambda hf=hf, sl=sl, xcur=xcur: nc.vector.tensor_tensor(out=h1[:, sl], in0=bank[3 + hf][:], in1=xcur[:, sl], op=ALU.add),
                             r=["b%d" % (3 + hf), kx], w=[("h1", hf)])
                    hk = [("h1", 0), ("h1", 1)]
                    P.dma("pool", lambda t=t: nc.gpsimd.dma_start(out=sc_h1[t * 128:(t + 1) * 128, :], in_=h1[:]), r=hk, w=[("sc_h1", t)])
                    P.op("act", lambda: nc.scalar.activation(out=u[:], in_=h1[:], func=AF.Square, accum_out=st8[:, 2:3]), r=hk, w=["u", "ssh"])
                    rstd_ops(st8[:, 2:3], st8[:, 3:4], D, "ssh", "rsh")
                    P.op("dve", lambda: nc.vector.scalar_tensor_tensor(out=vh[:], in0=h1[:], scalar=st8[:, 3:4], in1=gffn[:], op0=ALU.mult, op1=ALU.mult),
                         r=hk + ["rsh", "gffn"], w=["vh"])
                    P.dma("pool", lambda t=t: nc.gpsimd.dma_start(out=sc_vh[t * 128:(t + 1) * 128, :], in_=vh[:]), r=["vh"], w=[("sc_vh", t)])
                    P.op("pe", tr8(vh, 0), r=["vh", "ident"], w=["b0"])
                    P.op("act", lambda: nc.scalar.copy(out=vT[:].rearrange("p c t -> p (c t)"), in_=bfv(0)), r=["b0"], w=["vT"])

                    def mmR():
                        for c in range(KC):
                            ins = nc.tensor.matmul(bank[7][:, 0:36], lhsT=vT[:, c, :], rhs=wrb[:, c, :], start=(c == 0), stop=(c == KC - 1))
                        return ins
                    P.op("pe", mmR, r=["vT", "wrb"], w=["b7"])
                    P.op("dve", lambda: nc.vector.tensor_tensor(out=lg[:], in0=bank[7][:, 0:36], in1=brt[:], op=ALU.add), r=["b7", "brt"], w=["lg"])
                    V = nc.vector
                    P.op("dve", lambda: V.reduce_max(out=sm[:, 0:1], in_=lg[:, 0:4], axis=AX.X), r=["lg"], w=["gmax"])
                    P.op("dve", lambda: V.tensor_scalar(out=goh[:], in0=lg[:, 0:4], scalar1=sm[:, 0:1], scalar2=None, op0=ALU.is_equal), r=["lg", "gmax"], w=["goh"])
                    P.op("dve", lambda: V.tensor_scalar(out=sm[:, 1:2], in0=sm[:, 0:1], scalar1=-1.0, scalar2=None, op0=ALU.mult), r=["gmax"], w=["ngmax"])
                    P.op("act", lambda: nc.scalar.activation(out=pen[:], in_=lg[:, 0:4], func=AF.Exp, bias=sm[:, 1:2], accum_out=sm[:, 2:3]),
                         r=["lg", "ngmax", "goh"], w=["pen", "gsum"])
                    P.op("dve", lambda: V.reciprocal(out=sm[:, 3:4], in_=sm[:, 2:3]), r=["gsum"], w=["pg"])
                    P.op("dve", lambda: V.tensor_scalar(out=pen[:], in0=goh[:], scalar1=-1.0, scalar2=1e9, op0=ALU.add, op1=ALU.mult), r=["goh", "pen"], w=["pen2"])
                    P.op("dve", lambda: V.tensor_tensor(out=elm[:].rearrange("p (g e) -> p g e", g=4), in0=lg[:, 4:36].rearrange("p (g e) -> p g e", g=4),
                                                        in1=pen[:].unsqueeze(2).to_broadcast([128, 4, 8]), op=ALU.add), r=["lg", "pen2"], w=["elm"])
                    P.op("dve", lambda: V.reduce_max(out=sm[:, 4:5], in_=elm[:], axis=AX.X), r=["elm"], w=["m1"])
                    P.op("dve", lambda r_=r_: V.tensor_scalar(out=oh1[:, r_, :], in0=elm[:], scalar1=sm[:, 4:5], scalar2=None, op0=ALU.is_equal),
                         r=["elm", "m1"], w=[("oh1", r_)])
                    P.op("dve", lambda r_=r_: V.scalar_tensor_tensor(out=elm2[:], in0=oh1[:, r_, :], scalar=-1e9, in1=elm[:], op0=ALU.mult, op1=ALU.add),
                         r=[("oh1", r_), "elm"], w=["elm2"])
                    P.op("dve", lambda: V.reduce_max(out=sm[:, 5:6], in_=elm2[:], axis=AX.X), r=["elm2"], w=["m2"])
                    P.op("dve", lambda r_=r_: V.tensor_scalar(out=oh2[:, r_, :], in0=elm2[:], scalar1=sm[:, 5:6], scalar2=None, op0=ALU.is_equal),
                         r=["elm2", "m2"], w=[("oh2", r_)])
                    P.op("dve", lambda: V.tensor_tensor(out=sm[:, 6:7], in0=sm[:, 5:6], in1=sm[:, 4:5], op=ALU.subtract), r=["m1", "m2"], w=["dd"])
                    P.op("act", lambda: nc.scalar.activation(out=sm[:, 7:8], in_=sm[:, 6:7], func=AF.Exp), r=["dd"], w=["e2"])
                    P.op("dve", lambda: V.tensor_scalar(out=sm[:, 8:9], in0=sm[:, 7:8], scalar1=1.0, scalar2=None, op0=ALU.add), r=["e2"], w=["t1"])
                    P.op("dve", lambda: V.reciprocal(out=sm[:, 9:10], in_=sm[:, 8:9]), r=["t1"], w=["rt"])
                    P.op("dve", lambda r_=r_: V.tensor_tensor(out=wts[:, r_, 0:1], in0=sm[:, 9:10], in1=sm[:, 3:4], op=ALU.mult), r=["rt", "pg"], w=[("w1", r_)])
                    P.op("dve", lambda r_=r_: V.tensor_tensor(out=wts[:, r_, 1:2], in0=sm[:, 3:4], in1=wts[:, r_, 0:1], op=ALU.subtract),
                         r=["pg", ("w1", r_)], w=[("w2", r_)])
                    P.op("dve", lambda r_=r_: V.tensor_tensor(out=Mt[:], in0=oh1[:, r_, :], in1=oh2[:, r_, :], op=ALU.add), r=[("oh1", r_), ("oh2", r_)], w=["Mt"])
                    P.op("dve", lambda: V.tensor_copy(out=Mtb[:], in_=Mt[:]), r=["Mt"], w=["Mtb"])
                    P.op("dve", lambda: V.tensor_copy(out=Maccb[:], in_=Macc[:]), r=["Macc"], w=["Maccb"])

                    def mmRk():
                        nc.tensor.matmul(bank[7][:, 64:96], lhsT=ustr[:], rhs=Mtb[:], start=True, stop=False)
                        return nc.tensor.matmul(bank[7][:, 64:96], lhsT=ones_b[:], rhs=Maccb[:], start=False, stop=True)
                    P.op("pe", mmRk, r=["ustr", "Mtb", "ones_b", "Maccb", "lg"], w=["b7"])
                    P.op("dve", lambda r_=r_: V.tensor_copy(out=rank[:, r_, :], in_=bank[7][:, 64:96]), r=["b7"], w=[("rank", r_)])
                    P.op("dve", lambda: V.tensor_tensor(out=Macc[:], in0=Macc[:], in1=Mt[:], op=ALU.add), r=["Macc", "Mt", "Maccb"], w=["Macc"])
                P.flush()
                if STOP <= 3:
                    return nc
        with ExitStack() as e4:
            s4 = lambda shape, dt: P.sb(e4, shape, dt)
            V = nc.vector
            cnt = s4([128, 32], F32)
            cnti = s4([128, 32], I32)
            pcf = s4([128, 32], F32)
            incl = s4([128, 32], F32)
            offx = s4([128, 32], F32)
            pos = s4([128, NR, 32], F32)
            tmp = s4([128, NR, 32], F32)
            slf = s4([128, 2, NR], F32)
            sli = s4([128, 2, NR], I32)
            thr = s4([128, NSLOT], F32)
            cmpt = s4([128, NSLOT, 32], F32)
            eidf = s4([128, NSLOT], F32)
            pidx = s4([128, 1], F32)
            widx = s4([128, NSLOT], I32)
            Maccb = s4([128, 32], BF16)
            P.op("dve", lambda: V.tensor_copy(out=Maccb[:], in_=Macc[:]), w=["Maccb"])
            P.op("pe", lambda: nc.tensor.matmul(bank[0][:, 0:32], lhsT=ones_b[:], rhs=Maccb[:], start=True, stop=True), r=["Maccb"], w=["b0"])
            P.op("dve", lambda: V.tensor_copy(out=cnt[:], in_=bank[0][:, 0:32]), r=["b0"], w=["cnt"])
            P.op("dve", lambda: V.tensor_copy(out=cnti[:], in_=cnt[:]), r=["cnt"], w=["cnti"])
            P.op("dve", lambda: V.tensor_single_scalar(out=cnti[:], in_=cnti[:], scalar=127, op=ALU.add), r=["cnti"], w=["cnti"])
            P.op("dve", lambda: V.tensor_scalar(out=cnti[:], in0=cnti[:], scalar1=7, scalar2=7, op0=ALU.arith_shift_right, op1=ALU.logical_shift_left),
                 r=["cnti"], w=["cnti"])
            P.op("dve", lambda: V.tensor_copy(out=pcf[:], in_=cnti[:]), r=["cnti"], w=["pcf"])
            P.op("dve", lambda: V.tensor_tensor_scan(out=incl[:], data0=ones_f[:, 0:32], data1=pcf[:], initial=0.0, op0=ALU.mult, op1=ALU.add),
                 r=["pcf"], w=["incl"])
            P.op("dve", lambda: V.tensor_tensor(out=offx[:], in0=incl[:], in1=pcf[:], op=ALU.subtract), r=["incl", "pcf"], w=["offx"])
            P.op("dve", lambda: V.tensor_tensor(out=pos[:], in0=rank[:], in1=offx[:].unsqueeze(1).to_broadcast([128, NR, 32]), op=ALU.add), r=["offx"], w=["pos"])
            for k, ohk in enumerate((oh1, oh2)):
                P.op("dve", lambda ohk=ohk: V.tensor_tensor(out=tmp[:], in0=pos[:], in1=ohk[:], op=ALU.mult), r=["pos", "slf"], w=["tmp"])
                P.op("dve", lambda k=k: V.reduce_sum(out=slf[:, k, :], in_=tmp[:], axis=AX.X), r=["tmp"], w=["slf"])
            P.op("dve", lambda: V.tensor_copy(out=sli[:], in_=slf[:]), r=["slf"], w=["sli"])
            P.op("pool", lambda: nc.gpsimd.iota(thr[:], pattern=[[128, NSLOT]], base=0, channel_multiplier=0, allow_small_or_imprecise_dtypes=True), w=["thr"])
            P.op("pool", lambda: nc.gpsimd.iota(pidx[:], pattern=[[0, 1]], base=-128, channel_multiplier=1, allow_small_or_imprecise_dtypes=True), w=["pidx"])
            P.op("dve", lambda: V.tensor_tensor(out=cmpt[:], in0=offx[:].unsqueeze(1).to_broadcast([128, NSLOT, 32]),
                                                in1=thr[:].unsqueeze(2).to_broadcast([128, NSLOT, 32]), op=ALU.is_le), r=["offx", "thr"], w=["cmpt"])
            P.op("dve", lambda: V.reduce_sum(out=eidf[:], in_=cmpt[:], axis=AX.X), r=["cmpt"], w=["eidf"])
            P.op("dve", lambda: V.tensor_scalar(out=eidf[:], in0=eidf[:], scalar1=128.0, scalar2=pidx[:, 0:1], op0=ALU.mult, op1=ALU.add),
                 r=["eidf", "pidx"], w=["eidf"])
            P.op("dve", lambda: V.tensor_copy(out=widx[:], in_=eidf[:]), r=["eidf"], w=["widx"])
            vt = [s4([128, D], BF16) for _ in range(2)]
            for r_ in range(NR):
                t = r_ + 1
                b = r_ % 2
                P.dma("sp", lambda t=t, b=b: nc.sync.dma_start(out=vt[b][:], in_=sc_vh[t * 128:(t + 1) * 128, :]), w=[("vt", b)])
                for k in range(2):
                    P.dma("pool", lambda r_=r_, k=k, b=b: nc.gpsimd.indirect_dma_start(
                        out=sc_xs, out_offset=bass.IndirectOffsetOnAxis(ap=sli[:, k, r_:r_ + 1], axis=0), in_=vt[b][:, :], in_offset=None),
                        r=[("vt", b), "sli"], w=[("sc_xs", r_, k)])
            NWB = 3
            wcat = [s4([128, WC], BF16) for _ in range(NWB)]
            NXB = 3
            xsl = [s4([128, D], BF16) for _ in range(NXB)]
            xsT = [s4([128, KC, 128], BF16) for _ in range(2)]
            sgt = s4([128, 256], F32)
            hh = [s4([128, 256], BF16) for _ in range(2)]
            hT = s4([128, 2, 128], BF16)
            ysb = [s4([128, D], F32) for _ in range(2)]
            scat_keys = [("sc_xs", rr, kk) for rr in range(NR) for kk in range(2)]

            def load_x(i):
                xb = i % NXB
                P.dma("sp", lambda: nc.sync.dma_start(out=xsl[xb][:], in_=sc_xs[i * 128:(i + 1) * 128, :]), r=scat_keys, w=[("xsl", xb)])

            def stage4A(i):
                b2, xb, wb = i % 2, i % NXB, i % NWB
                bx = 0 if b2 == 0 else 5
                bg = 1 if b2 == 0 else 6
                P.dma("pool", lambda: nc.gpsimd.indirect_dma_start(
                    out=wcat[wb][:, :], out_offset=None, in_=sc_wc, in_offset=bass.IndirectOffsetOnAxis(ap=widx[:, i:i + 1], axis=0)),
                    r=["widx"], w=[("wcat", wb)])
                if i + 2 < NSLOT:
                    load_x(i + 2)

                def trX():
                    for c in range(KC):
                        ins = nc.tensor.transpose(out=bfv(bx)[:, c * 128:(c + 1) * 128], in_=xsl[xb][:, c * 128:(c + 1) * 128], identity=ident[:])
                    return ins
                P.op("pe", trX, r=[("xsl", xb), "ident"], w=["b%d" % bx])
                P.op("dve", lambda: V.tensor_copy(out=xsT[b2][:].rearrange("p c t -> p (c t)"), in_=bfv(bx)), r=["b%d" % bx], w=[("xsT", b2)])
                yield

                def mmGU():
                    for c in range(KC):
                        ins = nc.tensor.matmul(bank[bg][:], lhsT=xsT[b2][:, c, :], rhs=wcat[wb][:, c * 512:(c + 1) * 512], start=(c == 0), stop=(c == KC - 1))
                    return ins
                P.op("pe", mmGU, r=[("xsT", b2), ("wcat", wb)], w=["b%d" % bg])
                P.op("act", lambda: nc.scalar.activation(out=sgt[:], in_=bank[bg][:, 0:256], func=AF.Silu), r=["b%d" % bg], w=["sgt"])
                P.op("dve", lambda: V.tensor_tensor(out=hh[b2][:], in0=bank[bg][:, 256:512], in1=sgt[:], op=ALU.mult), r=["b%d" % bg, "sgt"], w=[("hh", b2)])
                yield

            def stage4B(i):
                b2, wb = i % 2, i % NWB
                bh = 2 if b2 == 0 else 7

                def trH():
                    for c in range(2):
                        ins = nc.tensor.transpose(out=bfv(bh)[:, c * 128:(c + 1) * 128], in_=hh[b2][:, c * 128:(c + 1) * 128], identity=ident[:])
                    return ins
                P.op("pe", trH, r=[("hh", b2), "ident"], w=["b%d" % bh])
                P.op("act", lambda: nc.scalar.copy(out=hT[:].rearrange("p c t -> p (c t)"), in_=bfv(bh)[:, 0:256]), r=["b%d" % bh], w=["hT"])
                yield

                def mmD():
                    for hf in range(2):
                        for c in range(2):
                            ins = nc.tensor.matmul(bank[3 + hf][:], lhsT=hT[:, c, :], rhs=wcat[wb][:, WGU_C + c * D + hf * 512: WGU_C + c * D + (hf + 1) * 512],
                                                   start=(c == 0), stop=(c == 1))
                    return ins
                P.op("pe", mmD, r=["hT", ("wcat", wb)], w=["b3", "b4"])
                P.op("act", lambda: nc.scalar.copy(out=ysb[b2][:, 0:512], in_=bank[3][:]), r=["b3"], w=[("ysbA", b2)])
                P.op("dve", lambda: V.tensor_copy(out=ysb[b2][:, 512:1024], in_=bank[4][:]), r=["b4"], w=[("ysbB", b2)])
                P.dma("sp", lambda: nc.sync.dma_start(out=sc_ys[i * 128:(i + 1) * 128, :], in_=ysb[b2][:]), r=[("ysbA", b2), ("ysbB", b2)], w=[("sc_ys", i)])
                yield

            load_x(0)
            load_x(1)
            for n in range(NSLOT + 1):
                gens = []
                if n - 1 >= 0:
                    gens.append(stage4B(n - 1))
                if n < NSLOT:
                    gens.append(stage4A(n))
                while gens:
                    for g_ in list(gens):
                        try:
                            next(g_)
                        except StopIteration:
                            gens.remove(g_)
            P.flush()
            if STOP <= 4:
                return nc
            gfin = s4([128, D], F32)
            P.dma("sp", lambda: nc.sync.dma_start(out=gfin[:], in_=g_fin), w=["gfin"])
            NB5 = 4
            h1t = [s4([128, D], F32) for _ in range(NB5)]
            y1 = [s4([128, D], F32) for _ in range(NB5)]
            y2 = [s4([128, D], F32) for _ in range(NB5)]
            ot = [s4([128, D], F32) for _ in range(NB5)]
            junk5 = s4([128, D], BF16)
            st5 = s4([128, 4], F32)
            for r_ in range(NR):
                t = r_ + 1
                b = r_ % NB5
                P.dma("sp", lambda t=t, b=b: nc.sync.dma_start(out=h1t[b][:], in_=sc_h1[t * 128:(t + 1) * 128, :]), w=[("h1t", b)])
                for k, yk in enumerate((y1, y2)):
                    P.dma("pool", lambda r_=r_, k=k, b=b, yk=yk: nc.gpsimd.indirect_dma_start(
                        out=yk[b][:, :], out_offset=None, in_=sc_ys, in_offset=bass.IndirectOffsetOnAxis(ap=sli[:, k, r_:r_ + 1], axis=0)),
                        w=[("y", k, b)])
                P.op("dve", lambda r_=r_, b=b: V.scalar_tensor_tensor(out=ot[b][:], in0=y1[b][:], scalar=wts[:, r_, 0:1], in1=h1t[b][:], op0=ALU.mult, op1=ALU.add),
                     r=[("y", 0, b), ("h1t", b)], w=[("ot", b)])
                P.op("dve", lambda r_=r_, b=b: V.scalar_tensor_tensor(out=ot[b][:], in0=y2[b][:], scalar=wts[:, r_, 1:2], in1=ot[b][:], op0=ALU.mult, op1=ALU.add),
                     r=[("y", 1, b), ("ot", b)], w=[("ot", b)])
                P.op("act", lambda b=b: nc.scalar.activation(out=junk5[:], in_=ot[b][:], func=AF.Square, accum_out=st5[:, 0:1]), r=[("ot", b)], w=["junk5", "ss5"])
                rstd_ops(st5[:, 0:1], st5[:, 1:2], D, "ss5", "rs5")
                P.op("dve", lambda b=b: V.scalar_tensor_tensor(out=ot[b][:], in0=ot[b][:], scalar=st5[:, 1:2], in1=gfin[:], op0=ALU.mult, op1=ALU.mult),
                     r=[("ot", b), "rs5", "gfin"], w=[("ot", b)])
                P.dma("sp", lambda r_=r_, b=b: nc.sync.dma_start(out=y_out[r_ * 128:(r_ + 1) * 128, :], in_=ot[b][:]), r=[("ot", b)])
            P.flush()
    return nc


def _kc(w):
    K, N = w.shape
    return np.ascontiguousarray(w.reshape(K // 128, 128, N).transpose(1, 0, 2))


def _bc(v, n=128):
    return np.ascontiguousarray(np.broadcast_to(np.asarray(v, np.float32).reshape(1, -1), (n, v.size)))


def _rope_tables(NT):
    TOK = NT * 128
    pos = np.maximum(np.arange(TOK) - 112, 0).astype(np.float32)
    inv = (np.float32(10000.0) ** (-np.arange(0, 32, 2, dtype=np.float32) / np.float32(32))).astype(np.float32)
    ang = (pos[:, None] * inv[None, :]).astype(np.float32)
    cos, sin = np.cos(ang).astype(np.float32), np.sin(ang).astype(np.float32)
    C2 = np.concatenate([cos, cos], axis=1)
    S2 = np.concatenate([-sin, sin], axis=1)
    c2tok = np.ascontiguousarray(C2.reshape(NT, 128, 32).transpose(1, 0, 2))
    s2tok = np.ascontiguousarray(S2.reshape(NT, 128, 32).transpose(1, 0, 2))
    return c2tok, s2tok, np.ascontiguousarray(C2.T), np.ascontiguousarray(S2.T)


def prep_shared(inp, NT):
    f = lambda k: np.asarray(inp[k], np.float32)
    w_in = f("w_in")[0]
    cols = np.concatenate([np.arange(0, 416), np.arange(400, 416), np.arange(384, 400), np.arange(2720, 2736),
                           np.arange(416, 1440), np.arange(1440, 2720), np.arange(2736, 3760), np.arange(3760, 4784)])
    assert cols.size == W_IN_COLS
    m = {"w_in": _kc(w_in[:, cols]), "meta": f("meta_tokens")}
    m["g_mix"] = _bc(f("norm_mix")[0]); m["g_q"] = _bc(f("mla_q_norm")[0]); m["g_kv"] = _bc(f("mla_kv_norm")[0])
    m["g_ssm"] = _bc(f("ssm_norm")[0]); m["g_ffn"] = _bc(f("norm_ffn")[0]); m["g_fin"] = _bc(f("norm_final"))
    wq = f("mla_w_uq")[0].reshape(256, 16, 96)
    wqa = np.zeros((256, 16, 128), np.float32)
    wqa[:, :, 0:32] = wq[:, :, 64:96]
    wqa[:, :, 64:128] = wq[:, :, 0:64]
    wqs = np.concatenate([wq[:, :, 80:96], wq[:, :, 64:80]], axis=2)
    m["w_uqa"] = np.ascontiguousarray(wqa.reshape(2, 128, 16, 128).transpose(1, 0, 2, 3))
    m["w_uqs"] = np.ascontiguousarray(wqs.reshape(2, 128, 16, 32).transpose(1, 0, 2, 3))
    wkv = f("mla_w_ukv")[0].reshape(128, 16, 128)
    m["w_uk"] = np.ascontiguousarray(wkv[:, :, 0:64]); m["w_uv"] = np.ascontiguousarray(wkv[:, :, 64:128])
    m["c2tok"], m["s2tok"], m["c2T"], m["s2T"] = _rope_tables(NT)
    cw = f("ssm_conv_w")[0]
    m["conv_w"] = np.ascontiguousarray(cw.reshape(4, 10, 128).transpose(2, 1, 0))
    m["conv_b"] = np.ascontiguousarray(f("ssm_conv_b")[0].reshape(10, 128).T)
    m["dt_bias"] = _bc(f("ssm_dt_bias")[0]); m["a_log"] = _bc(f("ssm_a_log")[0]); m["d_skip"] = _bc(f("ssm_d_skip")[0])
    m["w_ba"] = _kc(f("w_branch_attn")[0]); m["w_bs"] = _kc(f("w_branch_ssm")[0]); m["w_out"] = _kc(f("w_out")[0])
    m["w_r"] = _kc(np.concatenate([f("moe_w_group")[0], f("moe_w_expert")[0]], axis=1))
    m["b_r"] = _bc(np.concatenate([f("moe_b_group")[0], f("moe_b_expert")[0]]))
    wg, wu, wd = f("moe_w_gate")[0], f("moe_w_up")[0], f("moe_w_down")[0]
    gu = np.concatenate([wg, wu], axis=2).reshape(NEXP, KC, 128, 512).transpose(0, 2, 1, 3)
    m["w_gu"] = np.ascontiguousarray(gu).reshape(NEXP * 128, KC * 512)
    dn = wd.reshape(NEXP, 2, 128, D).transpose(0, 2, 1, 3)
    m["w_dn"] = np.ascontiguousarray(dn).reshape(NEXP * 128, 2 * D)
    return m


_CACHE = {}


def run(inputs, n_cores=None):
    x = np.asarray(inputs["x"], np.float32)
    B, S, _ = x.shape
    NT = S // 128 + 1
    if NT not in _CACHE:
        _CACHE[NT] = build(NT)
    nc = _CACHE[NT]
    shared = prep_shared(inputs, NT)
    in_maps = []
    for b in range(B):
        m = dict(shared)
        m["x"] = np.ascontiguousarray(x[b])
        in_maps.append(m)
    res = run_bass_kernel_spmd(nc, in_maps, core_ids=list(range(B)))
    return np.stack([np.asarray(r["y"], np.float32).reshape(S, D) for r in res.results], axis=0)


def kernel(**inputs):
    return run(inputs)
```

```python
import os
import numpy as np
from contextlib import ExitStack
import concourse.bass as bass
import concourse.mybir as mybir
from concourse.bass_utils import run_bass_kernel_spmd

F32 = mybir.dt.float32
BF16 = mybir.dt.bfloat16
I32 = mybir.dt.int32
ALU = mybir.AluOpType
AF = mybir.ActivationFunctionType
AX = mybir.AxisListType

N_DMA_SEMS = 3
D = 1024
KC = 8
NEXP = 32
EPS = 1e-6
A_W = 464
OFF_Z, OFF_X, OFF_GA, OFF_GS = 464, 1488, 2768, 3792
P1_COLS = 2768
W_IN_COLS = 4816


class Prog:
    def __init__(self, nc, es):
        self.nc = nc
        self.es = es
        self.eng = {"pe": nc.tensor, "act": nc.scalar, "dve": nc.vector,
                    "pool": nc.gpsimd, "sp": nc.sync}
        self.ops = []
        self.sem = {k: es.enter_context(nc.semaphore("s_" + k)) for k in self.eng}
        self.dsem = {}
        for q in ("sp", "pool", "act"):
            self.dsem[q] = [es.enter_context(nc.semaphore(f"d_{q}{i}")) for i in range(N_DMA_SEMS)]
        self.cnt = {k: 0 for k in self.eng}
        self.known = {}
        self.dcount = {}
        self.drr = {q: 0 for q in self.dsem}
        self.n = 0
        self.total_ops = 0

    def sb(self, es, shape, dtype, name=None):
        self.n += 1
        return es.enter_context(self.nc.sbuf_tensor(name or f"sb{self.n}", list(shape), dtype))

    def ps(self, es, shape, dtype, name=None):
        self.n += 1
        return es.enter_context(self.nc.psum_tensor(name or f"ps{self.n}", list(shape), dtype))

    def op(self, eng, fn, r=(), w=(), dma=False):
        self.nrec = getattr(self, "nrec", 0) + 1
        if self.nrec > int(os.environ.get("KOPS", "100000000")):
            return
        self.ops.append((eng, fn, tuple(r), tuple(w), dma))

    def dma(self, q, fn, r=(), w=()):
        self.op(q, fn, r, w, dma=True)

    def _wait(self, eng, s, v):
        kk = (eng, id(s))
        if self.known.get(kk, 0) >= v:
            return
        self.known[kk] = v
        self.eng[eng].wait_ge(s, v)

    def flush(self):
        ops = self.ops
        self.ops = []
        n = len(ops)
        self.total_ops += n
        last_w, readers = {}, {}
        deps = [None] * n
        for i, (eng, fn, r, w, dma) in enumerate(ops):
            d = set()
            for k in r:
                if k in last_w:
                    d.add(last_w[k])
            for k in w:
                if k in last_w:
                    d.add(last_w[k])
                d.update(readers.get(k, ()))
            d.discard(i)
            deps[i] = d
            for k in r:
                readers.setdefault(k, []).append(i)
            for k in w:
                last_w[k] = i
                readers[k] = []
        signals = [False] * n
        last_of = {}
        for i in range(n):
            eng_i, dma_i = ops[i][0], ops[i][4]
            if not dma_i:
                last_of[eng_i] = i
            keep = {}
            for j in deps[i]:
                eng_j, dma_j = ops[j][0], ops[j][4]
                if dma_j:
                    keep[("dma", j)] = j
                    continue
                if eng_j == "pe" and eng_i == "pe" and not dma_i:
                    continue
                key = ("eng", eng_j)
                if key not in keep or keep[key] < j:
                    keep[key] = j
            deps[i] = sorted(keep.values())
            for j in deps[i]:
                signals[j] = True
        for e, i in last_of.items():
            signals[i] = True
        tok = [None] * n
        for i, (eng, fn, r, w, dma) in enumerate(ops):
            waits = [tok[j] for j in deps[i]]
            s = None
            if dma:
                pool = self.dsem[eng]
                s = pool[self.drr[eng] % len(pool)]
                self.drr[eng] += 1
                c = self.dcount.get(id(s), 0)
                if c > 0:
                    waits.append((s, c))
            for (ws, wv) in waits:
                self._wait(eng, ws, wv)
            res = fn()
            if dma:
                lst = res if isinstance(res, (list, tuple)) else [res]
                c = self.dcount.get(id(s), 0)
                for ins in lst:
                    ins.then_inc(s, 16)
                    c += 16
                self.dcount[id(s)] = c
                tok[i] = (s, c)
            elif signals[i]:
                self.cnt[eng] += 1
                res.then_inc(self.sem[eng], 1)
                tok[i] = (self.sem[eng], self.cnt[eng])
        for wname in self.eng:
            for e2 in self.eng:
                if e2 != wname and self.cnt[e2] > 0:
                    self._wait(wname, self.sem[e2], self.cnt[e2])
            for q, pool in self.dsem.items():
                for s in pool:
                    c = self.dcount.get(id(s), 0)
                    if c > 0:
                        self._wait(wname, s, c)


def build(NT):
    STOP = int(os.environ.get('KSTOP', '99'))
    NR = NT - 1
    TOK = NT * 128
    NSLOT = 2 * NR + NEXP
    nc = bass.Bass("TRN2", target_bir_lowering=False)

    def din(name, shape, dt=F32):
        return nc.dram_tensor(name, list(shape), dt, kind="ExternalInput").ap()

    x = din("x", [NR * 128, D])
    meta = din("meta", [16, D])
    w_in = din("w_in", [128, KC, W_IN_COLS])
    g_mix = din("g_mix", [128, D])
    g_q = din("g_q", [128, 256])
    g_kv = din("g_kv", [128, 128])
    g_ssm = din("g_ssm", [128, D])
    g_ffn = din("g_ffn", [128, D])
    g_fin = din("g_fin", [128, D])
    w_uqa = din("w_uqa", [128, 2, 16, 128])
    w_uqs = din("w_uqs", [128, 2, 16, 32])
    w_uk = din("w_uk", [128, 16, 64])
    w_uv = din("w_uv", [128, 16, 64])
    c2tok = din("c2tok", [128, NT, 32])
    s2tok = din("s2tok", [128, NT, 32])
    c2T = din("c2T", [32, TOK])
    s2T = din("s2T", [32, TOK])
    conv_w = din("conv_w", [128, 10, 4])
    conv_b = din("conv_b", [128, 10])
    dt_bias = din("dt_bias", [128, 16])
    a_log = din("a_log", [128, 16])
    d_skip = din("d_skip", [128, 16])
    w_ba = din("w_ba", [128, KC, D])
    w_bs = din("w_bs", [128, KC, D])
    w_out = din("w_out", [128, KC, D])
    w_r = din("w_r", [128, KC, 36])
    b_r = din("b_r", [128, 36])
    w_gu = din("w_gu", [NEXP * 128, KC * 512])
    w_dn = din("w_dn", [NEXP * 128, 2 * D])
    y_out = nc.dram_tensor("y", [NR * 128, D], F32, kind="ExternalOutput").ap()

    def scratch(name, shape, dt):
        return nc.dram_tensor(name, list(shape), dt, kind="Internal").ap()

    sc_osT = scratch("sc_osT", [TOK, D], BF16)
    sc_cq = scratch("sc_cq", [128, 2, TOK], BF16)
    sc_ckv = scratch("sc_ckv", [128, TOK], BF16)
    sc_kr = scratch("sc_kr", [32, TOK], BF16)
    sc_h1 = scratch("sc_h1", [TOK, D], F32)
    sc_vh = scratch("sc_vh", [TOK, D], BF16)
    sc_xs = scratch("sc_xs", [NSLOT * 128, D], BF16)
    sc_ys = scratch("sc_ys", [NSLOT * 128, D], F32)
    WGU_C, WC = KC * 512, KC * 512 + 2 * D
    sc_wc = scratch("sc_wc", [NEXP * 128, WC], BF16)

    with ExitStack() as es:
        P = Prog(nc, es)
        sb = lambda shape, dt, e=es: P.sb(e, shape, dt)
        bank = [P.ps(es, [128, 512], F32, name=f"bank{i}") for i in range(8)]

        def bfv(i):
            return bank[i][:].bitcast(BF16)

        ident = sb([128, 128], BF16)
        identf = sb([128, 128], F32)
        uincl = sb([128, 128], F32)
        mask01 = sb([128, 128], F32)
        negm = sb([128, 8, 128], BF16)
        ones_f = sb([128, 128], F32)
        ones_b = sb([128, 128], BF16)
        epst = sb([128, 1], F32)
        onet = sb([128, 1], F32)
        mask0 = sb([128, 1], F32)
        A_bc = sb([128, 16], F32)
        dtb_bc = sb([128, 16], F32)
        dsk_bc = sb([128, 16], F32)
        oh1 = sb([128, NR, 32], F32)
        oh2 = sb([128, NR, 32], F32)
        rank = sb([128, NR, 32], F32)
        wts = sb([128, NR, 2], F32)
        Macc = sb([128, 32], F32)

        P.op("pool", lambda: nc.gpsimd.memset(identf[:], 1.0), w=["identf"])
        P.op("pool", lambda: nc.gpsimd.affine_select(out=identf[:], in_=identf[:], pattern=[[-1, 128]],
                                                      compare_op=ALU.is_equal, fill=0.0, base=0, channel_multiplier=1),
             r=["identf"], w=["identf"])
        P.op("dve", lambda: nc.vector.tensor_copy(out=ident[:], in_=identf[:]), r=["identf"], w=["ident"])
        P.op("pool", lambda: nc.gpsimd.memset(ones_f[:], 1.0), w=["ones_f"])
        P.op("pool", lambda: nc.gpsimd.memset(ones_b[:], 1.0), w=["ones_b"])
        P.op("pool", lambda: nc.gpsimd.affine_select(out=uincl[:], in_=ones_f[:], pattern=[[1, 128]],
                                                      compare_op=ALU.is_ge, fill=0.0, base=0, channel_multiplier=-1),
             r=["ones_f"], w=["uincl"])
        P.op("dve", lambda: nc.vector.tensor_copy(out=mask01[:], in_=uincl[:]), r=["uincl"], w=["mask01"])
        P.op("dve", lambda: nc.vector.tensor_scalar(out=negm[:], in0=uincl[:].unsqueeze(1).to_broadcast([128, 8, 128]),
                                                    scalar1=-1.0, scalar2=30000.0, op0=ALU.add, op1=ALU.mult),
             r=["uincl"], w=["negm"])
        P.op("dve", lambda: nc.vector.memset(epst[:], EPS), w=["eps"])
        P.op("dve", lambda: nc.vector.memset(onet[:], 1.0), w=["onet"])
        P.op("dve", lambda: nc.vector.memset(mask0[:], 1.0), w=["mask0"])
        P.op("dve", lambda: nc.vector.memset(mask0[0:112, :], 0.0), r=["mask0"], w=["mask0"])
        P.dma("sp", lambda: nc.sync.dma_start(out=A_bc[:], in_=a_log), w=["A_bc"])
        P.dma("sp", lambda: nc.sync.dma_start(out=dtb_bc[:], in_=dt_bias), w=["dtb"])
        P.dma("sp", lambda: nc.sync.dma_start(out=dsk_bc[:], in_=d_skip), w=["dsk"])
        P.op("act", lambda: nc.scalar.activation(out=A_bc[:], in_=A_bc[:], func=AF.Exp), r=["A_bc"], w=["A_bc"])
        P.op("dve", lambda: nc.vector.tensor_scalar(out=A_bc[:], in0=A_bc[:], scalar1=-1.0, scalar2=None, op0=ALU.mult),
             r=["A_bc"], w=["A_bc"])
        P.flush()

        def rstd_ops(ss, out, n, key_in, key_out, extra_scale=None):
            P.op("act", lambda: nc.scalar.activation(out=out, in_=ss, func=AF.Ln, scale=1.0 / n, bias=epst[:, 0:1]),
                 r=[key_in, "eps"], w=[key_out])
            P.op("act", lambda: nc.scalar.activation(out=out, in_=out, func=AF.Exp, scale=-0.5), r=[key_out], w=[key_out])
            if extra_scale is not None:
                P.op("dve", lambda: nc.vector.tensor_scalar(out=out, in0=out, scalar1=float(extra_scale), scalar2=None,
                                                            op0=ALU.mult), r=[key_out], w=[key_out])

        ldc = [0]

        def load_w(dst, src, ncols, src_off, stg, key, nk=KC):
            for c in range(nk):
                P.dma("pool", lambda c=c: nc.gpsimd.dma_start(out=dst[:, c, 0:ncols], in_=src[:, c, src_off:src_off + ncols]), w=[key])

        with ExitStack() as e1:
            s1 = lambda shape, dt: P.sb(e1, shape, dt)
            w_inb = s1([128, KC, P1_COLS], BF16)
            stg = [s1([128, 512], F32) for _ in range(2)]
            load_w(w_inb, w_in, P1_COLS, 0, stg, "w_inb")
            gmix = s1([128, D], F32)
            gq = s1([128, 256], F32)
            gkv = s1([128, 128], F32)
            gss = s1([128, D], F32)
            c2t = s1([128, NT, 32], F32)
            s2t = s1([128, NT, 32], F32)
            cw = s1([128, 10, 4], F32)
            cb = s1([128, 10], F32)
            for dst, src, k in ((gmix, g_mix, "gmix"), (gq, g_q, "gq"), (gkv, g_kv, "gkv"), (gss, g_ssm, "gss"),
                                (c2t, c2tok, "c2t"), (s2t, s2tok, "s2t"), (cw, conv_w, "cw"), (cb, conv_b, "cb")):
                P.dma("sp", lambda dst=dst, src=src: nc.sync.dma_start(out=dst[:], in_=src), w=[k])

            def mb(n, shape, dt):
                return [s1(shape, dt) for _ in range(n)]
            xt = mb(2, [128, D], F32)
            xt0 = s1([128, D], F32)
            junk = s1([128, D], BF16)
            u = s1([128, D], BF16)
            uT = mb(2, [128, KC, 128], BF16)
            st8 = s1([128, 8], F32)
            tA = s1([128, 512], BF16)
            tAT = s1([128, 512], BF16)
            kr1 = s1([128, 32], F32)
            kr2 = s1([128, 32], F32)
            dtt = mb(2, [128, 16], F32)
            at = mb(2, [128, 16], F32)
            zs = mb(3, [128, D], BF16)
            xh = s1([128, 10, 132], F32)
            cacc = s1([128, 10, 128], F32)
            xbcs = mb(3, [128, 10, 128], BF16)
            xtok = mb(2, [128, D], BF16)
            btok = mb(2, [128, 128], BF16)
            xdt = mb(2, [128, D], BF16)
            xdtd = mb(2, [128, D], BF16)
            acs = s1([128, 16], F32)
            eacs = mb(2, [128, 16], F32)
            Rg = s1([128, 8, 128], F32)
            Dg = s1([128, 8, 128], F32)
            LT = s1([128, 8, 128], F32)
            dec = s1([128, 16], F32)
            cbtm = s1([128, 2, 128], F32)
            MT = mb(2, [128, 16, 128], BF16)
            cdsel = mb(2, [128, 8], F32)
            Sst = s1([128, 512], F32)
            prevb = s1([128, 512], BF16)
            yd = s1([128, D], F32)
            yy = s1([128, D], F32)
            ssg = s1([128, 4], F32)
            osm = s1([128, D], BF16)
            osmT = s1([128, KC, 128], BF16)

            P.op("dve", lambda: nc.vector.memset(xt0[:], 0.0), w=["xt0"])
            P.dma("sp", lambda: nc.sync.dma_start(out=xt0[112:128, :], in_=meta), r=["xt0"], w=["xt0"])
            P.op("dve", lambda: nc.vector.memset(xh[:], 0.0), w=["xh", ("xh", 0), ("xh", 1), ("xh", 2)])
            P.op("dve", lambda: nc.vector.memset(Sst[:], 0.0), w=["Sst"])
            P.op("pool", lambda: nc.gpsimd.memset(tA[:], 0.0), w=["tA"])

            def stageA(t):
                i2, i3 = t % 2, t % 3
                xcur = xt0 if t == 0 else xt[i2]
                kx = "xt0" if t == 0 else ("xt", i2)
                uTc, kuT = uT[i2], ("uT", i2)
                dttc, kdt = dtt[i2], ("dtt", i2)
                atc, kat = at[i2], ("at", i2)
                zsc, kzs = zs[i3], ("zs", i3)
                xb, kxb = xbcs[i3], ("xbcs", i3)
                if t > 0:
                    P.dma("sp", lambda: nc.sync.dma_start(out=xcur[:], in_=x[(t - 1) * 128:t * 128, :]), w=[kx])
                P.op("act", lambda: nc.scalar.activation(out=junk[:], in_=xcur[:], func=AF.Square, accum_out=st8[:, 0:1]),
                     r=[kx], w=["junk", "ssx"])
                rstd_ops(st8[:, 0:1], st8[:, 1:2], D, "ssx", "rsx")
                P.op("dve", lambda: nc.vector.scalar_tensor_tensor(out=u[:], in0=xcur[:], scalar=st8[:, 1:2], in1=gmix[:],
                                                                   op0=ALU.mult, op1=ALU.mult), r=[kx, "rsx", "gmix"], w=["u"])

                def tr_u():
                    for c in range(KC):
                        ins = nc.tensor.transpose(out=bfv(0)[:, c * 128:(c + 1) * 128], in_=u[:, c * 128:(c + 1) * 128], identity=ident[:])
                    return ins
                P.op("pe", tr_u, r=["u", "ident"], w=["b0"])
                P.op("act", lambda: nc.scalar.copy(out=uTc[:].rearrange("p c t -> p (c t)"), in_=bfv(0)), r=["b0"], w=[kuT])
                yield

                def mmA():
                    for c in range(KC):
                        ins = nc.tensor.matmul(bank[1][:, 0:A_W], lhsT=uTc[:, c, :], rhs=w_inb[:, c, 0:A_W], start=(c == 0), stop=(c == KC - 1))
                    return ins
                P.op("pe", mmA, r=[kuT, "w_inb"], w=["b1"])
                P.op("act", lambda: nc.scalar.activation(out=junk[:, 0:256], in_=bank[1][:, 0:256], func=AF.Square, accum_out=st8[:, 2:3]),
                     r=["b1"], w=["junk", "ssq"])
                P.op("act", lambda: nc.scalar.activation(out=junk[:, 256:384], in_=bank[1][:, 256:384], func=AF.Square, accum_out=st8[:, 4:5]),
                     r=["b1"], w=["junk", "sskv"])
                rstd_ops(st8[:, 2:3], st8[:, 3:4], 256, "ssq", "rsq", extra_scale=96 ** -0.5)
                rstd_ops(st8[:, 4:5], st8[:, 5:6], 128, "sskv", "rskv")
                P.op("dve", lambda: nc.vector.scalar_tensor_tensor(out=tA[:, 0:256], in0=bank[1][:, 0:256], scalar=st8[:, 3:4], in1=gq[:],
                                                                   op0=ALU.mult, op1=ALU.mult), r=["b1", "rsq", "gq"], w=["tA"])
                P.op("dve", lambda: nc.vector.scalar_tensor_tensor(out=tA[:, 256:384], in0=bank[1][:, 256:384], scalar=st8[:, 5:6], in1=gkv[:],
                                                                   op0=ALU.mult, op1=ALU.mult), r=["b1", "rskv", "gkv"], w=["tA"])
                P.op("dve", lambda: nc.vector.tensor_tensor(out=kr1[:], in0=bank[1][:, 384:416], in1=c2t[:, t, :], op=ALU.mult),
                     r=["b1", "c2t"], w=["kr1"])
                P.op("dve", lambda: nc.vector.tensor_tensor(out=kr2[:], in0=bank[1][:, 416:448], in1=s2t[:, t, :], op=ALU.mult),
                     r=["b1", "s2t"], w=["kr2"])
                P.op("dve", lambda: nc.vector.tensor_tensor(out=tA[:, 384:416], in0=kr1[:], in1=kr2[:], op=ALU.add),
                     r=["kr1", "kr2"], w=["tA"])
                P.op("dve", lambda: nc.vector.tensor_tensor(out=dttc[:], in0=bank[1][:, 448:464], in1=dtb_bc[:], op=ALU.add),
                     r=["b1", "dtb"], w=[kdt])
                P.op("act", lambda: nc.scalar.activation(out=dttc[:], in_=dttc[:], func=AF.Exp), r=[kdt], w=[kdt])
                P.op("act", lambda: nc.scalar.activation(out=dttc[:], in_=dttc[:], func=AF.Ln, bias=onet[:, 0:1]), r=[kdt, "onet"], w=[kdt])
                if t == 0:
                    P.op("dve", lambda: nc.vector.tensor_scalar(out=dttc[:], in0=dttc[:], scalar1=mask0[:, 0:1], scalar2=None, op0=ALU.mult),
                         r=[kdt, "mask0"], w=[kdt])
                P.op("dve", lambda: nc.vector.tensor_tensor(out=atc[:], in0=dttc[:], in1=A_bc[:], op=ALU.mult), r=[kdt, "A_bc"], w=[kat])
                yield

                def mm_z():
                    for hf in range(2):
                        for c in range(KC):
                            ins = nc.tensor.matmul(bank[2 + hf][:], lhsT=uTc[:, c, :], rhs=w_inb[:, c, OFF_Z + hf * 512: OFF_Z + (hf + 1) * 512],
                                                   start=(c == 0), stop=(c == KC - 1))
                    return ins
                P.op("pe", mm_z, r=[kuT, "w_inb"], w=["b2", "b3"])
                for hf in range(2):
                    P.op("act", lambda hf=hf: nc.scalar.activation(out=zsc[:, hf * 512:(hf + 1) * 512], in_=bank[2 + hf][:], func=AF.Silu),
                         r=["b%d" % (2 + hf)], w=[kzs])
                yield

                def tr_A():
                    for c in range(3):
                        nc.tensor.transpose(out=bfv(0)[:, c * 128:(c + 1) * 128], in_=tA[:, c * 128:(c + 1) * 128], identity=ident[:])
                    return nc.tensor.transpose(out=bfv(0)[0:32, 384:512], in_=tA[:, 384:416], identity=ident[:])
                P.op("pe", tr_A, r=["tA", "ident"], w=["b0"])
                P.op("act", lambda: nc.scalar.copy(out=tAT[:, 0:384], in_=bfv(0)[:, 0:384]), r=["b0"], w=["tATa"])
                P.op("act", lambda: nc.scalar.copy(out=tAT[0:32, 384:512], in_=bfv(0)[0:32, 384:512]), r=["b0"], w=["tATb"])
                P.dma("sp", lambda: nc.sync.dma_start(out=sc_cq[:, :, t * 128:(t + 1) * 128],
                                                      in_=tAT[:, 0:256].rearrange("p (c t) -> p c t", c=2)), r=["tATa"], w=[("sc_cq", t)])
                P.dma("sp", lambda: nc.sync.dma_start(out=sc_ckv[:, t * 128:(t + 1) * 128], in_=tAT[:, 256:384]), r=["tATa"], w=[("sc_ckv", t)])
                P.dma("sp", lambda: nc.sync.dma_start(out=sc_kr[:, t * 128:(t + 1) * 128], in_=tAT[0:32, 384:512]), r=["tATb"], w=[("sc_kr", t)])
                yield
                xhk = [("xh", 0), ("xh", 1), ("xh", 2)]
                for grp in range(3):
                    chunks = list(range(grp * 4, min(10, grp * 4 + 4)))

                    def mmX(chunks=chunks):
                        for i, j in enumerate(chunks):
                            for c in range(KC):
                                ins = nc.tensor.matmul(bank[4][:, i * 128:(i + 1) * 128], lhsT=w_inb[:, c, OFF_X + j * 128: OFF_X + (j + 1) * 128],
                                                       rhs=uTc[:, c, :], start=(c == 0), stop=(c == KC - 1))
                        return ins
                    P.op("pe", mmX, r=[kuT, "w_inb"], w=["b4"])
                    nchk = len(chunks)
                    P.op("act", lambda grp=grp, nchk=nchk: nc.scalar.copy(
                        out=xh[:, grp * 4: grp * 4 + nchk, 3:131],
                        in_=bank[4][:, 0:nchk * 128].rearrange("p (c t) -> p c t", c=nchk)), r=["b4", "xh"], w=[("xh", grp)])
                    for j in chunks:
                        P.op("dve", lambda j=j: nc.vector.tensor_scalar(out=cacc[:, j, :], in0=xh[:, j, 0:128], scalar1=cw[:, j, 0:1], scalar2=cb[:, j:j + 1],
                                                                       op0=ALU.mult, op1=ALU.add), r=["xh", ("xh", grp), "cw", "cb"], w=[("cacc", j)])
                        for k in range(1, 4):
                            P.op("dve", lambda j=j, k=k: nc.vector.scalar_tensor_tensor(out=cacc[:, j, :], in0=xh[:, j, k:k + 128], scalar=cw[:, j, k:k + 1],
                                                                                       in1=cacc[:, j, :], op0=ALU.mult, op1=ALU.add),
                                 r=["xh", ("xh", grp), "cw", ("cacc", j)], w=[("cacc", j)])
                    yield
                ck = [("cacc", j) for j in range(10)]
                P.op("act", lambda: nc.scalar.activation(out=xb[:].rearrange("p c t -> p (c t)"), in_=cacc[:].rearrange("p c t -> p (c t)"), func=AF.Silu),
                     r=ck, w=[kxb])
                P.op("pool", lambda: nc.gpsimd.tensor_copy(out=xh[:, :, 0:3], in_=xh[:, :, 128:131]), r=xhk + ["xh"], w=["xh"] + xhk)
                if t == 0:
                    P.op("dve", lambda: nc.vector.memset(xb[:, :, 0:112], 0.0), r=[kxb], w=[kxb])
                yield

            def stageB(t):
                i2, i3 = t % 2, t % 3
                dttc, kdt = dtt[i2], ("dtt", i2)
                atc, kat = at[i2], ("at", i2)
                xb, kxb = xbcs[i3], ("xbcs", i3)
                xtk, kxt = xtok[i2], ("xtok", i2)
                btk, kbt = btok[i2], ("btok", i2)
                xd, kxd = xdt[i2], ("xdt", i2)
                xdd, kxdd = xdtd[i2], ("xdtd", i2)
                ea, kea = eacs[i2], ("eacs", i2)
                MTc = MT[i2]
                cds = cdsel[i2]

                def tr_x():
                    for c in range(8):
                        ins = nc.tensor.transpose(out=bfv(5)[:, c * 128:(c + 1) * 128], in_=xb[:, c, :], identity=ident[:])
                    return nc.tensor.transpose(out=bfv(6)[:, 0:128], in_=xb[:, 8, :], identity=ident[:])
                P.op("pe", tr_x, r=[kxb, "ident"], w=["b5", "b6"])
                P.op("act", lambda: nc.scalar.copy(out=xtk[:], in_=bfv(5)), r=["b5"], w=[kxt])
                P.op("act", lambda: nc.scalar.copy(out=btk[:], in_=bfv(6)[:, 0:128]), r=["b6"], w=[kbt])
                P.op("pe", lambda: nc.tensor.matmul(bank[7][:, 0:16], lhsT=uincl[:], rhs=atc[:], start=True, stop=True), r=["uincl", kat], w=["b7"])
                P.op("dve", lambda: nc.vector.tensor_copy(out=acs[:], in_=bank[7][:, 0:16]), r=["b7"], w=["acs"])
                P.op("act", lambda: nc.scalar.activation(out=ea[:], in_=bank[7][:, 0:16], func=AF.Exp), r=["b7"], w=[kea])

                P.op("pe", lambda: nc.tensor.matmul(bank[7][:, 128:256], lhsT=xb[0:64, 8, :], rhs=xb[0:64, 9, :], start=True, stop=True),
                     r=[kxb], w=["b7"])
                P.op("pe", lambda: nc.tensor.matmul(bank[4][:, 0:128], lhsT=xb[64:128, 8, :], rhs=xb[64:128, 9, :], start=True, stop=True),
                     r=[kxb], w=["b4"])
                P.op("dve", lambda: nc.vector.tensor_tensor(out=cbtm[:, 0, :], in0=bank[7][:, 128:256], in1=mask01[:], op=ALU.mult),
                     r=["b7", "mask01"], w=["cbtm0"])
                P.op("dve", lambda: nc.vector.tensor_tensor(out=cbtm[:, 1, :], in0=bank[4][:, 0:128], in1=mask01[:], op=ALU.mult),
                     r=["b4", "mask01"], w=["cbtm1"])
                P.op("dve", lambda: nc.vector.tensor_tensor(out=xd[:].rearrange("p (h d) -> p h d", h=16),
                                                            in0=xtk[:].rearrange("p (h d) -> p h d", h=16),
                                                            in1=dttc[:].unsqueeze(2).to_broadcast([128, 16, 64]), op=ALU.mult),
                     r=[kxt, kdt], w=[kxd])
                yield
                for g in range(2):
                    P.op("pool", lambda g=g: nc.gpsimd.tensor_tensor(out=Rg[:], in0=uincl[:].unsqueeze(1).to_broadcast([128, 8, 128]),
                                                                    in1=atc[:, g * 8:(g + 1) * 8].unsqueeze(2).to_broadcast([128, 8, 128]), op=ALU.mult),
                         r=["uincl", kat], w=["Rg"])

                    def mmbc(g=g):
                        for q4 in range(2):
                            nc.tensor.matmul(bank[5 + q4][:], lhsT=ones_f[:], rhs=Rg[:, q4 * 4:(q4 + 1) * 4, :].rearrange("p e l -> p (e l)"),
                                             start=True, stop=False)
                            ins = nc.tensor.matmul(bank[5 + q4][:], lhsT=ident[:], rhs=negm[:, q4 * 4:(q4 + 1) * 4, :].rearrange("p e l -> p (e l)"),
                                                   start=False, stop=True)
                        return ins
                    P.op("pe", mmbc, r=["ones_f", "Rg", "ident", "negm"], w=["b5", "b6"])
                    for q4 in range(2):
                        P.op("dve", lambda g=g, q4=q4: nc.vector.tensor_tensor(
                            out=Dg[:, q4 * 4:(q4 + 1) * 4, :], in0=bank[5 + q4][:].rearrange("p (e l) -> p e l", e=4),
                            in1=acs[:, g * 8 + q4 * 4: g * 8 + q4 * 4 + 4].unsqueeze(2).to_broadcast([128, 4, 128]), op=ALU.subtract),
                            r=["b%d" % (5 + q4), "acs"], w=[("Dg", q4)])
                    P.op("act", lambda: nc.scalar.activation(out=LT[:], in_=Dg[:], func=AF.Exp),
                         r=[("Dg", 0), ("Dg", 1)], w=["LT"])
                    P.op("dve", lambda g=g: nc.vector.tensor_copy(out=dec[:, g * 8:(g + 1) * 8], in_=LT[:, :, 127]), r=["LT"], w=[("dec", g)])
                    P.op("act", lambda g=g: nc.scalar.activation(
                        out=cds[g * 64:(g + 1) * 64, 0:4],
                        in_=bank[5][g * 64:(g + 1) * 64, :].rearrange("p (e l) -> p e l", e=4)[:, :, 127], func=AF.Exp),
                        r=["b5"], w=[("cdselA", i2, g)])
                    P.op("act", lambda g=g: nc.scalar.activation(
                        out=cds[g * 64:(g + 1) * 64, 4:8],
                        in_=bank[6][g * 64:(g + 1) * 64, :].rearrange("p (e l) -> p e l", e=4)[:, :, 127], func=AF.Exp),
                        r=["b6", ("cdselA", i2, g)], w=[("cdsel", i2, g)])
                    P.op("pool", lambda g=g: nc.gpsimd.tensor_tensor(
                        out=MTc[:, g * 8:(g + 1) * 8, :], in0=LT[:],
                        in1=cbtm[:, g, :].unsqueeze(1).to_broadcast([128, 8, 128]), op=ALU.mult), r=["LT", "cbtm0", "cbtm1"], w=[("MT", i2, g)])
                    yield
                P.op("dve", lambda: nc.vector.tensor_tensor(out=xdd[:].rearrange("p (h d) -> p h d", h=16),
                                                            in0=xd[:].rearrange("p (h d) -> p h d", h=16),
                                                            in1=dec[:].unsqueeze(2).to_broadcast([128, 16, 64]), op=ALU.mult),
                     r=[kxd, ("dec", 0), ("dec", 1)], w=[kxdd])
                yield

            def stageC(t):
                i2, i3 = t % 2, t % 3
                zsc, kzs = zs[i3], ("zs", i3)
                xb, kxb = xbcs[i3], ("xbcs", i3)
                xtk, kxt = xtok[i2], ("xtok", i2)
                btk, kbt = btok[i2], ("btok", i2)
                xd, kxd = xdt[i2], ("xdt", i2)
                xdd, kxdd = xdtd[i2], ("xdtd", i2)
                ea, kea = eacs[i2], ("eacs", i2)
                MTc = MT[i2]
                cds = cdsel[i2]
                P.op("pool", lambda: nc.gpsimd.tensor_copy(out=prevb[:], in_=Sst[:]), r=["Sst"], w=["prevb"])
                for g in range(2):
                    P.op("pe", lambda g=g: nc.tensor.matmul(bank[4][g * 64:(g + 1) * 64, :], lhsT=btk[:, g * 64:(g + 1) * 64], rhs=xdd[:, g * 512:(g + 1) * 512],
                                                            start=True, stop=True), r=[kbt, kxdd], w=["b4", "b4s%d" % g])
                P.op("dve", lambda: nc.vector.tensor_tensor(out=Sst[:].rearrange("p (e d) -> p e d", e=8), in0=Sst[:].rearrange("p (e d) -> p e d", e=8),
                                                            in1=cds[:].unsqueeze(2).to_broadcast([128, 8, 64]), op=ALU.mult),
                     r=["Sst", ("cdsel", i2, 0), ("cdsel", i2, 1), "prevb"], w=["Sst"])
                P.op("dve", lambda: nc.vector.tensor_tensor(out=Sst[:], in0=Sst[:], in1=bank[4][:], op=ALU.add), r=["Sst", "b4", "b4s0", "b4s1"], w=["Sst"])
                yield
                if t > 0:
                    def mmY():
                        for h in range(16):
                            ins = nc.tensor.matmul(bank[2 + h // 8][:, (h % 8) * 64:(h % 8 + 1) * 64], lhsT=MTc[:, h, :], rhs=xd[:, h * 64:(h + 1) * 64],
                                                   start=True, stop=True)
                        return ins
                    P.op("pe", mmY, r=[("MT", i2, 0), ("MT", i2, 1), kxd], w=["b2", "b3"])

                    for g in range(2):
                        P.op("pe", lambda g=g: nc.tensor.matmul(bank[5 + g][:], lhsT=xb[g * 64:(g + 1) * 64, 9, :], rhs=prevb[g * 64:(g + 1) * 64, :],
                                                                start=True, stop=True), r=[kxb, "prevb"], w=["b%d" % (5 + g)])
                    for g in range(2):
                        P.op("act", lambda g=g: nc.scalar.copy(out=yd[:, g * 512:(g + 1) * 512], in_=bank[2 + g][:]), r=["b%d" % (2 + g)], w=[("yd", g)])
                        P.op("dve", lambda g=g: nc.vector.tensor_tensor(
                            out=yy[:, g * 512:(g + 1) * 512].rearrange("p (e d) -> p e d", e=8),
                            in0=bank[5 + g][:].rearrange("p (e d) -> p e d", e=8),
                            in1=ea[:, g * 8:(g + 1) * 8].unsqueeze(2).to_broadcast([128, 8, 64]), op=ALU.mult),
                            r=["b%d" % (5 + g), kea], w=[("yy", g)])
                    yield
                    for g in range(2):
                        P.op("pool", lambda g=g: nc.gpsimd.tensor_tensor(out=yy[:, g * 512:(g + 1) * 512], in0=yy[:, g * 512:(g + 1) * 512],
                                                                        in1=yd[:, g * 512:(g + 1) * 512], op=ALU.add),
                             r=[("yy", g), ("yd", g)], w=[("yy", g)])
                        P.op("dve", lambda g=g: nc.vector.tensor_tensor(
                            out=yd[:, g * 512:(g + 1) * 512].rearrange("p (e d) -> p e d", e=8),
                            in0=xtk[:, g * 512:(g + 1) * 512].rearrange("p (e d) -> p e d", e=8),
                            in1=dsk_bc[:, g * 8:(g + 1) * 8].unsqueeze(2).to_broadcast([128, 8, 64]), op=ALU.mult),
                            r=[kxt, "dsk", ("yy", g)], w=[("yd", g)])
                        P.op("pool", lambda g=g: nc.gpsimd.tensor_tensor(out=yy[:, g * 512:(g + 1) * 512], in0=yy[:, g * 512:(g + 1) * 512],
                                                                        in1=yd[:, g * 512:(g + 1) * 512], op=ALU.add),
                             r=[("yy", g), ("yd", g)], w=[("yy", g)])
                        P.op("dve", lambda g=g: nc.vector.tensor_tensor(out=yy[:, g * 512:(g + 1) * 512], in0=yy[:, g * 512:(g + 1) * 512],
                                                                       in1=zsc[:, g * 512:(g + 1) * 512], op=ALU.mult),
                             r=[("yy", g), kzs], w=[("yy", g)])
                        P.op("act", lambda g=g: nc.scalar.activation(out=junk[:, g * 512:(g + 1) * 512], in_=yy[:, g * 512:(g + 1) * 512],
                                                                     func=AF.Square, accum_out=ssg[:, g:g + 1]), r=[("yy", g)], w=["junk", ("ssg", g)])
                        rstd_ops(ssg[:, g:g + 1], ssg[:, 2 + g:3 + g], 512, ("ssg", g), ("rsg", g))
                        P.op("dve", lambda g=g: nc.vector.scalar_tensor_tensor(out=osm[:, g * 512:(g + 1) * 512], in0=yy[:, g * 512:(g + 1) * 512],
                                                                              scalar=ssg[:, 2 + g:3 + g], in1=gss[:, g * 512:(g + 1) * 512],
                                                                              op0=ALU.mult, op1=ALU.mult), r=[("yy", g), ("rsg", g), "gss"], w=[("osm", g)])
                        yield
                    def tr_o():
                        for c in range(KC):
                            ins = nc.tensor.transpose(out=bfv(0)[:, c * 128:(c + 1) * 128], in_=osm[:, c * 128:(c + 1) * 128], identity=ident[:])
                        return ins
                    P.op("pe", tr_o, r=[("osm", 0), ("osm", 1), "ident"], w=["b0"])
                    P.op("act", lambda: nc.scalar.copy(out=osmT[:].rearrange("p c t -> p (c t)"), in_=bfv(0)), r=["b0"], w=["osmT"])
                    P.dma("sp", lambda: nc.sync.dma_start(out=sc_osT[t * 128:(t + 1) * 128, :], in_=osmT[:].rearrange("p c t -> p (c t)")),
                          r=["osmT"], w=[("sc_osT", t)])
                yield

            for n in range(NT + 2):
                gens = []
                if n - 2 >= 0:
                    gens.append(stageC(n - 2))
                if 0 <= n - 1 < NT:
                    gens.append(stageB(n - 1))
                if n < NT:
                    gens.append(stageA(n))
                while gens:
                    for g_ in list(gens):
                        try:
                            next(g_)
                        except StopIteration:
                            gens.remove(g_)
            P.flush()
            if STOP <= 1:
                return nc

        def load_flat(dst2, src2, ncols, stg, key):
            P.dma("pool", lambda: nc.gpsimd.dma_start(out=dst2[:, 0:ncols], in_=src2[:, 0:ncols]), w=[key])

        groups = [[0]] + [list(range(g0, min(g0 + 4, NT))) for g0 in range(1, NT, 4)]

        with ExitStack() as e23:
            o_attnT = P.sb(e23, [128, KC, TOK], BF16)
            with ExitStack() as e2:
                s2 = lambda shape, dt: P.sb(e2, shape, dt)
                stg = [s2([128, 512], F32) for _ in range(2)]
                wqa = s2([128, 2, 16, 128], BF16)
                wqs = s2([128, 2, 16, 32], BF16)
                wkb = s2([128, 16, 64], BF16)
                wvb = s2([128, 16, 64], BF16)
                load_flat(wqa[:].rearrange("p c h m -> p (c h m)"), w_uqa.rearrange("p c h m -> p (c h m)"), 4096, stg, "wqa")
                load_flat(wqs[:].rearrange("p c h m -> p (c h m)"), w_uqs.rearrange("p c h m -> p (c h m)"), 1024, stg, "wqs")
                load_flat(wkb[:].rearrange("p h m -> p (h m)"), w_uk.rearrange("p h m -> p (h m)"), 1024, stg, "wkb")
                load_flat(wvb[:].rearrange("p h m -> p (h m)"), w_uv.rearrange("p h m -> p (h m)"), 1024, stg, "wvb")
                c2b = s2([32, TOK], BF16)
                s2b = s2([32, TOK], BF16)
                for o in range(0, TOK, 512):
                    wd = min(512, TOK - o)
                    for dst, src, k in ((c2b, c2T, "c2b"), (s2b, s2T, "s2b")):
                        i = ldc[0] % 2
                        ldc[0] += 1
                        st = stg[i]
                        P.dma("sp", lambda o=o, wd=wd, st=st, src=src: nc.sync.dma_start(out=st[0:32, 0:wd], in_=src[:, o:o + wd]), w=[("stg", i)])
                        P.op("dve", lambda o=o, wd=wd, st=st, dst=dst: nc.vector.tensor_copy(out=dst[:, o:o + wd], in_=st[0:32, 0:wd]),
                             r=[("stg", i)], w=[k])
                cqnT = s2([128, 2, TOK], BF16)
                ckvnT = s2([128, TOK], BF16)
                P.dma("sp", lambda: nc.sync.dma_start(out=cqnT[:, 0, :], in_=sc_cq[:, 0, :]), w=["cqnT0"])
                P.dma("sp", lambda: nc.sync.dma_start(out=cqnT[:, 1, :], in_=sc_cq[:, 1, :]), w=["cqnT1"])
                P.dma("sp", lambda: nc.sync.dma_start(out=ckvnT[:], in_=sc_ckv), w=["ckvnT"])
                KT = [s2([128, TOK], BF16) for _ in range(2)]
                QT = [s2([128, TOK], BF16) for _ in range(2)]
                Vaug = [s2([128, NT, 128], BF16) for _ in range(2)]
                PT = [s2([128, 512], BF16) for _ in range(4)]
                qr1 = s2([32, 512], F32)
                qr2 = s2([32, 512], F32)
                rrow = s2([128, 512], F32)
                bcs = s2([128, 512], F32)
                for par in range(2):
                    oc = 64 if par == 0 else 0
                    P.op("pool", lambda par=par: nc.gpsimd.memset(KT[par][:], 0.0), w=[("KT", par)])
                    P.op("pool", lambda par=par: nc.gpsimd.memset(QT[par][:], 0.0), w=[("QT", par)])
                    P.dma("sp", lambda par=par: nc.sync.dma_start(out=KT[par][0:32, :], in_=sc_kr), r=[("KT", par)], w=[("KT", par)])
                    P.op("pool", lambda par=par: nc.gpsimd.memset(Vaug[par][:], 0.0), w=[("Vaug", par)])
                    P.op("pool", lambda par=par, oc=oc: nc.gpsimd.memset(Vaug[par][:, :, oc:oc + 1], 1.0), r=[("Vaug", par)], w=[("Vaug", par)])
                    P.op("pool", lambda par=par, oc=oc: nc.gpsimd.memset(Vaug[par][0:112, 0, oc:oc + 1], 0.0), r=[("Vaug", par)], w=[("Vaug", par)])
                P.op("dve", lambda: nc.vector.memset(rrow[:], 1.0), w=["rrow"])
                NG = len(groups)

                def proj_items(h):
                    par = h % 2
                    voff = 0 if par == 0 else 64
                    items = []
                    for gi, grp in enumerate(groups):
                        c0, c1 = grp[0] * 128, (grp[-1] + 1) * 128
                        n = c1 - c0

                        def itemKQ(h=h, par=par, gi=gi, c0=c0, c1=c1, n=n):
                            P.op("pe", lambda: nc.tensor.matmul(bank[6][64:128, 0:n], lhsT=wkb[:, h, :], rhs=ckvnT[:, c0:c1], start=True, stop=True),
                                 r=["wkb", "ckvnT"], w=["b6"])
                            P.op("dve", lambda: nc.vector.tensor_copy(out=KT[par][64:128, c0:c1], in_=bank[6][64:128, 0:n]),
                                 r=["b6", ("KT", par)], w=[("KTg", par, gi)])

                            def mmQs():
                                for c in range(2):
                                    ins = nc.tensor.matmul(bank[7][0:32, 0:n], lhsT=wqs[:, c, h, :], rhs=cqnT[:, c, c0:c1], start=(c == 0), stop=(c == 1))
                                return ins

                            def mmQa():
                                for c in range(2):
                                    ins = nc.tensor.matmul(bank[6][:, 0:n], lhsT=wqa[:, c, h, :], rhs=cqnT[:, c, c0:c1], start=(c == 0), stop=(c == 1))
                                return ins
                            P.op("pe", mmQs, r=["wqs", "cqnT0", "cqnT1"], w=["b7"])
                            P.op("pe", mmQa, r=["wqa", "cqnT0", "cqnT1"], w=["b6"])
                            P.op("dve", lambda: nc.vector.tensor_copy(out=QT[par][64:128, c0:c1], in_=bank[6][64:128, 0:n]),
                                 r=["b6", ("QT", par)], w=[("QTa", par, gi)])
                            P.op("dve", lambda: nc.vector.tensor_tensor(out=qr1[:, 0:n], in0=bank[6][0:32, 0:n], in1=c2b[:, c0:c1], op=ALU.mult),
                                 r=["b6", "c2b"], w=["qr1"])
                            P.op("dve", lambda: nc.vector.tensor_tensor(out=qr2[:, 0:n], in0=bank[7][0:32, 0:n], in1=s2b[:, c0:c1], op=ALU.mult),
                                 r=["b7", "s2b"], w=["qr2"])
                            P.op("pool", lambda: nc.gpsimd.tensor_tensor(out=QT[par][0:32, c0:c1], in0=qr1[:, 0:n], in1=qr2[:, 0:n], op=ALU.add),
                                 r=["qr1", "qr2", ("QT", par)], w=[("QTb", par, gi)])
                        items.append(itemKQ)
                    for t0 in range(0, NT, 8):
                        tn = min(8, NT - t0)

                        def itemV(h=h, par=par, voff=voff, t0=t0, tn=tn):
                            def mmV():
                                for j in range(tn):
                                    ins = nc.tensor.matmul(bank[6][:, j * 64:(j + 1) * 64], lhsT=ckvnT[:, (t0 + j) * 128:(t0 + j + 1) * 128], rhs=wvb[:, h, :],
                                                           start=True, stop=True)
                                return ins
                            P.op("pe", mmV, r=["wvb", "ckvnT"], w=["b6"])
                            P.op("dve", lambda: nc.vector.tensor_copy(
                                out=Vaug[par][:, t0:t0 + tn, voff:voff + 64], in_=bank[6][:, 0:tn * 64].rearrange("p (t d) -> p t d", t=tn)),
                                r=["b6", ("Vaug", par)], w=[("Vaug", par)])
                        items.append(itemV)
                    return items

                def kq_keys(par):
                    return [("KT", par), ("QT", par)] + [("KTg", par, gi) for gi in range(NG)] + [("QTa", par, gi) for gi in range(NG)] + \
                           [("QTb", par, gi) for gi in range(NG)]

                its = []
                octr = 0
                for h in range(16):
                    for gi, grp in enumerate(groups):
                        ob = 3 + (octr % 2)
                        octr += 1
                        for kt in range(grp[-1] + 1):
                            its.append((h, gi, kt, ob))
                NI = len(its)
                LOOK = 3
                SBK = [0, 1, 2, 5]
                pend = {h: proj_items(h) for h in range(16)}
                for it_ in pend[0]:
                    it_()
                pend[0] = []
                deferred = []

                def emit_S(idx):
                    h, gi, kt, ob = its[idx]
                    par = h % 2
                    if pend[h]:
                        for it_ in pend[h]:
                            it_()
                        pend[h] = []
                    grp = groups[gi]
                    q0, q1 = grp[0] * 128, (grp[-1] + 1) * 128
                    j = max(0, kt - grp[0])
                    qs = q0 + j * 128
                    n = q1 - qs
                    sbk = idx % 4
                    P.op("pe", lambda: nc.tensor.matmul(bank[SBK[sbk]][:, 0:n], lhsT=KT[par][:, kt * 128:(kt + 1) * 128], rhs=QT[par][:, qs:q1],
                                                        start=True, stop=True), r=kq_keys(par), w=[("S", sbk)])

                def emit_rest(idx):
                    h, gi, kt, ob = its[idx]
                    par = h % 2
                    grp = groups[gi]
                    q0, q1 = grp[0] * 128, (grp[-1] + 1) * 128
                    nq = q1 - q0
                    j = max(0, kt - grp[0])
                    n = q1 - (q0 + j * 128)
                    sbk = idx % 4
                    M = 65 if par == 0 else 128
                    last_kt = grp[-1]
                    P.op("act", lambda: nc.scalar.activation(out=PT[sbk][:, 0:n], in_=bank[SBK[sbk]][:, 0:n], func=AF.Exp),
                         r=[("S", sbk)], w=[("PT", sbk)])
                    if kt >= grp[0]:
                        P.op("dve", lambda: nc.vector.tensor_tensor(out=PT[sbk][:, 0:128], in0=PT[sbk][:, 0:128], in1=mask01[:], op=ALU.mult),
                             r=[("PT", sbk), "mask01"], w=[("PT", sbk)])
                    assert all(d[2] != ob for d in deferred), "pending normalisation on the accumulator bank"
                    P.op("pe", lambda: nc.tensor.matmul(bank[ob][0:M, j * 128:nq], lhsT=Vaug[par][:, kt, 0:M], rhs=PT[sbk][:, 0:n],
                                                        start=(kt == 0), stop=(kt == last_kt)),
                         r=[("PT", sbk), ("Vaug", par)], w=["b%d" % ob])
                    if kt == last_kt:
                        dr = 64 if par == 0 else 0
                        r0, r1 = (0, 64) if par == 0 else (64, 128)
                        P.op("dve", lambda: nc.vector.tensor_scalar(out=rrow[dr:dr + 1, 0:nq], in0=bank[ob][dr:dr + 1, 0:nq],
                                                                    scalar1=1e-30, scalar2=None, op0=ALU.max), r=["b%d" % ob], w=["rrow"])
                        P.op("dve", lambda: nc.vector.reciprocal(out=rrow[dr:dr + 1, 0:nq], in_=rrow[dr:dr + 1, 0:nq]), r=["rrow"], w=["rrow"])

                        def fin():
                            P.op("pe", lambda: nc.tensor.matmul(bank[7][0:r1, 0:nq], lhsT=ones_f[dr:dr + 1, 0:r1], rhs=rrow[dr:dr + 1, 0:nq],
                                                                start=True, stop=True), r=["rrow", "ones_f"], w=["b7"])
                            P.op("dve", lambda: nc.vector.tensor_copy(out=bcs[r0:r1, 0:nq], in_=bank[7][r0:r1, 0:nq]), r=["b7"], w=["bcs"])
                            P.op("dve", lambda: nc.vector.tensor_tensor(out=o_attnT[r0:r1, h // 2, q0:q1], in0=bank[ob][r0:r1, 0:nq], in1=bcs[r0:r1, 0:nq],
                                                                        op=ALU.mult), r=["b%d" % ob, "bcs"], w=[("oT", h, gi)])
                        deferred.append((idx + 1, fin, ob))

                cast_items = []
                CR = 256
                for r0_ in range(0, NEXP * 128, CR):
                    cast_items.append(lambda r0_=r0_: P.dma("pool", lambda: nc.gpsimd.dma_start(out=sc_wc[r0_:r0_ + CR, 0:WGU_C], in_=w_gu[r0_:r0_ + CR, :]),
                                                            w=[("sc_wgu", r0_)]))
                    cast_items.append(lambda r0_=r0_: P.dma("pool", lambda: nc.gpsimd.dma_start(out=sc_wc[r0_:r0_ + CR, WGU_C:WC], in_=w_dn[r0_:r0_ + CR, :]),
                                                            w=[("sc_wdn", r0_)]))
                cast_every = max(1, (NI - 8) // len(cast_items))
                head_start = {}
                for idx, (h, gi, kt, ob) in enumerate(its):
                    head_start.setdefault(h, idx)
                for idx in range(NI + LOOK):
                    if idx < NI:
                        emit_S(idx)
                    jdx = idx - LOOK
                    if jdx >= 0:
                        emit_rest(jdx)
                        while deferred and deferred[0][0] <= jdx:
                            deferred.pop(0)[1]()
                        h = its[jdx][0]
                        loc = jdx - head_start[h]
                        if h + 1 < 16 and loc >= 4 and (loc - 4) % 16 == 0 and pend[h + 1]:
                            pend[h + 1].pop(0)()
                        if cast_items and jdx % cast_every == 0:
                            cast_items.pop(0)()
                for d in deferred:
                    d[1]()
                for ci in cast_items:
                    ci()
                P.flush()
                if STOP <= 2:
                    return nc

            with ExitStack() as e3:
                s3 = lambda shape, dt: P.sb(e3, shape, dt)
                wg = s3([128, KC, 2048], BF16)
                wba = s3([128, KC, D], BF16)
                wbs = s3([128, KC, D], BF16)
                wo = s3([128, KC, D], BF16)
                wrb = s3([128, KC, 36], BF16)
                with ExitStack() as eL:
                    stg = [P.sb(eL, [128, 512], F32) for _ in range(2)]
                    load_w(wg, w_in, 2048, OFF_GA, stg, "wg")
                    load_w(wba, w_ba, D, 0, stg, "wba")
                    load_w(wbs, w_bs, D, 0, stg, "wbs")
                    load_w(wo, w_out, D, 0, stg, "wo")
                    load_w(wrb, w_r, 36, 0, stg, "wrb")
                    P.flush()
                gmix = s3([128, D], F32)
                gffn = s3([128, D], F32)
                brt = s3([128, 36], F32)
                ustr = s3([128, 128], BF16)
                for dst, src, k in ((gmix, g_mix, "gmix"), (gffn, g_ffn, "gffn"), (brt, b_r, "brt")):
                    P.dma("sp", lambda dst=dst, src=src: nc.sync.dma_start(out=dst[:], in_=src), w=[k])
                P.op("pool", lambda: nc.gpsimd.affine_select(out=ustr[:], in_=ones_b[:], pattern=[[1, 128]], compare_op=ALU.is_gt, fill=0.0,
                                                              base=0, channel_multiplier=-1), r=["ones_b"], w=["ustr"])
                P.op("dve", lambda: nc.vector.memset(Macc[:], 0.0), w=["Macc"])
                xt = [s3([128, D], F32)] * 2
                u = s3([128, D], BF16)
                uT = s3([128, KC, 128], BF16)
                osT = s3([128, KC, 128], BF16)
                st8 = s3([128, 16], F32)
                sga = s3([128, D], F32)
                sgs = s3([128, D], F32)
                mrb = s3([128, D], BF16)
                mT = s3([128, KC, 128], BF16)
                h1 = s3([128, D], F32)
                vh = s3([128, D], BF16)
                vT = s3([128, KC, 128], BF16)
                lg = s3([128, 36], F32)
                sm = s3([128, 16], F32)
                pen = s3([128, 4], F32)
                goh = s3([128, 4], F32)
                elm = s3([128, 32], F32)
                elm2 = s3([128, 32], F32)
                Mt = s3([128, 32], F32)
                Mtb = s3([128, 32], BF16)
                Maccb = s3([128, 32], BF16)

                for t in range(1, NT):
                    r_ = t - 1
                    xcur = xt[t % 2]
                    kx = "xt3"
                    P.dma("sp", lambda t=t, xcur=xcur: nc.sync.dma_start(out=xcur[:], in_=x[(t - 1) * 128:t * 128, :]), w=[kx])
                    P.dma("sp", lambda t=t: nc.sync.dma_start(out=osT[:].rearrange("p c t -> p (c t)"), in_=sc_osT[t * 128:(t + 1) * 128, :]), w=["osT"])
                    P.op("act", lambda xcur=xcur: nc.scalar.activation(out=mrb[:], in_=xcur[:], func=AF.Square, accum_out=st8[:, 0:1]),
                         r=[kx], w=[("mrb", 0), ("mrb", 1), "ssx"])
                    rstd_ops(st8[:, 0:1], st8[:, 1:2], D, "ssx", "rsx")
                    P.op("dve", lambda xcur=xcur: nc.vector.scalar_tensor_tensor(out=u[:], in0=xcur[:], scalar=st8[:, 1:2], in1=gmix[:],
                                                                                op0=ALU.mult, op1=ALU.mult), r=[kx, "rsx", "gmix"], w=["u"])

                    def tr8(src, bnk):
                        def f():
                            for c in range(KC):
                                ins = nc.tensor.transpose(out=bfv(bnk)[:, c * 128:(c + 1) * 128], in_=src[:, c * 128:(c + 1) * 128], identity=ident[:])
                            return ins
                        return f
                    P.op("pe", tr8(u, 0), r=["u", "ident"], w=["b0"])
                    P.op("act", lambda: nc.scalar.copy(out=uT[:].rearrange("p c t -> p (c t)"), in_=bfv(0)), r=["b0"], w=["uT"])

                    def mmG():
                        for q4 in range(4):
                            for c in range(KC):
                                ins = nc.tensor.matmul(bank[1 + q4][:], lhsT=uT[:, c, :], rhs=wg[:, c, q4 * 512:(q4 + 1) * 512], start=(c == 0), stop=(c == KC - 1))
                        return ins
                    P.op("pe", mmG, r=["uT", "wg"], w=["b1", "b2", "b3", "b4"])
                    for q4 in range(4):
                        dst = sga if q4 < 2 else sgs
                        P.op("act", lambda q4=q4, dst=dst: nc.scalar.activation(out=dst[:, (q4 % 2) * 512:(q4 % 2 + 1) * 512], in_=bank[1 + q4][:], func=AF.Sigmoid),
                             r=["b%d" % (1 + q4)], w=[("sg", q4)])

                    def mmBr(t=t):
                        for hf in range(2):
                            for c in range(KC):
                                nc.tensor.matmul(bank[5 + hf][:], lhsT=o_attnT[:, c, t * 128:(t + 1) * 128], rhs=wba[:, c, hf * 512:(hf + 1) * 512],
                                                 start=(c == 0), stop=(c == KC - 1))
                        for hf in range(2):
                            for c in range(KC):
                                ins = nc.tensor.matmul(bank[1 + hf][:], lhsT=osT[:, c, :], rhs=wbs[:, c, hf * 512:(hf + 1) * 512],
                                                       start=(c == 0), stop=(c == KC - 1))
                        return ins
                    P.op("pe", mmBr, r=["wba", "wbs", "osT"], w=["b5", "b6", "b1", "b2"])
                    for hf in range(2):
                        sl = slice(hf * 512, (hf + 1) * 512)
                        P.op("dve", lambda hf=hf, sl=sl: nc.vector.tensor_tensor(out=sga[:, sl], in0=bank[5 + hf][:], in1=sga[:, sl], op=ALU.mult),
                             r=["b%d" % (5 + hf), ("sg", hf)], w=[("sg", hf)])
                        P.op("dve", lambda hf=hf, sl=sl: nc.vector.tensor_tensor(out=sgs[:, sl], in0=bank[1 + hf][:], in1=sgs[:, sl], op=ALU.mult),
                             r=["b%d" % (1 + hf), ("sg", 2 + hf)], w=[("sg", 2 + hf)])
                        P.op("pool", lambda hf=hf, sl=sl: nc.gpsimd.tensor_tensor(out=mrb[:, sl], in0=sga[:, sl], in1=sgs[:, sl], op=ALU.add),
                             r=[("sg", hf), ("sg", 2 + hf)], w=[("mrb", hf)])
                    P.op("pe", tr8(mrb, 0), r=[("mrb", 0), ("mrb", 1), "ident"], w=["b0"])
                    P.op("act", lambda: nc.scalar.copy(out=mT[:].rearrange("p c t -> p (c t)"), in_=bfv(0)), r=["b0"], w=["mT"])

                    def mmO():
                        for hf in range(2):
                            for c in range(KC):
                                ins = nc.tensor.matmul(bank[3 + hf][:], lhsT=mT[:, c, :], rhs=wo[:, c, hf * 512:(hf + 1) * 512], start=(c == 0), stop=(c == KC - 1))
                        return ins
                    P.op("pe", mmO, r=["mT", "wo"], w=["b3", "b4"])
                    for hf in range(2):
                        sl = slice(hf * 512, (hf + 1) * 512)
                        P.op("dve", lambda hf=hf, sl=sl, xcur=xcur: nc.vector.tensor_tensor(out=h1[:, sl], in0=bank[3 + hf][:], in1=xcur[:, sl], op=ALU.add),
                             r=["b%d" % (3 + hf), kx], w=[("h1", hf)])
                    hk = [("h1", 0), ("h1", 1)]
                    P.dma("pool", lambda t=t: nc.gpsimd.dma_start(out=sc_h1[t * 128:(t + 1) * 128, :], in_=h1[:]), r=hk, w=[("sc_h1", t)])
                    P.op("act", lambda: nc.scalar.activation(out=u[:], in_=h1[:], func=AF.Square, accum_out=st8[:, 2:3]), r=hk, w=["u", "ssh"])
                    rstd_ops(st8[:, 2:3], st8[:, 3:4], D, "ssh", "rsh")
                    P.op("dve", lambda: nc.vector.scalar_tensor_tensor(out=vh[:], in0=h1[:], scalar=st8[:, 3:4], in1=gffn[:], op0=ALU.mult, op1=ALU.mult),
                         r=hk + ["rsh", "gffn"], w=["vh"])
                    P.dma("pool", lambda t=t: nc.gpsimd.dma_start(out=sc_vh[t * 128:(t + 1) * 128, :], in_=vh[:]), r=["vh"], w=[("sc_vh", t)])
                    P.op("pe", tr8(vh, 0), r=["vh", "ident"], w=["b0"])
                    P.op("act", lambda: nc.scalar.copy(out=vT[:].rearrange("p c t -> p (c t)"), in_=bfv(0)), r=["b0"], w=["vT"])

                    def mmR():
                        for c in range(KC):
                            ins = nc.tensor.matmul(bank[7][:, 0:36], lhsT=vT[:, c, :], rhs=wrb[:, c, :], start=(c == 0), stop=(c == KC - 1))
                        return ins
                    P.op("pe", mmR, r=["vT", "wrb"], w=["b7"])
                    P.op("dve", lambda: nc.vector.tensor_tensor(out=lg[:], in0=bank[7][:, 0:36], in1=brt[:], op=ALU.add), r=["b7", "brt"], w=["lg"])
                    V = nc.vector
                    P.op("dve", lambda: V.reduce_max(out=sm[:, 0:1], in_=lg[:, 0:4], axis=AX.X), r=["lg"], w=["gmax"])
                    P.op("dve", lambda: V.tensor_scalar(out=goh[:], in0=lg[:, 0:4], scalar1=sm[:, 0:1], scalar2=None, op0=ALU.is_equal), r=["lg", "gmax"], w=["goh"])
                    P.op("dve", lambda: V.tensor_scalar(out=sm[:, 1:2], in0=sm[:, 0:1], scalar1=-1.0, scalar2=None, op0=ALU.mult), r=["gmax"], w=["ngmax"])
                    P.op("act", lambda: nc.scalar.activation(out=pen[:], in_=lg[:, 0:4], func=AF.Exp, bias=sm[:, 1:2], accum_out=sm[:, 2:3]),
                         r=["lg", "ngmax", "goh"], w=["pen", "gsum"])
                    P.op("dve", lambda: V.reciprocal(out=sm[:, 3:4], in_=sm[:, 2:3]), r=["gsum"], w=["pg"])
                    P.op("dve", lambda: V.tensor_scalar(out=pen[:], in0=goh[:], scalar1=-1.0, scalar2=1e9, op0=ALU.add, op1=ALU.mult), r=["goh", "pen"], w=["pen2"])
                    P.op("dve", lambda: V.tensor_tensor(out=elm[:].rearrange("p (g e) -> p g e", g=4), in0=lg[:, 4:36].rearrange("p (g e) -> p g e", g=4),
                                                        in1=pen[:].unsqueeze(2).to_broadcast([128, 4, 8]), op=ALU.add), r=["lg", "pen2"], w=["elm"])
                    P.op("dve", lambda: V.reduce_max(out=sm[:, 4:5], in_=elm[:], axis=AX.X), r=["elm"], w=["m1"])
                    P.op("dve", lambda r_=r_: V.tensor_scalar(out=oh1[:, r_, :], in0=elm[:], scalar1=sm[:, 4:5], scalar2=None, op0=ALU.is_equal),
                         r=["elm", "m1"], w=[("oh1", r_)])
                    P.op("dve", lambda r_=r_: V.scalar_tensor_tensor(out=elm2[:], in0=oh1[:, r_, :], scalar=-1e9, in1=elm[:], op0=ALU.mult, op1=ALU.add),
                         r=[("oh1", r_), "elm"], w=["elm2"])
                    P.op("dve", lambda: V.reduce_max(out=sm[:, 5:6], in_=elm2[:], axis=AX.X), r=["elm2"], w=["m2"])
                    P.op("dve", lambda r_=r_: V.tensor_scalar(out=oh2[:, r_, :], in0=elm2[:], scalar1=sm[:, 5:6], scalar2=None, op0=ALU.is_equal),
                         r=["elm2", "m2"], w=[("oh2", r_)])
                    P.op("dve", lambda: V.tensor_tensor(out=sm[:, 6:7], in0=sm[:, 5:6], in1=sm[:, 4:5], op=ALU.subtract), r=["m1", "m2"], w=["dd"])
                    P.op("act", lambda: nc.scalar.activation(out=sm[:, 7:8], in_=sm[:, 6:7], func=AF.Exp), r=["dd"], w=["e2"])
                    P.op("dve", lambda: V.tensor_scalar(out=sm[:, 8:9], in0=sm[:, 7:8], scalar1=1.0, scalar2=None, op0=ALU.add), r=["e2"], w=["t1"])
                    P.op("dve", lambda: V.reciprocal(out=sm[:, 9:10], in_=sm[:, 8:9]), r=["t1"], w=["rt"])
                    P.op("dve", lambda r_=r_: V.tensor_tensor(out=wts[:, r_, 0:1], in0=sm[:, 9:10], in1=sm[:, 3:4], op=ALU.mult), r=["rt", "pg"], w=[("w1", r_)])
                    P.op("dve", lambda r_=r_: V.tensor_tensor(out=wts[:, r_, 1:2], in0=sm[:, 3:4], in1=wts[:, r_, 0:1], op=ALU.subtract),
                         r=["pg", ("w1", r_)], w=[("w2", r_)])
                    P.op("dve", lambda r_=r_: V.tensor_tensor(out=Mt[:], in0=oh1[:, r_, :], in1=oh2[:, r_, :], op=ALU.add), r=[("oh1", r_), ("oh2", r_)], w=["Mt"])
                    P.op("dve", lambda: V.tensor_copy(out=Mtb[:], in_=Mt[:]), r=["Mt"], w=["Mtb"])
                    P.op("dve", lambda: V.tensor_copy(out=Maccb[:], in_=Macc[:]), r=["Macc"], w=["Maccb"])

                    def mmRk():
                        nc.tensor.matmul(bank[7][:, 64:96], lhsT=ustr[:], rhs=Mtb[:], start=True, stop=False)
                        return nc.tensor.matmul(bank[7][:, 64:96], lhsT=ones_b[:], rhs=Maccb[:], start=False, stop=True)
                    P.op("pe", mmRk, r=["ustr", "Mtb", "ones_b", "Maccb", "lg"], w=["b7"])
                    P.op("dve", lambda r_=r_: V.tensor_copy(out=rank[:, r_, :], in_=bank[7][:, 64:96]), r=["b7"], w=[("rank", r_)])
                    P.op("dve", lambda: V.tensor_tensor(out=Macc[:], in0=Macc[:], in1=Mt[:], op=ALU.add), r=["Macc", "Mt", "Maccb"], w=["Macc"])
                P.flush()
                if STOP <= 3:
                    return nc
        with ExitStack() as e4:
            s4 = lambda shape, dt: P.sb(e4, shape, dt)
            V = nc.vector
            cnt = s4([128, 32], F32)
            cnti = s4([128, 32], I32)
            pcf = s4([128, 32], F32)
            incl = s4([128, 32], F32)
            offx = s4([128, 32], F32)
            pos = s4([128, NR, 32], F32)
            tmp = s4([128, NR, 32], F32)
            slf = s4([128, 2, NR], F32)
            sli = s4([128, 2, NR], I32)
            thr = s4([128, NSLOT], F32)
            cmpt = s4([128, NSLOT, 32], F32)
            eidf = s4([128, NSLOT], F32)
            pidx = s4([128, 1], F32)
            widx = s4([128, NSLOT], I32)
            Maccb = s4([128, 32], BF16)
            P.op("dve", lambda: V.tensor_copy(out=Maccb[:], in_=Macc[:]), w=["Maccb"])
            P.op("pe", lambda: nc.tensor.matmul(bank[0][:, 0:32], lhsT=ones_b[:], rhs=Maccb[:], start=True, stop=True), r=["Maccb"], w=["b0"])
            P.op("dve", lambda: V.tensor_copy(out=cnt[:], in_=bank[0][:, 0:32]), r=["b0"], w=["cnt"])
            P.op("dve", lambda: V.tensor_copy(out=cnti[:], in_=cnt[:]), r=["cnt"], w=["cnti"])
            P.op("dve", lambda: V.tensor_single_scalar(out=cnti[:], in_=cnti[:], scalar=127, op=ALU.add), r=["cnti"], w=["cnti"])
            P.op("dve", lambda: V.tensor_scalar(out=cnti[:], in0=cnti[:], scalar1=7, scalar2=7, op0=ALU.arith_shift_right, op1=ALU.logical_shift_left),
                 r=["cnti"], w=["cnti"])
            P.op("dve", lambda: V.tensor_copy(out=pcf[:], in_=cnti[:]), r=["cnti"], w=["pcf"])
            P.op("dve", lambda: V.tensor_tensor_scan(out=incl[:], data0=ones_f[:, 0:32], data1=pcf[:], initial=0.0, op0=ALU.mult, op1=ALU.add),
                 r=["pcf"], w=["incl"])
            P.op("dve", lambda: V.tensor_tensor(out=offx[:], in0=incl[:], in1=pcf[:], op=ALU.subtract), r=["incl", "pcf"], w=["offx"])
            P.op("dve", lambda: V.tensor_tensor(out=pos[:], in0=rank[:], in1=offx[:].unsqueeze(1).to_broadcast([128, NR, 32]), op=ALU.add), r=["offx"], w=["pos"])
            for k, ohk in enumerate((oh1, oh2)):
                P.op("dve", lambda ohk=ohk: V.tensor_tensor(out=tmp[:], in0=pos[:], in1=ohk[:], op=ALU.mult), r=["pos", "slf"], w=["tmp"])
                P.op("dve", lambda k=k: V.reduce_sum(out=slf[:, k, :], in_=tmp[:], axis=AX.X), r=["tmp"], w=["slf"])
            P.op("dve", lambda: V.tensor_copy(out=sli[:], in_=slf[:]), r=["slf"], w=["sli"])
            P.op("pool", lambda: nc.gpsimd.iota(thr[:], pattern=[[128, NSLOT]], base=0, channel_multiplier=0, allow_small_or_imprecise_dtypes=True), w=["thr"])
            P.op("pool", lambda: nc.gpsimd.iota(pidx[:], pattern=[[0, 1]], base=-128, channel_multiplier=1, allow_small_or_imprecise_dtypes=True), w=["pidx"])
            P.op("dve", lambda: V.tensor_tensor(out=cmpt[:], in0=offx[:].unsqueeze(1).to_broadcast([128, NSLOT, 32]),
                                                in1=thr[:].unsqueeze(2).to_broadcast([128, NSLOT, 32]), op=ALU.is_le), r=["offx", "thr"], w=["cmpt"])
            P.op("dve", lambda: V.reduce_sum(out=eidf[:], in_=cmpt[:], axis=AX.X), r=["cmpt"], w=["eidf"])
            P.op("dve", lambda: V.tensor_scalar(out=eidf[:], in0=eidf[:], scalar1=128.0, scalar2=pidx[:, 0:1], op0=ALU.mult, op1=ALU.add),
                 r=["eidf", "pidx"], w=["eidf"])
            P.op("dve", lambda: V.tensor_copy(out=widx[:], in_=eidf[:]), r=["eidf"], w=["widx"])
            vt = [s4([128, D], BF16) for _ in range(2)]
            for r_ in range(NR):
                t = r_ + 1
                b = r_ % 2
                P.dma("sp", lambda t=t, b=b: nc.sync.dma_start(out=vt[b][:], in_=sc_vh[t * 128:(t + 1) * 128, :]), w=[("vt", b)])
                for k in range(2):
                    P.dma("pool", lambda r_=r_, k=k, b=b: nc.gpsimd.indirect_dma_start(
                        out=sc_xs, out_offset=bass.IndirectOffsetOnAxis(ap=sli[:, k, r_:r_ + 1], axis=0), in_=vt[b][:, :], in_offset=None),
                        r=[("vt", b), "sli"], w=[("sc_xs", r_, k)])
            NWB = 3
            wcat = [s4([128, WC], BF16) for _ in range(NWB)]
            NXB = 3
            xsl = [s4([128, D], BF16) for _ in range(NXB)]
            xsT = [s4([128, KC, 128], BF16) for _ in range(2)]
            sgt = s4([128, 256], F32)
            hh = [s4([128, 256], BF16) for _ in range(2)]
            hT = s4([128, 2, 128], BF16)
            ysb = [s4([128, D], F32) for _ in range(2)]
            scat_keys = [("sc_xs", rr, kk) for rr in range(NR) for kk in range(2)]

            def load_x(i):
                xb = i % NXB
                P.dma("sp", lambda: nc.sync.dma_start(out=xsl[xb][:], in_=sc_xs[i * 128:(i + 1) * 128, :]), r=scat_keys, w=[("xsl", xb)])

            def stage4A(i):
                b2, xb, wb = i % 2, i % NXB, i % NWB
                bx = 0 if b2 == 0 else 5
                bg = 1 if b2 == 0 else 6
                P.dma("pool", lambda: nc.gpsimd.indirect_dma_start(
                    out=wcat[wb][:, :], out_offset=None, in_=sc_wc, in_offset=bass.IndirectOffsetOnAxis(ap=widx[:, i:i + 1], axis=0)),
                    r=["widx"], w=[("wcat", wb)])
                if i + 2 < NSLOT:
                    load_x(i + 2)

                def trX():
                    for c in range(KC):
                        ins = nc.tensor.transpose(out=bfv(bx)[:, c * 128:(c + 1) * 128], in_=xsl[xb][:, c * 128:(c + 1) * 128], identity=ident[:])
                    return ins
                P.op("pe", trX, r=[("xsl", xb), "ident"], w=["b%d" % bx])
                P.op("dve", lambda: V.tensor_copy(out=xsT[b2][:].rearrange("p c t -> p (c t)"), in_=bfv(bx)), r=["b%d" % bx], w=[("xsT", b2)])
                yield

                def mmGU():
                    for c in range(KC):
                        ins = nc.tensor.matmul(bank[bg][:], lhsT=xsT[b2][:, c, :], rhs=wcat[wb][:, c * 512:(c + 1) * 512], start=(c == 0), stop=(c == KC - 1))
                    return ins
                P.op("pe", mmGU, r=[("xsT", b2), ("wcat", wb)], w=["b%d" % bg])
                P.op("act", lambda: nc.scalar.activation(out=sgt[:], in_=bank[bg][:, 0:256], func=AF.Silu), r=["b%d" % bg], w=["sgt"])
                P.op("dve", lambda: V.tensor_tensor(out=hh[b2][:], in0=bank[bg][:, 256:512], in1=sgt[:], op=ALU.mult), r=["b%d" % bg, "sgt"], w=[("hh", b2)])
                yield

            def stage4B(i):
                b2, wb = i % 2, i % NWB
                bh = 2 if b2 == 0 else 7

                def trH():
                    for c in range(2):
                        ins = nc.tensor.transpose(out=bfv(bh)[:, c * 128:(c + 1) * 128], in_=hh[b2][:, c * 128:(c + 1) * 128], identity=ident[:])
                    return ins
                P.op("pe", trH, r=[("hh", b2), "ident"], w=["b%d" % bh])
                P.op("act", lambda: nc.scalar.copy(out=hT[:].rearrange("p c t -> p (c t)"), in_=bfv(bh)[:, 0:256]), r=["b%d" % bh], w=["hT"])
                yield

                def mmD():
                    for hf in range(2):
                        for c in range(2):
                            ins = nc.tensor.matmul(bank[3 + hf][:], lhsT=hT[:, c, :], rhs=wcat[wb][:, WGU_C + c * D + hf * 512: WGU_C + c * D + (hf + 1) * 512],
                                                   start=(c == 0), stop=(c == 1))
                    return ins
                P.op("pe", mmD, r=["hT", ("wcat", wb)], w=["b3", "b4"])
                P.op("act", lambda: nc.scalar.copy(out=ysb[b2][:, 0:512], in_=bank[3][:]), r=["b3"], w=[("ysbA", b2)])
                P.op("dve", lambda: V.tensor_copy(out=ysb[b2][:, 512:1024], in_=bank[4][:]), r=["b4"], w=[("ysbB", b2)])
                P.dma("sp", lambda: nc.sync.dma_start(out=sc_ys[i * 128:(i + 1) * 128, :], in_=ysb[b2][:]), r=[("ysbA", b2), ("ysbB", b2)], w=[("sc_ys", i)])
                yield

            load_x(0)
            load_x(1)
            for n in range(NSLOT + 1):
                gens = []
                if n - 1 >= 0:
                    gens.append(stage4B(n - 1))
                if n < NSLOT:
                    gens.append(stage4A(n))
                while gens:
                    for g_ in list(gens):
                        try:
                            next(g_)
                        except StopIteration:
                            gens.remove(g_)
            P.flush()
            if STOP <= 4:
                return nc
            gfin = s4([128, D], F32)
            P.dma("sp", lambda: nc.sync.dma_start(out=gfin[:], in_=g_fin), w=["gfin"])
            NB5 = 4
            h1t = [s4([128, D], F32) for _ in range(NB5)]
            y1 = [s4([128, D], F32) for _ in range(NB5)]
            y2 = [s4([128, D], F32) for _ in range(NB5)]
            ot = [s4([128, D], F32) for _ in range(NB5)]
            junk5 = s4([128, D], BF16)
            st5 = s4([128, 4], F32)
            for r_ in range(NR):
                t = r_ + 1
                b = r_ % NB5
                P.dma("sp", lambda t=t, b=b: nc.sync.dma_start(out=h1t[b][:], in_=sc_h1[t * 128:(t + 1) * 128, :]), w=[("h1t", b)])
                for k, yk in enumerate((y1, y2)):
                    P.dma("pool", lambda r_=r_, k=k, b=b, yk=yk: nc.gpsimd.indirect_dma_start(
                        out=yk[b][:, :], out_offset=None, in_=sc_ys, in_offset=bass.IndirectOffsetOnAxis(ap=sli[:, k, r_:r_ + 1], axis=0)),
                        w=[("y", k, b)])
                P.op("dve", lambda r_=r_, b=b: V.scalar_tensor_tensor(out=ot[b][:], in0=y1[b][:], scalar=wts[:, r_, 0:1], in1=h1t[b][:], op0=ALU.mult, op1=ALU.add),
                     r=[("y", 0, b), ("h1t", b)], w=[("ot", b)])
                P.op("dve", lambda r_=r_, b=b: V.scalar_tensor_tensor(out=ot[b][:], in0=y2[b][:], scalar=wts[:, r_, 1:2], in1=ot[b][:], op0=ALU.mult, op1=ALU.add),
                     r=[("y", 1, b), ("ot", b)], w=[("ot", b)])
                P.op("act", lambda b=b: nc.scalar.activation(out=junk5[:], in_=ot[b][:], func=AF.Square, accum_out=st5[:, 0:1]), r=[("ot", b)], w=["junk5", "ss5"])
                rstd_ops(st5[:, 0:1], st5[:, 1:2], D, "ss5", "rs5")
                P.op("dve", lambda b=b: V.scalar_tensor_tensor(out=ot[b][:], in0=ot[b][:], scalar=st5[:, 1:2], in1=gfin[:], op0=ALU.mult, op1=ALU.mult),
                     r=[("ot", b), "rs5", "gfin"], w=[("ot", b)])
                P.dma("sp", lambda r_=r_, b=b: nc.sync.dma_start(out=y_out[r_ * 128:(r_ + 1) * 128, :], in_=ot[b][:]), r=[("ot", b)])
            P.flush()
    return nc


def _kc(w):
    K, N = w.shape
    return np.ascontiguousarray(w.reshape(K // 128, 128, N).transpose(1, 0, 2))


def _bc(v, n=128):
    return np.ascontiguousarray(np.broadcast_to(np.asarray(v, np.float32).reshape(1, -1), (n, v.size)))


def _rope_tables(NT):
    TOK = NT * 128
    pos = np.maximum(np.arange(TOK) - 112, 0).astype(np.float32)
    inv = (np.float32(10000.0) ** (-np.arange(0, 32, 2, dtype=np.float32) / np.float32(32))).astype(np.float32)
    ang = (pos[:, None] * inv[None, :]).astype(np.float32)
    cos, sin = np.cos(ang).astype(np.float32), np.sin(ang).astype(np.float32)
    C2 = np.concatenate([cos, cos], axis=1)
    S2 = np.concatenate([-sin, sin], axis=1)
    c2tok = np.ascontiguousarray(C2.reshape(NT, 128, 32).transpose(1, 0, 2))
    s2tok = np.ascontiguousarray(S2.reshape(NT, 128, 32).transpose(1, 0, 2))
    return c2tok, s2tok, np.ascontiguousarray(C2.T), np.ascontiguousarray(S2.T)


def prep_shared(inp, NT):
    f = lambda k: np.asarray(inp[k], np.float32)
    w_in = f("w_in")[0]
    cols = np.concatenate([np.arange(0, 416), np.arange(400, 416), np.arange(384, 400), np.arange(2720, 2736),
                           np.arange(416, 1440), np.arange(1440, 2720), np.arange(2736, 3760), np.arange(3760, 4784)])
    assert cols.size == W_IN_COLS
    m = {"w_in": _kc(w_in[:, cols]), "meta": f("meta_tokens")}
    m["g_mix"] = _bc(f("norm_mix")[0]); m["g_q"] = _bc(f("mla_q_norm")[0]); m["g_kv"] = _bc(f("mla_kv_norm")[0])
    m["g_ssm"] = _bc(f("ssm_norm")[0]); m["g_ffn"] = _bc(f("norm_ffn")[0]); m["g_fin"] = _bc(f("norm_final"))
    wq = f("mla_w_uq")[0].reshape(256, 16, 96)
    wqa = np.zeros((256, 16, 128), np.float32)
    wqa[:, :, 0:32] = wq[:, :, 64:96]
    wqa[:, :, 64:128] = wq[:, :, 0:64]
    wqs = np.concatenate([wq[:, :, 80:96], wq[:, :, 64:80]], axis=2)
    m["w_uqa"] = np.ascontiguousarray(wqa.reshape(2, 128, 16, 128).transpose(1, 0, 2, 3))
    m["w_uqs"] = np.ascontiguousarray(wqs.reshape(2, 128, 16, 32).transpose(1, 0, 2, 3))
    wkv = f("mla_w_ukv")[0].reshape(128, 16, 128)
    m["w_uk"] = np.ascontiguousarray(wkv[:, :, 0:64]); m["w_uv"] = np.ascontiguousarray(wkv[:, :, 64:128])
    m["c2tok"], m["s2tok"], m["c2T"], m["s2T"] = _rope_tables(NT)
    cw = f("ssm_conv_w")[0]
    m["conv_w"] = np.ascontiguousarray(cw.reshape(4, 10, 128).transpose(2, 1, 0))
    m["conv_b"] = np.ascontiguousarray(f("ssm_conv_b")[0].reshape(10, 128).T)
    m["dt_bias"] = _bc(f("ssm_dt_bias")[0]); m["a_log"] = _bc(f("ssm_a_log")[0]); m["d_skip"] = _bc(f("ssm_d_skip")[0])
    m["w_ba"] = _kc(f("w_branch_attn")[0]); m["w_bs"] = _kc(f("w_branch_ssm")[0]); m["w_out"] = _kc(f("w_out")[0])
    m["w_r"] = _kc(np.concatenate([f("moe_w_group")[0], f("moe_w_expert")[0]], axis=1))
    m["b_r"] = _bc(np.concatenate([f("moe_b_group")[0], f("moe_b_expert")[0]]))
    wg, wu, wd = f("moe_w_gate")[0], f("moe_w_up")[0], f("moe_w_down")[0]
    gu = np.concatenate([wg, wu], axis=2).reshape(NEXP, KC, 128, 512).transpose(0, 2, 1, 3)
    m["w_gu"] = np.ascontiguousarray(gu).reshape(NEXP * 128, KC * 512)
    dn = wd.reshape(NEXP, 2, 128, D).transpose(0, 2, 1, 3)
    m["w_dn"] = np.ascontiguousarray(dn).reshape(NEXP * 128, 2 * D)
    return m


_CACHE = {}


def run(inputs, n_cores=None):
    x = np.asarray(inputs["x"], np.float32)
    B, S, _ = x.shape
    NT = S // 128 + 1
    if NT not in _CACHE:
        _CACHE[NT] = build(NT)
    nc = _CACHE[NT]
    shared = prep_shared(inputs, NT)
    in_maps = []
    for b in range(B):
        m = dict(shared)
        m["x"] = np.ascontiguousarray(x[b])
        in_maps.append(m)
    res = run_bass_kernel_spmd(nc, in_maps, core_ids=list(range(B)))
    return np.stack([np.asarray(r["y"], np.float32).reshape(S, D) for r in res.results], axis=0)


def kernel(**inputs):
    return run(inputs)
```

```python
import os
import numpy as np
from contextlib import ExitStack
import concourse.bass as bass
import concourse.mybir as mybir
from concourse.bass_utils import run_bass_kernel_spmd

F32 = mybir.dt.float32
BF16 = mybir.dt.bfloat16
I32 = mybir.dt.int32
ALU = mybir.AluOpType
AF = mybir.ActivationFunctionType
AX = mybir.AxisListType

N_DMA_SEMS = 3
D = 1024
KC = 8
NEXP = 32
EPS = 1e-6
A_W = 464
OFF_Z, OFF_X, OFF_GA, OFF_GS = 464, 1488, 2768, 3792
P1_COLS = 2768
W_IN_COLS = 4816


class Prog:
    def __init__(self, nc, es):
        self.nc = nc
        self.es = es
        self.eng = {"pe": nc.tensor, "act": nc.scalar, "dve": nc.vector,
                    "pool": nc.gpsimd, "sp": nc.sync}
        self.ops = []
        self.sem = {k: es.enter_context(nc.semaphore("s_" + k)) for k in self.eng}
        self.dsem = {}
        for q in ("sp", "pool", "act"):
            self.dsem[q] = [es.enter_context(nc.semaphore(f"d_{q}{i}")) for i in range(N_DMA_SEMS)]
        self.cnt = {k: 0 for k in self.eng}
        self.known = {}
        self.dcount = {}
        self.drr = {q: 0 for q in self.dsem}
        self.n = 0
        self.total_ops = 0

    def sb(self, es, shape, dtype, name=None):
        self.n += 1
        return es.enter_context(self.nc.sbuf_tensor(name or f"sb{self.n}", list(shape), dtype))

    def ps(self, es, shape, dtype, name=None):
        self.n += 1
        return es.enter_context(self.nc.psum_tensor(name or f"ps{self.n}", list(shape), dtype))

    def op(self, eng, fn, r=(), w=(), dma=False):
        self.nrec = getattr(self, "nrec", 0) + 1
        if self.nrec > int(os.environ.get("KOPS", "100000000")):
            return
        self.ops.append((eng, fn, tuple(r), tuple(w), dma))

    def dma(self, q, fn, r=(), w=()):
        self.op(q, fn, r, w, dma=True)

    def _wait(self, eng, s, v):
        kk = (eng, id(s))
        if self.known.get(kk, 0) >= v:
            return
        self.known[kk] = v
        self.eng[eng].wait_ge(s, v)

    def flush(self):
        ops = self.ops
        self.ops = []
        n = len(ops)
        self.total_ops += n
        last_w, readers = {}, {}
        deps = [None] * n
        for i, (eng, fn, r, w, dma) in enumerate(ops):
            d = set()
            for k in r:
                if k in last_w:
                    d.add(last_w[k])
            for k in w:
                if k in last_w:
                    d.add(last_w[k])
                d.update(readers.get(k, ()))
            d.discard(i)
            deps[i] = d
            for k in r:
                readers.setdefault(k, []).append(i)
            for k in w:
                last_w[k] = i
                readers[k] = []
        signals = [False] * n
        last_of = {}
        for i in range(n):
            eng_i, dma_i = ops[i][0], ops[i][4]
            if not dma_i:
                last_of[eng_i] = i
            keep = {}
            for j in deps[i]:
                eng_j, dma_j = ops[j][0], ops[j][4]
                if dma_j:
                    keep[("dma", j)] = j
                    continue
                if eng_j == "pe" and eng_i == "pe" and not dma_i:
                    continue
                key = ("eng", eng_j)
                if key not in keep or keep[key] < j:
                    keep[key] = j
            deps[i] = sorted(keep.values())
            for j in deps[i]:
                signals[j] = True
        for e, i in last_of.items():
            signals[i] = True
        tok = [None] * n
        for i, (eng, fn, r, w, dma) in enumerate(ops):
            waits = [tok[j] for j in deps[i]]
            s = None
            if dma:
                pool = self.dsem[eng]
                s = pool[self.drr[eng] % len(pool)]
                self.drr[eng] += 1
                c = self.dcount.get(id(s), 0)
                if c > 0:
                    waits.append((s, c))
            for (ws, wv) in waits:
                self._wait(eng, ws, wv)
            res = fn()
            if dma:
                lst = res if isinstance(res, (list, tuple)) else [res]
                c = self.dcount.get(id(s), 0)
                for ins in lst:
                    ins.then_inc(s, 16)
                    c += 16
                self.dcount[id(s)] = c
                tok[i] = (s, c)
            elif signals[i]:
                self.cnt[eng] += 1
                res.then_inc(self.sem[eng], 1)
                tok[i] = (self.sem[eng], self.cnt[eng])
        for wname in self.eng:
            for e2 in self.eng:
                if e2 != wname and self.cnt[e2] > 0:
                    self._wait(wname, self.sem[e2], self.cnt[e2])
            for q, pool in self.dsem.items():
                for s in pool:
                    c = self.dcount.get(id(s), 0)
                    if c > 0:
                        self._wait(wname, s, c)


def build(NT):
    STOP = int(os.environ.get('KSTOP', '99'))
    NR = NT - 1
    TOK = NT * 128
    NSLOT = 2 * NR + NEXP
    nc = bass.Bass("TRN2", target_bir_lowering=False)

    def din(name, shape, dt=F32):
        return nc.dram_tensor(name, list(shape), dt, kind="ExternalInput").ap()

    x = din("x", [NR * 128, D])
    meta = din("meta", [16, D])
    w_in = din("w_in", [128, KC, W_IN_COLS])
    g_mix = din("g_mix", [128, D])
    g_q = din("g_q", [128, 256])
    g_kv = din("g_kv", [128, 128])
    g_ssm = din("g_ssm", [128, D])
    g_ffn = din("g_ffn", [128, D])
    g_fin = din("g_fin", [128, D])
    w_uqa = din("w_uqa", [128, 2, 16, 128])
    w_uqs = din("w_uqs", [128, 2, 16, 32])
    w_uk = din("w_uk", [128, 16, 64])
    w_uv = din("w_uv", [128, 16, 64])
    c2tok = din("c2tok", [128, NT, 32])
    s2tok = din("s2tok", [128, NT, 32])
    c2T = din("c2T", [32, TOK])
    s2T = din("s2T", [32, TOK])
    conv_w = din("conv_w", [128, 10, 4])
    conv_b = din("conv_b", [128, 10])
    dt_bias = din("dt_bias", [128, 16])
    a_log = din("a_log", [128, 16])
    d_skip = din("d_skip", [128, 16])
    w_ba = din("w_ba", [128, KC, D])
    w_bs = din("w_bs", [128, KC, D])
    w_out = din("w_out", [128, KC, D])
    w_r = din("w_r", [128, KC, 36])
    b_r = din("b_r", [128, 36])
    w_gu = din("w_gu", [NEXP * 128, KC * 512])
    w_dn = din("w_dn", [NEXP * 128, 2 * D])
    y_out = nc.dram_tensor("y", [NR * 128, D], F32, kind="ExternalOutput").ap()

    def scratch(name, shape, dt):
        return nc.dram_tensor(name, list(shape), dt, kind="Internal").ap()

    sc_osT = scratch("sc_osT", [TOK, D], BF16)
    sc_cq = scratch("sc_cq", [128, 2, TOK], BF16)
    sc_ckv = scratch("sc_ckv", [128, TOK], BF16)
    sc_kr = scratch("sc_kr", [32, TOK], BF16)
    sc_h1 = scratch("sc_h1", [TOK, D], F32)
    sc_vh = scratch("sc_vh", [TOK, D], BF16)
    sc_xs = scratch("sc_xs", [NSLOT * 128, D], BF16)
    sc_ys = scratch("sc_ys", [NSLOT * 128, D], F32)
    WGU_C, WC = KC * 512, KC * 512 + 2 * D
    sc_wc = scratch("sc_wc", [NEXP * 128, WC], BF16)

    with ExitStack() as es:
        P = Prog(nc, es)
        sb = lambda shape, dt, e=es: P.sb(e, shape, dt)
        bank = [P.ps(es, [128, 512], F32, name=f"bank{i}") for i in range(8)]

        def bfv(i):
            return bank[i][:].bitcast(BF16)

        ident = sb([128, 128], BF16)
        identf = sb([128, 128], F32)
        uincl = sb([128, 128], F32)
        mask01 = sb([128, 128], F32)
        negm = sb([128, 8, 128], BF16)
        ones_f = sb([128, 128], F32)
        ones_b = sb([128, 128], BF16)
        epst = sb([128, 1], F32)
        onet = sb([128, 1], F32)
        mask0 = sb([128, 1], F32)
        A_bc = sb([128, 16], F32)
        dtb_bc = sb([128, 16], F32)
        dsk_bc = sb([128, 16], F32)
        oh1 = sb([128, NR, 32], F32)
        oh2 = sb([128, NR, 32], F32)
        rank = sb([128, NR, 32], F32)
        wts = sb([128, NR, 2], F32)
        Macc = sb([128, 32], F32)

        P.op("pool", lambda: nc.gpsimd.memset(identf[:], 1.0), w=["identf"])
        P.op("pool", lambda: nc.gpsimd.affine_select(out=identf[:], in_=identf[:], pattern=[[-1, 128]],
                                                      compare_op=ALU.is_equal, fill=0.0, base=0, channel_multiplier=1),
             r=["identf"], w=["identf"])
        P.op("dve", lambda: nc.vector.tensor_copy(out=ident[:], in_=identf[:]), r=["identf"], w=["ident"])
        P.op("pool", lambda: nc.gpsimd.memset(ones_f[:], 1.0), w=["ones_f"])
        P.op("pool", lambda: nc.gpsimd.memset(ones_b[:], 1.0), w=["ones_b"])
        P.op("pool", lambda: nc.gpsimd.affine_select(out=uincl[:], in_=ones_f[:], pattern=[[1, 128]],
                                                      compare_op=ALU.is_ge, fill=0.0, base=0, channel_multiplier=-1),
             r=["ones_f"], w=["uincl"])
        P.op("dve", lambda: nc.vector.tensor_copy(out=mask01[:], in_=uincl[:]), r=["uincl"], w=["mask01"])
        P.op("dve", lambda: nc.vector.tensor_scalar(out=negm[:], in0=uincl[:].unsqueeze(1).to_broadcast([128, 8, 128]),
                                                    scalar1=-1.0, scalar2=30000.0, op0=ALU.add, op1=ALU.mult),
             r=["uincl"], w=["negm"])
        P.op("dve", lambda: nc.vector.memset(epst[:], EPS), w=["eps"])
        P.op("dve", lambda: nc.vector.memset(onet[:], 1.0), w=["onet"])
        P.op("dve", lambda: nc.vector.memset(mask0[:], 1.0), w=["mask0"])
        P.op("dve", lambda: nc.vector.memset(mask0[0:112, :], 0.0), r=["mask0"], w=["mask0"])
        P.dma("sp", lambda: nc.sync.dma_start(out=A_bc[:], in_=a_log), w=["A_bc"])
        P.dma("sp", lambda: nc.sync.dma_start(out=dtb_bc[:], in_=dt_bias), w=["dtb"])
        P.dma("sp", lambda: nc.sync.dma_start(out=dsk_bc[:], in_=d_skip), w=["dsk"])
        P.op("act", lambda: nc.scalar.activation(out=A_bc[:], in_=A_bc[:], func=AF.Exp), r=["A_bc"], w=["A_bc"])
        P.op("dve", lambda: nc.vector.tensor_scalar(out=A_bc[:], in0=A_bc[:], scalar1=-1.0, scalar2=None, op0=ALU.mult),
             r=["A_bc"], w=["A_bc"])
        P.flush()

        def rstd_ops(ss, out, n, key_in, key_out, extra_scale=None):
            P.op("act", lambda: nc.scalar.activation(out=out, in_=ss, func=AF.Ln, scale=1.0 / n, bias=epst[:, 0:1]),
                 r=[key_in, "eps"], w=[key_out])
            P.op("act", lambda: nc.scalar.activation(out=out, in_=out, func=AF.Exp, scale=-0.5), r=[key_out], w=[key_out])
            if extra_scale is not None:
                P.op("dve", lambda: nc.vector.tensor_scalar(out=out, in0=out, scalar1=float(extra_scale), scalar2=None,
                                                            op0=ALU.mult), r=[key_out], w=[key_out])

        ldc = [0]

        def load_w(dst, src, ncols, src_off, stg, key, nk=KC):
            for c in range(nk):
                P.dma("pool", lambda c=c: nc.gpsimd.dma_start(out=dst[:, c, 0:ncols], in_=src[:, c, src_off:src_off + ncols]), w=[key])

        with ExitStack() as e1:
            s1 = lambda shape, dt: P.sb(e1, shape, dt)
            w_inb = s1([128, KC, P1_COLS], BF16)
            stg = [s1([128, 512], F32) for _ in range(2)]
            load_w(w_inb, w_in, P1_COLS, 0, stg, "w_inb")
            gmix = s1([128, D], F32)
            gq = s1([128, 256], F32)
            gkv = s1([128, 128], F32)
            gss = s1([128, D], F32)
            c2t = s1([128, NT, 32], F32)
            s2t = s1([128, NT, 32], F32)
            cw = s1([128, 10, 4], F32)
            cb = s1([128, 10], F32)
            for dst, src, k in ((gmix, g_mix, "gmix"), (gq, g_q, "gq"), (gkv, g_kv, "gkv"), (gss, g_ssm, "gss"),
                                (c2t, c2tok, "c2t"), (s2t, s2tok, "s2t"), (cw, conv_w, "cw"), (cb, conv_b, "cb")):
                P.dma("sp", lambda dst=dst, src=src: nc.sync.dma_start(out=dst[:], in_=src), w=[k])

            def mb(n, shape, dt):
                return [s1(shape, dt) for _ in range(n)]
            xt = mb(2, [128, D], F32)
            xt0 = s1([128, D], F32)
            junk = s1([128, D], BF16)
            u = s1([128, D], BF16)
            uT = mb(2, [128, KC, 128], BF16)
            st8 = s1([128, 8], F32)
            tA = s1([128, 512], BF16)
            tAT = s1([128, 512], BF16)
            kr1 = s1([128, 32], F32)
            kr2 = s1([128, 32], F32)
            dtt = mb(2, [128, 16], F32)
            at = mb(2, [128, 16], F32)
            zs = mb(3, [128, D], BF16)
            xh = s1([128, 10, 132], F32)
            cacc = s1([128, 10, 128], F32)
            xbcs = mb(3, [128, 10, 128], BF16)
            xtok = mb(2, [128, D], BF16)
            btok = mb(2, [128, 128], BF16)
            xdt = mb(2, [128, D], BF16)
            xdtd = mb(2, [128, D], BF16)
            acs = s1([128, 16], F32)
            eacs = mb(2, [128, 16], F32)
            Rg = s1([128, 8, 128], F32)
            Dg = s1([128, 8, 128], F32)
            LT = s1([128, 8, 128], F32)
            dec = s1([128, 16], F32)
            cbtm = s1([128, 2, 128], F32)
            MT = mb(2, [128, 16, 128], BF16)
            cdsel = mb(2, [128, 8], F32)
            Sst = s1([128, 512], F32)
            prevb = s1([128, 512], BF16)
            yd = s1([128, D], F32)
            yy = s1([128, D], F32)
            ssg = s1([128, 4], F32)
            osm = s1([128, D], BF16)
            osmT = s1([128, KC, 128], BF16)

            P.op("dve", lambda: nc.vector.memset(xt0[:], 0.0), w=["xt0"])
            P.dma("sp", lambda: nc.sync.dma_start(out=xt0[112:128, :], in_=meta), r=["xt0"], w=["xt0"])
            P.op("dve", lambda: nc.vector.memset(xh[:], 0.0), w=["xh", ("xh", 0), ("xh", 1), ("xh", 2)])
            P.op("dve", lambda: nc.vector.memset(Sst[:], 0.0), w=["Sst"])
            P.op("pool", lambda: nc.gpsimd.memset(tA[:], 0.0), w=["tA"])

            def stageA(t):
                i2, i3 = t % 2, t % 3
                xcur = xt0 if t == 0 else xt[i2]
                kx = "xt0" if t == 0 else ("xt", i2)
                uTc, kuT = uT[i2], ("uT", i2)
                dttc, kdt = dtt[i2], ("dtt", i2)
                atc, kat = at[i2], ("at", i2)
                zsc, kzs = zs[i3], ("zs", i3)
                xb, kxb = xbcs[i3], ("xbcs", i3)
                if t > 0:
                    P.dma("sp", lambda: nc.sync.dma_start(out=xcur[:], in_=x[(t - 1) * 128:t * 128, :]), w=[kx])
                P.op("act", lambda: nc.scalar.activation(out=junk[:], in_=xcur[:], func=AF.Square, accum_out=st8[:, 0:1]),
                     r=[kx], w=["junk", "ssx"])
                rstd_ops(st8[:, 0:1], st8[:, 1:2], D, "ssx", "rsx")
                P.op("dve", lambda: nc.vector.scalar_tensor_tensor(out=u[:], in0=xcur[:], scalar=st8[:, 1:2], in1=gmix[:],
                                                                   op0=ALU.mult, op1=ALU.mult), r=[kx, "rsx", "gmix"], w=["u"])

                def tr_u():
                    for c in range(KC):
                        ins = nc.tensor.transpose(out=bfv(0)[:, c * 128:(c + 1) * 128], in_=u[:, c * 128:(c + 1) * 128], identity=ident[:])
                    return ins
                P.op("pe", tr_u, r=["u", "ident"], w=["b0"])
                P.op("act", lambda: nc.scalar.copy(out=uTc[:].rearrange("p c t -> p (c t)"), in_=bfv(0)), r=["b0"], w=[kuT])
                yield

                def mmA():
                    for c in range(KC):
                        ins = nc.tensor.matmul(bank[1][:, 0:A_W], lhsT=uTc[:, c, :], rhs=w_inb[:, c, 0:A_W], start=(c == 0), stop=(c == KC - 1))
                    return ins
                P.op("pe", mmA, r=[kuT, "w_inb"], w=["b1"])
                P.op("act", lambda: nc.scalar.activation(out=junk[:, 0:256], in_=bank[1][:, 0:256], func=AF.Square, accum_out=st8[:, 2:3]),
                     r=["b1"], w=["junk", "ssq"])
                P.op("act", lambda: nc.scalar.activation(out=junk[:, 256:384], in_=bank[1][:, 256:384], func=AF.Square, accum_out=st8[:, 4:5]),
                     r=["b1"], w=["junk", "sskv"])
                rstd_ops(st8[:, 2:3], st8[:, 3:4], 256, "ssq", "rsq", extra_scale=96 ** -0.5)
                rstd_ops(st8[:, 4:5], st8[:, 5:6], 128, "sskv", "rskv")
                P.op("dve", lambda: nc.vector.scalar_tensor_tensor(out=tA[:, 0:256], in0=bank[1][:, 0:256], scalar=st8[:, 3:4], in1=gq[:],
                                                                   op0=ALU.mult, op1=ALU.mult), r=["b1", "rsq", "gq"], w=["tA"])
                P.op("dve", lambda: nc.vector.scalar_tensor_tensor(out=tA[:, 256:384], in0=bank[1][:, 256:384], scalar=st8[:, 5:6], in1=gkv[:],
                                                                   op0=ALU.mult, op1=ALU.mult), r=["b1", "rskv", "gkv"], w=["tA"])
                P.op("dve", lambda: nc.vector.tensor_tensor(out=kr1[:], in0=bank[1][:, 384:416], in1=c2t[:, t, :], op=ALU.mult),
                     r=["b1", "c2t"], w=["kr1"])
                P.op("dve", lambda: nc.vector.tensor_tensor(out=kr2[:], in0=bank[1][:, 416:448], in1=s2t[:, t, :], op=ALU.mult),
                     r=["b1", "s2t"], w=["kr2"])
                P.op("dve", lambda: nc.vector.tensor_tensor(out=tA[:, 384:416], in0=kr1[:], in1=kr2[:], op=ALU.add),
                     r=["kr1", "kr2"], w=["tA"])
                P.op("dve", lambda: nc.vector.tensor_tensor(out=dttc[:], in0=bank[1][:, 448:464], in1=dtb_bc[:], op=ALU.add),
                     r=["b1", "dtb"], w=[kdt])
                P.op("act", lambda: nc.scalar.activation(out=dttc[:], in_=dttc[:], func=AF.Exp), r=[kdt], w=[kdt])
                P.op("act", lambda: nc.scalar.activation(out=dttc[:], in_=dttc[:], func=AF.Ln, bias=onet[:, 0:1]), r=[kdt, "onet"], w=[kdt])
                if t == 0:
                    P.op("dve", lambda: nc.vector.tensor_scalar(out=dttc[:], in0=dttc[:], scalar1=mask0[:, 0:1], scalar2=None, op0=ALU.mult),
                         r=[kdt, "mask0"], w=[kdt])
                P.op("dve", lambda: nc.vector.tensor_tensor(out=atc[:], in0=dttc[:], in1=A_bc[:], op=ALU.mult), r=[kdt, "A_bc"], w=[kat])
                yield

                def mm_z():
                    for hf in range(2):
                        for c in range(KC):
                            ins = nc.tensor.matmul(bank[2 + hf][:], lhsT=uTc[:, c, :], rhs=w_inb[:, c, OFF_Z + hf * 512: OFF_Z + (hf + 1) * 512],
                                                   start=(c == 0), stop=(c == KC - 1))
                    return ins
                P.op("pe", mm_z, r=[kuT, "w_inb"], w=["b2", "b3"])
                for hf in range(2):
                    P.op("act", lambda hf=hf: nc.scalar.activation(out=zsc[:, hf * 512:(hf + 1) * 512], in_=bank[2 + hf][:], func=AF.Silu),
                         r=["b%d" % (2 + hf)], w=[kzs])
                yield

                def tr_A():
                    for c in range(3):
                        nc.tensor.transpose(out=bfv(0)[:, c * 128:(c + 1) * 128], in_=tA[:, c * 128:(c + 1) * 128], identity=ident[:])
                    return nc.tensor.transpose(out=bfv(0)[0:32, 384:512], in_=tA[:, 384:416], identity=ident[:])
                P.op("pe", tr_A, r=["tA", "ident"], w=["b0"])
                P.op("act", lambda: nc.scalar.copy(out=tAT[:, 0:384], in_=bfv(0)[:, 0:384]), r=["b0"], w=["tATa"])
                P.op("act", lambda: nc.scalar.copy(out=tAT[0:32, 384:512], in_=bfv(0)[0:32, 384:512]), r=["b0"], w=["tATb"])
                P.dma("sp", lambda: nc.sync.dma_start(out=sc_cq[:, :, t * 128:(t + 1) * 128],
                                                      in_=tAT[:, 0:256].rearrange("p (c t) -> p c t", c=2)), r=["tATa"], w=[("sc_cq", t)])
                P.dma("sp", lambda: nc.sync.dma_start(out=sc_ckv[:, t * 128:(t + 1) * 128], in_=tAT[:, 256:384]), r=["tATa"], w=[("sc_ckv", t)])
                P.dma("sp", lambda: nc.sync.dma_start(out=sc_kr[:, t * 128:(t + 1) * 128], in_=tAT[0:32, 384:512]), r=["tATb"], w=[("sc_kr", t)])
                yield
                xhk = [("xh", 0), ("xh", 1), ("xh", 2)]
                for grp in range(3):
                    chunks = list(range(grp * 4, min(10, grp * 4 + 4)))

                    def mmX(chunks=chunks):
                        for i, j in enumerate(chunks):
                            for c in range(KC):
                                ins = nc.tensor.matmul(bank[4][:, i * 128:(i + 1) * 128], lhsT=w_inb[:, c, OFF_X + j * 128: OFF_X + (j + 1) * 128],
                                                       rhs=uTc[:, c, :], start=(c == 0), stop=(c == KC - 1))
                        return ins
                    P.op("pe", mmX, r=[kuT, "w_inb"], w=["b4"])
                    nchk = len(chunks)
                    P.op("act", lambda grp=grp, nchk=nchk: nc.scalar.copy(
                        out=xh[:, grp * 4: grp * 4 + nchk, 3:131],
                        in_=bank[4][:, 0:nchk * 128].rearrange("p (c t) -> p c t", c=nchk)), r=["b4", "xh"], w=[("xh", grp)])
                    for j in chunks:
                        P.op("dve", lambda j=j: nc.vector.tensor_scalar(out=cacc[:, j, :], in0=xh[:, j, 0:128], scalar1=cw[:, j, 0:1], scalar2=cb[:, j:j + 1],
                                                                       op0=ALU.mult, op1=ALU.add), r=["xh", ("xh", grp), "cw", "cb"], w=[("cacc", j)])
                        for k in range(1, 4):
                            P.op("dve", lambda j=j, k=k: nc.vector.scalar_tensor_tensor(out=cacc[:, j, :], in0=xh[:, j, k:k + 128], scalar=cw[:, j, k:k + 1],
                                                                                       in1=cacc[:, j, :], op0=ALU.mult, op1=ALU.add),
                                 r=["xh", ("xh", grp), "cw", ("cacc", j)], w=[("cacc", j)])
                    yield
                ck = [("cacc", j) for j in range(10)]
                P.op("act", lambda: nc.scalar.activation(out=xb[:].rearrange("p c t -> p (c t)"), in_=cacc[:].rearrange("p c t -> p (c t)"), func=AF.Silu),
                     r=ck, w=[kxb])
                P.op("pool", lambda: nc.gpsimd.tensor_copy(out=xh[:, :, 0:3], in_=xh[:, :, 128:131]), r=xhk + ["xh"], w=["xh"] + xhk)
                if t == 0:
                    P.op("dve", lambda: nc.vector.memset(xb[:, :, 0:112], 0.0), r=[kxb], w=[kxb])
                yield

            def stageB(t):
                i2, i3 = t % 2, t % 3
                dttc, kdt = dtt[i2], ("dtt", i2)
                atc, kat = at[i2], ("at", i2)
                xb, kxb = xbcs[i3], ("xbcs", i3)
                xtk, kxt = xtok[i2], ("xtok", i2)
                btk, kbt = btok[i2], ("btok", i2)
                xd, kxd = xdt[i2], ("xdt", i2)
                xdd, kxdd = xdtd[i2], ("xdtd", i2)
                ea, kea = eacs[i2], ("eacs", i2)
                MTc = MT[i2]
                cds = cdsel[i2]

                def tr_x():
                    for c in range(8):
                        ins = nc.tensor.transpose(out=bfv(5)[:, c * 128:(c + 1) * 128], in_=xb[:, c, :], identity=ident[:])
                    return nc.tensor.transpose(out=bfv(6)[:, 0:128], in_=xb[:, 8, :], identity=ident[:])
                P.op("pe", tr_x, r=[kxb, "ident"], w=["b5", "b6"])
                P.op("act", lambda: nc.scalar.copy(out=xtk[:], in_=bfv(5)), r=["b5"], w=[kxt])
                P.op("act", lambda: nc.scalar.copy(out=btk[:], in_=bfv(6)[:, 0:128]), r=["b6"], w=[kbt])
                P.op("pe", lambda: nc.tensor.matmul(bank[7][:, 0:16], lhsT=uincl[:], rhs=atc[:], start=True, stop=True), r=["uincl", kat], w=["b7"])
                P.op("dve", lambda: nc.vector.tensor_copy(out=acs[:], in_=bank[7][:, 0:16]), r=["b7"], w=["acs"])
                P.op("act", lambda: nc.scalar.activation(out=ea[:], in_=bank[7][:, 0:16], func=AF.Exp), r=["b7"], w=[kea])

                P.op("pe", lambda: nc.tensor.matmul(bank[7][:, 128:256], lhsT=xb[0:64, 8, :], rhs=xb[0:64, 9, :], start=True, stop=True),
                     r=[kxb], w=["b7"])
                P.op("pe", lambda: nc.tensor.matmul(bank[4][:, 0:128], lhsT=xb[64:128, 8, :], rhs=xb[64:128, 9, :], start=True, stop=True),
                     r=[kxb], w=["b4"])
                P.op("dve", lambda: nc.vector.tensor_tensor(out=cbtm[:, 0, :], in0=bank[7][:, 128:256], in1=mask01[:], op=ALU.mult),
                     r=["b7", "mask01"], w=["cbtm0"])
                P.op("dve", lambda: nc.vector.tensor_tensor(out=cbtm[:, 1, :], in0=bank[4][:, 0:128], in1=mask01[:], op=ALU.mult),
                     r=["b4", "mask01"], w=["cbtm1"])
                P.op("dve", lambda: nc.vector.tensor_tensor(out=xd[:].rearrange("p (h d) -> p h d", h=16),
                                                            in0=xtk[:].rearrange("p (h d) -> p h d", h=16),
                                                            in1=dttc[:].unsqueeze(2).to_broadcast([128, 16, 64]), op=ALU.mult),
                     r=[kxt, kdt], w=[kxd])
                yield
                for g in range(2):
                    P.op("pool", lambda g=g: nc.gpsimd.tensor_tensor(out=Rg[:], in0=uincl[:].unsqueeze(1).to_broadcast([128, 8, 128]),
                                                                    in1=atc[:, g * 8:(g + 1) * 8].unsqueeze(2).to_broadcast([128, 8, 128]), op=ALU.mult),
                         r=["uincl", kat], w=["Rg"])

                    def mmbc(g=g):
                        for q4 in range(2):
                            nc.tensor.matmul(bank[5 + q4][:], lhsT=ones_f[:], rhs=Rg[:, q4 * 4:(q4 + 1) * 4, :].rearrange("p e l -> p (e l)"),
                                             start=True, stop=False)
                            ins = nc.tensor.matmul(bank[5 + q4][:], lhsT=ident[:], rhs=negm[:, q4 * 4:(q4 + 1) * 4, :].rearrange("p e l -> p (e l)"),
                                                   start=False, stop=True)
                        return ins
                    P.op("pe", mmbc, r=["ones_f", "Rg", "ident", "negm"], w=["b5", "b6"])
                    for q4 in range(2):
                        P.op("dve", lambda g=g, q4=q4: nc.vector.tensor_tensor(
                            out=Dg[:, q4 * 4:(q4 + 1) * 4, :], in0=bank[5 + q4][:].rearrange("p (e l) -> p e l", e=4),
                            in1=acs[:, g * 8 + q4 * 4: g * 8 + q4 * 4 + 4].unsqueeze(2).to_broadcast([128, 4, 128]), op=ALU.subtract),
                            r=["b%d" % (5 + q4), "acs"], w=[("Dg", q4)])
                    P.op("act", lambda: nc.scalar.activation(out=LT[:], in_=Dg[:], func=AF.Exp),
                         r=[("Dg", 0), ("Dg", 1)], w=["LT"])
                    P.op("dve", lambda g=g: nc.vector.tensor_copy(out=dec[:, g * 8:(g + 1) * 8], in_=LT[:, :, 127]), r=["LT"], w=[("dec", g)])
                    P.op("act", lambda g=g: nc.scalar.activation(
                        out=cds[g * 64:(g + 1) * 64, 0:4],
                        in_=bank[5][g * 64:(g + 1) * 64, :].rearrange("p (e l) -> p e l", e=4)[:, :, 127], func=AF.Exp),
                        r=["b5"], w=[("cdselA", i2, g)])
                    P.op("act", lambda g=g: nc.scalar.activation(
                        out=cds[g * 64:(g + 1) * 64, 4:8],
                        in_=bank[6][g * 64:(g + 1) * 64, :].rearrange("p (e l) -> p e l", e=4)[:, :, 127], func=AF.Exp),
                        r=["b6", ("cdselA", i2, g)], w=[("cdsel", i2, g)])
                    P.op("pool", lambda g=g: nc.gpsimd.tensor_tensor(
                        out=MTc[:, g * 8:(g + 1) * 8, :], in0=LT[:],
                        in1=cbtm[:, g, :].unsqueeze(1).to_broadcast([128, 8, 128]), op=ALU.mult), r=["LT", "cbtm0", "cbtm1"], w=[("MT", i2, g)])
                    yield
                P.op("dve", lambda: nc.vector.tensor_tensor(out=xdd[:].rearrange("p (h d) -> p h d", h=16),
                                                            in0=xd[:].rearrange("p (h d) -> p h d", h=16),
                                                            in1=dec[:].unsqueeze(2).to_broadcast([128, 16, 64]), op=ALU.mult),
                     r=[kxd, ("dec", 0), ("dec", 1)], w=[kxdd])
                yield

            def stageC(t):
                i2, i3 = t % 2, t % 3
                zsc, kzs = zs[i3], ("zs", i3)
                xb, kxb = xbcs[i3], ("xbcs", i3)
                xtk, kxt = xtok[i2], ("xtok", i2)
                btk, kbt = btok[i2], ("btok", i2)
                xd, kxd = xdt[i2], ("xdt", i2)
                xdd, kxdd = xdtd[i2], ("xdtd", i2)
                ea, kea = eacs[i2], ("eacs", i2)
                MTc = MT[i2]
                cds = cdsel[i2]
                P.op("pool", lambda: nc.gpsimd.tensor_copy(out=prevb[:], in_=Sst[:]), r=["Sst"], w=["prevb"])
                for g in range(2):
                    P.op("pe", lambda g=g: nc.tensor.matmul(bank[4][g * 64:(g + 1) * 64, :], lhsT=btk[:, g * 64:(g + 1) * 64], rhs=xdd[:, g * 512:(g + 1) * 512],
                                                            start=True, stop=True), r=[kbt, kxdd], w=["b4", "b4s%d" % g])
                P.op("dve", lambda: nc.vector.tensor_tensor(out=Sst[:].rearrange("p (e d) -> p e d", e=8), in0=Sst[:].rearrange("p (e d) -> p e d", e=8),
                                                            in1=cds[:].unsqueeze(2).to_broadcast([128, 8, 64]), op=ALU.mult),
                     r=["Sst", ("cdsel", i2, 0), ("cdsel", i2, 1), "prevb"], w=["Sst"])
                P.op("dve", lambda: nc.vector.tensor_tensor(out=Sst[:], in0=Sst[:], in1=bank[4][:], op=ALU.add), r=["Sst", "b4", "b4s0", "b4s1"], w=["Sst"])
                yield
                if t > 0:
                    def mmY():
                        for h in range(16):
                            ins = nc.tensor.matmul(bank[2 + h // 8][:, (h % 8) * 64:(h % 8 + 1) * 64], lhsT=MTc[:, h, :], rhs=xd[:, h * 64:(h + 1) * 64],
                                                   start=True, stop=True)
                        return ins
                    P.op("pe", mmY, r=[("MT", i2, 0), ("MT", i2, 1), kxd], w=["b2", "b3"])

                    for g in range(2):
                        P.op("pe", lambda g=g: nc.tensor.matmul(bank[5 + g][:], lhsT=xb[g * 64:(g + 1) * 64, 9, :], rhs=prevb[g * 64:(g + 1) * 64, :],
                                                                start=True, stop=True), r=[kxb, "prevb"], w=["b%d" % (5 + g)])
                    for g in range(2):
                        P.op("act", lambda g=g: nc.scalar.copy(out=yd[:, g * 512:(g + 1) * 512], in_=bank[2 + g][:]), r=["b%d" % (2 + g)], w=[("yd", g)])
                        P.op("dve", lambda g=g: nc.vector.tensor_tensor(
                            out=yy[:, g * 512:(g + 1) * 512].rearrange("p (e d) -> p e d", e=8),
                            in0=bank[5 + g][:].rearrange("p (e d) -> p e d", e=8),
                            in1=ea[:, g * 8:(g + 1) * 8].unsqueeze(2).to_broadcast([128, 8, 64]), op=ALU.mult),
                            r=["b%d" % (5 + g), kea], w=[("yy", g)])
                    yield
                    for g in range(2):
                        P.op("pool", lambda g=g: nc.gpsimd.tensor_tensor(out=yy[:, g * 512:(g + 1) * 512], in0=yy[:, g * 512:(g + 1) * 512],
                                                                        in1=yd[:, g * 512:(g + 1) * 512], op=ALU.add),
                             r=[("yy", g), ("yd", g)], w=[("yy", g)])
                        P.op("dve", lambda g=g: nc.vector.tensor_tensor(
                            out=yd[:, g * 512:(g + 1) * 512].rearrange("p (e d) -> p e d", e=8),
                            in0=xtk[:, g * 512:(g + 1) * 512].rearrange("p (e d) -> p e d", e=8),
                            in1=dsk_bc[:, g * 8:(g + 1) * 8].unsqueeze(2).to_broadcast([128, 8, 64]), op=ALU.mult),
                            r=[kxt, "dsk", ("yy", g)], w=[("yd", g)])
                        P.op("pool", lambda g=g: nc.gpsimd.tensor_tensor(out=yy[:, g * 512:(g + 1) * 512], in0=yy[:, g * 512:(g + 1) * 512],
                                                                        in1=yd[:, g * 512:(g + 1) * 512], op=ALU.add),
                             r=[("yy", g), ("yd", g)], w=[("yy", g)])
                        P.op("dve", lambda g=g: nc.vector.tensor_tensor(out=yy[:, g * 512:(g + 1) * 512], in0=yy[:, g * 512:(g + 1) * 512],
                                                                       in1=zsc[:, g * 512:(g + 1) * 512], op=ALU.mult),
                             r=[("yy", g), kzs], w=[("yy", g)])
                        P.op("act", lambda g=g: nc.scalar.activation(out=junk[:, g * 512:(g + 1) * 512], in_=yy[:, g * 512:(g + 1) * 512],
                                                                     func=AF.Square, accum_out=ssg[:, g:g + 1]), r=[("yy", g)], w=["junk", ("ssg", g)])
                        rstd_ops(ssg[:, g:g + 1], ssg[:, 2 + g:3 + g], 512, ("ssg", g), ("rsg", g))
                        P.op("dve", lambda g=g: nc.vector.scalar_tensor_tensor(out=osm[:, g * 512:(g + 1) * 512], in0=yy[:, g * 512:(g + 1) * 512],
                                                                              scalar=ssg[:, 2 + g:3 + g], in1=gss[:, g * 512:(g + 1) * 512],
                                                                              op0=ALU.mult, op1=ALU.mult), r=[("yy", g), ("rsg", g), "gss"], w=[("osm", g)])
                        yield
                    def tr_o():
                        for c in range(KC):
                            ins = nc.tensor.transpose(out=bfv(0)[:, c * 128:(c + 1) * 128], in_=osm[:, c * 128:(c + 1) * 128], identity=ident[:])
                        return ins
                    P.op("pe", tr_o, r=[("osm", 0), ("osm", 1), "ident"], w=["b0"])
                    P.op("act", lambda: nc.scalar.copy(out=osmT[:].rearrange("p c t -> p (c t)"), in_=bfv(0)), r=["b0"], w=["osmT"])
                    P.dma("sp", lambda: nc.sync.dma_start(out=sc_osT[t * 128:(t + 1) * 128, :], in_=osmT[:].rearrange("p c t -> p (c t)")),
                          r=["osmT"], w=[("sc_osT", t)])
                yield

            for n in range(NT + 2):
                gens = []
                if n - 2 >= 0:
                    gens.append(stageC(n - 2))
                if 0 <= n - 1 < NT:
                    gens.append(stageB(n - 1))
                if n < NT:
                    gens.append(stageA(n))
                while gens:
                    for g_ in list(gens):
                        try:
                            next(g_)
                        except StopIteration:
                            gens.remove(g_)
            P.flush()
            if STOP <= 1:
                return nc

        def load_flat(dst2, src2, ncols, stg, key):
            P.dma("pool", lambda: nc.gpsimd.dma_start(out=dst2[:, 0:ncols], in_=src2[:, 0:ncols]), w=[key])

        groups = [[0]] + [list(range(g0, min(g0 + 4, NT))) for g0 in range(1, NT, 4)]

        with ExitStack() as e23:
            o_attnT = P.sb(e23, [128, KC, TOK], BF16)
            with ExitStack() as e2:
                s2 = lambda shape, dt: P.sb(e2, shape, dt)
                stg = [s2([128, 512], F32) for _ in range(2)]
                wqa = s2([128, 2, 16, 128], BF16)
                wqs = s2([128, 2, 16, 32], BF16)
                wkb = s2([128, 16, 64], BF16)
                wvb = s2([128, 16, 64], BF16)
                load_flat(wqa[:].rearrange("p c h m -> p (c h m)"), w_uqa.rearrange("p c h m -> p (c h m)"), 4096, stg, "wqa")
                load_flat(wqs[:].rearrange("p c h m -> p (c h m)"), w_uqs.rearrange("p c h m -> p (c h m)"), 1024, stg, "wqs")
                load_flat(wkb[:].rearrange("p h m -> p (h m)"), w_uk.rearrange("p h m -> p (h m)"), 1024, stg, "wkb")
                load_flat(wvb[:].rearrange("p h m -> p (h m)"), w_uv.rearrange("p h m -> p (h m)"), 1024, stg, "wvb")
                c2b = s2([32, TOK], BF16)
                s2b = s2([32, TOK], BF16)
                for o in range(0, TOK, 512):
                    wd = min(512, TOK - o)
                    for dst, src, k in ((c2b, c2T, "c2b"), (s2b, s2T, "s2b")):
                        i = ldc[0] % 2
                        ldc[0] += 1
                        st = stg[i]
                        P.dma("sp", lambda o=o, wd=wd, st=st, src=src: nc.sync.dma_start(out=st[0:32, 0:wd], in_=src[:, o:o + wd]), w=[("stg", i)])
                        P.op("dve", lambda o=o, wd=wd, st=st, dst=dst: nc.vector.tensor_copy(out=dst[:, o:o + wd], in_=st[0:32, 0:wd]),
                             r=[("stg", i)], w=[k])
                cqnT = s2([128, 2, TOK], BF16)
                ckvnT = s2([128, TOK], BF16)
                P.dma("sp", lambda: nc.sync.dma_start(out=cqnT[:, 0, :], in_=sc_cq[:, 0, :]), w=["cqnT0"])
                P.dma("sp", lambda: nc.sync.dma_start(out=cqnT[:, 1, :], in_=sc_cq[:, 1, :]), w=["cqnT1"])
                P.dma("sp", lambda: nc.sync.dma_start(out=ckvnT[:], in_=sc_ckv), w=["ckvnT"])
                KT = [s2([128, TOK], BF16) for _ in range(2)]
                QT = [s2([128, TOK], BF16) for _ in range(2)]
                Vaug = [s2([128, NT, 128], BF16) for _ in range(2)]
                PT = [s2([128, 512], BF16) for _ in range(4)]
                qr1 = s2([32, 512], F32)
                qr2 = s2([32, 512], F32)
                rrow = s2([128, 512], F32)
                bcs = s2([128, 512], F32)
                for par in range(2):
                    oc = 64 if par == 0 else 0
                    P.op("pool", lambda par=par: nc.gpsimd.memset(KT[par][:], 0.0), w=[("KT", par)])
                    P.op("pool", lambda par=par: nc.gpsimd.memset(QT[par][:], 0.0), w=[("QT", par)])
                    P.dma("sp", lambda par=par: nc.sync.dma_start(out=KT[par][0:32, :], in_=sc_kr), r=[("KT", par)], w=[("KT", par)])
                    P.op("pool", lambda par=par: nc.gpsimd.memset(Vaug[par][:], 0.0), w=[("Vaug", par)])
                    P.op("pool", lambda par=par, oc=oc: nc.gpsimd.memset(Vaug[par][:, :, oc:oc + 1], 1.0), r=[("Vaug", par)], w=[("Vaug", par)])
                    P.op("pool", lambda par=par, oc=oc: nc.gpsimd.memset(Vaug[par][0:112, 0, oc:oc + 1], 0.0), r=[("Vaug", par)], w=[("Vaug", par)])
                P.op("dve", lambda: nc.vector.memset(rrow[:], 1.0), w=["rrow"])
                NG = len(groups)

                def proj_items(h):
                    par = h % 2
                    voff = 0 if par == 0 else 64
                    items = []
                    for gi, grp in enumerate(groups):
                        c0, c1 = grp[0] * 128, (grp[-1] + 1) * 128
                        n = c1 - c0

                        def itemK(h=h, par=par, gi=gi, c0=c0, c1=c1, n=n):
                            P.op("pe", lambda: nc.tensor.matmul(bank[6][64:128, 0:n], lhsT=wkb[:, h, :], rhs=ckvnT[:, c0:c1], start=True, stop=True),
                                 r=["wkb", "ckvnT"], w=["b6"])
                            P.op("dve", lambda: nc.vector.tensor_copy(out=KT[par][64:128, c0:c1], in_=bank[6][64:128, 0:n]),
                                 r=["b6", ("KT", par)], w=[("KTg", par, gi)])

                        items.append(itemK)

                        def itemQ(h=h, par=par, gi=gi, c0=c0, c1=c1, n=n):
                            def mmQs():
                                for c in range(2):
                                    ins = nc.tensor.matmul(bank[7][0:32, 0:n], lhsT=wqs[:, c, h, :], rhs=cqnT[:, c, c0:c1], start=(c == 0), stop=(c == 1))
                                return ins

                            def mmQa():
                                for c in range(2):
                                    ins = nc.tensor.matmul(bank[6][:, 0:n], lhsT=wqa[:, c, h, :], rhs=cqnT[:, c, c0:c1], start=(c == 0), stop=(c == 1))
                                return ins
                            P.op("pe", mmQs, r=["wqs", "cqnT0", "cqnT1"], w=["b7"])
                            P.op("pe", mmQa, r=["wqa", "cqnT0", "cqnT1"], w=["b6"])
                            P.op("dve", lambda: nc.vector.tensor_copy(out=QT[par][64:128, c0:c1], in_=bank[6][64:128, 0:n]),
                                 r=["b6", ("QT", par)], w=[("QTa", par, gi)])
                            P.op("dve", lambda: nc.vector.tensor_tensor(out=qr1[:, 0:n], in0=bank[6][0:32, 0:n], in1=c2b[:, c0:c1], op=ALU.mult),
                                 r=["b6", "c2b"], w=["qr1"])
                            P.op("dve", lambda: nc.vector.tensor_tensor(out=qr2[:, 0:n], in0=bank[7][0:32, 0:n], in1=s2b[:, c0:c1], op=ALU.mult),
                                 r=["b7", "s2b"], w=["qr2"])
                            P.op("pool", lambda: nc.gpsimd.tensor_tensor(out=QT[par][0:32, c0:c1], in0=qr1[:, 0:n], in1=qr2[:, 0:n], op=ALU.add),
                                 r=["qr1", "qr2", ("QT", par)], w=[("QTb", par, gi)])
                        items.append(itemQ)
                    for t0 in range(0, NT, 8):
                        tn = min(8, NT - t0)

                        def itemV(h=h, par=par, voff=voff, t0=t0, tn=tn):
                            def mmV():
                                for j in range(tn):
                                    ins = nc.tensor.matmul(bank[6][:, j * 64:(j + 1) * 64], lhsT=ckvnT[:, (t0 + j) * 128:(t0 + j + 1) * 128], rhs=wvb[:, h, :],
                                                           start=True, stop=True)
                                return ins
                            P.op("pe", mmV, r=["wvb", "ckvnT"], w=["b6"])
                            P.op("dve", lambda: nc.vector.tensor_copy(
                                out=Vaug[par][:, t0:t0 + tn, voff:voff + 64], in_=bank[6][:, 0:tn * 64].rearrange("p (t d) -> p t d", t=tn)),
                                r=["b6", ("Vaug", par)], w=[("Vaug", par)])
                        items.append(itemV)
                    return items

                def kq_keys(par):
                    return [("KT", par), ("QT", par)] + [("KTg", par, gi) for gi in range(NG)] + [("QTa", par, gi) for gi in range(NG)] + \
                           [("QTb", par, gi) for gi in range(NG)]

                its = []
                octr = 0
                for h in range(16):
                    for gi, grp in enumerate(groups):
                        ob = 3 + (octr % 2)
                        octr += 1
                        for kt in range(grp[-1] + 1):
                            its.append((h, gi, kt, ob))
                NI = len(its)
                LOOK = 3
                SBK = [0, 1, 2, 5]
                pend = {h: proj_items(h) for h in range(16)}
                for it_ in pend[0]:
                    it_()
                pend[0] = []
                deferred = []

                def emit_S(idx):
                    h, gi, kt, ob = its[idx]
                    par = h % 2
                    if pend[h]:
                        for it_ in pend[h]:
                            it_()
                        pend[h] = []
                    grp = groups[gi]
                    q0, q1 = grp[0] * 128, (grp[-1] + 1) * 128
                    j = max(0, kt - grp[0])
                    qs = q0 + j * 128
                    n = q1 - qs
                    sbk = idx % 4
                    P.op("pe", lambda: nc.tensor.matmul(bank[SBK[sbk]][:, 0:n], lhsT=KT[par][:, kt * 128:(kt + 1) * 128], rhs=QT[par][:, qs:q1],
                                                        start=True, stop=True), r=kq_keys(par), w=[("S", sbk)])

                def emit_rest(idx):
                    h, gi, kt, ob = its[idx]
                    par = h % 2
                    grp = groups[gi]
                    q0, q1 = grp[0] * 128, (grp[-1] + 1) * 128
                    nq = q1 - q0
                    j = max(0, kt - grp[0])
                    n = q1 - (q0 + j * 128)
                    sbk = idx % 4
                    M = 65 if par == 0 else 128
                    last_kt = grp[-1]
                    P.op("act", lambda: nc.scalar.activation(out=PT[sbk][:, 0:n], in_=bank[SBK[sbk]][:, 0:n], func=AF.Exp),
                         r=[("S", sbk)], w=[("PT", sbk)])
                    if kt >= grp[0]:
                        P.op("dve", lambda: nc.vector.tensor_tensor(out=PT[sbk][:, 0:128], in0=PT[sbk][:, 0:128], in1=mask01[:], op=ALU.mult),
                             r=[("PT", sbk), "mask01"], w=[("PT", sbk)])
                    assert all(d[2] != ob for d in deferred), "pending normalisation on the accumulator bank"
                    P.op("pe", lambda: nc.tensor.matmul(bank[ob][0:M, j * 128:nq], lhsT=Vaug[par][:, kt, 0:M], rhs=PT[sbk][:, 0:n],
                                                        start=(kt == 0), stop=(kt == last_kt)),
                         r=[("PT", sbk), ("Vaug", par)], w=["b%d" % ob])
                    if kt == last_kt:
                        dr = 64 if par == 0 else 0
                        r0, r1 = (0, 64) if par == 0 else (64, 128)
                        P.op("dve", lambda: nc.vector.tensor_scalar(out=rrow[dr:dr + 1, 0:nq], in0=bank[ob][dr:dr + 1, 0:nq],
                                                                    scalar1=1e-30, scalar2=None, op0=ALU.max), r=["b%d" % ob], w=["rrow"])
                        P.op("dve", lambda: nc.vector.reciprocal(out=rrow[dr:dr + 1, 0:nq], in_=rrow[dr:dr + 1, 0:nq]), r=["rrow"], w=["rrow"])

                        def fin():
                            P.op("pe", lambda: nc.tensor.matmul(bank[7][0:r1, 0:nq], lhsT=ones_f[dr:dr + 1, 0:r1], rhs=rrow[dr:dr + 1, 0:nq],
                                                                start=True, stop=True), r=["rrow", "ones_f"], w=["b7"])
                            P.op("dve", lambda: nc.vector.tensor_copy(out=bcs[r0:r1, 0:nq], in_=bank[7][r0:r1, 0:nq]), r=["b7"], w=["bcs"])
                            P.op("dve", lambda: nc.vector.tensor_tensor(out=o_attnT[r0:r1, h // 2, q0:q1], in0=bank[ob][r0:r1, 0:nq], in1=bcs[r0:r1, 0:nq],
                                                                        op=ALU.mult), r=["b%d" % ob, "bcs"], w=[("oT", h, gi)])
                        deferred.append((idx + 1, fin, ob))

                cast_items = []
                CR = 256
                for r0_ in range(0, NEXP * 128, CR):
                    cast_items.append(lambda r0_=r0_: P.dma("pool", lambda: nc.gpsimd.dma_start(out=sc_wc[r0_:r0_ + CR, 0:WGU_C], in_=w_gu[r0_:r0_ + CR, :]),
                                                            w=[("sc_wgu", r0_)]))
                    cast_items.append(lambda r0_=r0_: P.dma("pool", lambda: nc.gpsimd.dma_start(out=sc_wc[r0_:r0_ + CR, WGU_C:WC], in_=w_dn[r0_:r0_ + CR, :]),
                                                            w=[("sc_wdn", r0_)]))
                cast_every = max(1, (NI - 8) // len(cast_items))
                head_start = {}
                for idx, (h, gi, kt, ob) in enumerate(its):
                    head_start.setdefault(h, idx)
                for idx in range(NI + LOOK):
                    if idx < NI:
                        emit_S(idx)
                    jdx = idx - LOOK
                    if jdx >= 0:
                        emit_rest(jdx)
                        while deferred and deferred[0][0] <= jdx:
                            deferred.pop(0)[1]()
                        h = its[jdx][0]
                        loc = jdx - head_start[h]
                        if h + 1 < 16 and loc >= 4 and (loc - 4) % 6 == 0 and pend[h + 1]:
                            pend[h + 1].pop(0)()
                        if cast_items and jdx % cast_every == 0:
                            cast_items.pop(0)()
                for d in deferred:
                    d[1]()
                for ci in cast_items:
                    ci()
                P.flush()
                if STOP <= 2:
                    return nc

            with ExitStack() as e3:
                s3 = lambda shape, dt: P.sb(e3, shape, dt)
                wg = s3([128, KC, 2048], BF16)
                wba = s3([128, KC, D], BF16)
                wbs = s3([128, KC, D], BF16)
                wo = s3([128, KC, D], BF16)
                wrb = s3([128, KC, 36], BF16)
                with ExitStack() as eL:
                    stg = [P.sb(eL, [128, 512], F32) for _ in range(2)]
                    load_w(wg, w_in, 2048, OFF_GA, stg, "wg")
                    load_w(wba, w_ba, D, 0, stg, "wba")
                    load_w(wbs, w_bs, D, 0, stg, "wbs")
                    load_w(wo, w_out, D, 0, stg, "wo")
                    load_w(wrb, w_r, 36, 0, stg, "wrb")
                    P.flush()
                gmix = s3([128, D], F32)
                gffn = s3([128, D], F32)
                brt = s3([128, 36], F32)
                ustr = s3([128, 128], BF16)
                for dst, src, k in ((gmix, g_mix, "gmix"), (gffn, g_ffn, "gffn"), (brt, b_r, "brt")):
                    P.dma("sp", lambda dst=dst, src=src: nc.sync.dma_start(out=dst[:], in_=src), w=[k])
                P.op("pool", lambda: nc.gpsimd.affine_select(out=ustr[:], in_=ones_b[:], pattern=[[1, 128]], compare_op=ALU.is_gt, fill=0.0,
                                                              base=0, channel_multiplier=-1), r=["ones_b"], w=["ustr"])
                P.op("dve", lambda: nc.vector.memset(Macc[:], 0.0), w=["Macc"])
                xt = [s3([128, D], F32)] * 2
                u = s3([128, D], BF16)
                uT = s3([128, KC, 128], BF16)
                osT = s3([128, KC, 128], BF16)
                st8 = s3([128, 16], F32)
                sga = s3([128, D], F32)
                sgs = s3([128, D], F32)
                mrb = s3([128, D], BF16)
                mT = s3([128, KC, 128], BF16)
                h1 = s3([128, D], F32)
                vh = s3([128, D], BF16)
                vT = s3([128, KC, 128], BF16)
                lg = s3([128, 36], F32)
                sm = s3([128, 16], F32)
                pen = s3([128, 4], F32)
                goh = s3([128, 4], F32)
                elm = s3([128, 32], F32)
                elm2 = s3([128, 32], F32)
                Mt = s3([128, 32], F32)
                Mtb = s3([128, 32], BF16)
                Maccb = s3([128, 32], BF16)

                for t in range(1, NT):
                    r_ = t - 1
                    xcur = xt[t % 2]
                    kx = "xt3"
                    P.dma("sp", lambda t=t, xcur=xcur: nc.sync.dma_start(out=xcur[:], in_=x[(t - 1) * 128:t * 128, :]), w=[kx])
                    P.dma("sp", lambda t=t: nc.sync.dma_start(out=osT[:].rearrange("p c t -> p (c t)"), in_=sc_osT[t * 128:(t + 1) * 128, :]), w=["osT"])
                    P.op("act", lambda xcur=xcur: nc.scalar.activation(out=mrb[:], in_=xcur[:], func=AF.Square, accum_out=st8[:, 0:1]),
                         r=[kx], w=[("mrb", 0), ("mrb", 1), "ssx"])
                    rstd_ops(st8[:, 0:1], st8[:, 1:2], D, "ssx", "rsx")
                    P.op("dve", lambda xcur=xcur: nc.vector.scalar_tensor_tensor(out=u[:], in0=xcur[:], scalar=st8[:, 1:2], in1=gmix[:],
                                                                                op0=ALU.mult, op1=ALU.mult), r=[kx, "rsx", "gmix"], w=["u"])

                    def tr8(src, bnk):
                        def f():
                            for c in range(KC):
                                ins = nc.tensor.transpose(out=bfv(bnk)[:, c * 128:(c + 1) * 128], in_=src[:, c * 128:(c + 1) * 128], identity=ident[:])
                            return ins
                        return f
                    P.op("pe", tr8(u, 0), r=["u", "ident"], w=["b0"])
                    P.op("act", lambda: nc.scalar.copy(out=uT[:].rearrange("p c t -> p (c t)"), in_=bfv(0)), r=["b0"], w=["uT"])

                    def mmG():
                        for q4 in range(4):
                            for c in range(KC):
                                ins = nc.tensor.matmul(bank[1 + q4][:], lhsT=uT[:, c, :], rhs=wg[:, c, q4 * 512:(q4 + 1) * 512], start=(c == 0), stop=(c == KC - 1))
                        return ins
                    P.op("pe", mmG, r=["uT", "wg"], w=["b1", "b2", "b3", "b4"])
                    for q4 in range(4):
                        dst = sga if q4 < 2 else sgs
                        P.op("act", lambda q4=q4, dst=dst: nc.scalar.activation(out=dst[:, (q4 % 2) * 512:(q4 % 2 + 1) * 512], in_=bank[1 + q4][:], func=AF.Sigmoid),
                             r=["b%d" % (1 + q4)], w=[("sg", q4)])

                    def mmBr(t=t):
                        for hf in range(2):
                            for c in range(KC):
                                nc.tensor.matmul(bank[5 + hf][:], lhsT=o_attnT[:, c, t * 128:(t + 1) * 128], rhs=wba[:, c, hf * 512:(hf + 1) * 512],
                                                 start=(c == 0), stop=(c == KC - 1))
                        for hf in range(2):
                            for c in range(KC):
                                ins = nc.tensor.matmul(bank[1 + hf][:], lhsT=osT[:, c, :], rhs=wbs[:, c, hf * 512:(hf + 1) * 512],
                                                       start=(c == 0), stop=(c == KC - 1))
                        return ins
                    P.op("pe", mmBr, r=["wba", "wbs", "osT"], w=["b5", "b6", "b1", "b2"])
                    for hf in range(2):
                        sl = slice(hf * 512, (hf + 1) * 512)
                        P.op("dve", lambda hf=hf, sl=sl: nc.vector.tensor_tensor(out=sga[:, sl], in0=bank[5 + hf][:], in1=sga[:, sl], op=ALU.mult),
                             r=["b%d" % (5 + hf), ("sg", hf)], w=[("sg", hf)])
                        P.op("dve", lambda hf=hf, sl=sl: nc.vector.tensor_tensor(out=sgs[:, sl], in0=bank[1 + hf][:], in1=sgs[:, sl], op=ALU.mult),
                             r=["b%d" % (1 + hf), ("sg", 2 + hf)], w=[("sg", 2 + hf)])
                        P.op("pool", lambda hf=hf, sl=sl: nc.gpsimd.tensor_tensor(out=mrb[:, sl], in0=sga[:, sl], in1=sgs[:, sl], op=ALU.add),
                             r=[("sg", hf), ("sg", 2 + hf)], w=[("mrb", hf)])
                    P.op("pe", tr8(mrb, 0), r=[("mrb", 0), ("mrb", 1), "ident"], w=["b0"])
                    P.op("act", lambda: nc.scalar.copy(out=mT[:].rearrange("p c t -> p (c t)"), in_=bfv(0)), r=["b0"], w=["mT"])

                    def mmO():
                        for hf in range(2):
                            for c in range(KC):
                                ins = nc.tensor.matmul(bank[3 + hf][:], lhsT=mT[:, c, :], rhs=wo[:, c, hf * 512:(hf + 1) * 512], start=(c == 0), stop=(c == KC - 1))
                        return ins
                    P.op("pe", mmO, r=["mT", "wo"], w=["b3", "b4"])
                    for hf in range(2):
                        sl = slice(hf * 512, (hf + 1) * 512)
                        P.op("dve", lambda hf=hf, sl=sl, xcur=xcur: nc.vector.tensor_tensor(out=h1[:, sl], in0=bank[3 + hf][:], in1=xcur[:, sl], op=ALU.add),
                             r=["b%d" % (3 + hf), kx], w=[("h1", hf)])
                    hk = [("h1", 0), ("h1", 1)]
                    P.dma("pool", lambda t=t: nc.gpsimd.dma_start(out=sc_h1[t * 128:(t + 1) * 128, :], in_=h1[:]), r=hk, w=[("sc_h1", t)])
                    P.op("act", lambda: nc.scalar.activation(out=u[:], in_=h1[:], func=AF.Square, accum_out=st8[:, 2:3]), r=hk, w=["u", "ssh"])
                    rstd_ops(st8[:, 2:3], st8[:, 3:4], D, "ssh", "rsh")
                    P.op("dve", lambda: nc.vector.scalar_tensor_tensor(out=vh[:], in0=h1[:], scalar=st8[:, 3:4], in1=gffn[:], op0=ALU.mult, op1=ALU.mult),
                         r=hk + ["rsh", "gffn"], w=["vh"])
                    P.dma("pool", lambda t=t: nc.gpsimd.dma_start(out=sc_vh[t * 128:(t + 1) * 128, :], in_=vh[:]), r=["vh"], w=[("sc_vh", t)])
                    P.op("pe", tr8(vh, 0), r=["vh", "ident"], w=["b0"])
                    P.op("act", lambda: nc.scalar.copy(out=vT[:].rearrange("p c t -> p (c t)"), in_=bfv(0)), r=["b0"], w=["vT"])

                    def mmR():
                        for c in range(KC):
                            ins = nc.tensor.matmul(bank[7][:, 0:36], lhsT=vT[:, c, :], rhs=wrb[:, c, :], start=(c == 0), stop=(c == KC - 1))
                        return ins
                    P.op("pe", mmR, r=["vT", "wrb"], w=["b7"])
                    P.op("dve", lambda: nc.vector.tensor_tensor(out=lg[:], in0=bank[7][:, 0:36], in1=brt[:], op=ALU.add), r=["b7", "brt"], w=["lg"])
                    V = nc.vector
                    P.op("dve", lambda: V.reduce_max(out=sm[:, 0:1], in_=lg[:, 0:4], axis=AX.X), r=["lg"], w=["gmax"])
                    P.op("dve", lambda: V.tensor_scalar(out=goh[:], in0=lg[:, 0:4], scalar1=sm[:, 0:1], scalar2=None, op0=ALU.is_equal), r=["lg", "gmax"], w=["goh"])
                    P.op("dve", lambda: V.tensor_scalar(out=sm[:, 1:2], in0=sm[:, 0:1], scalar1=-1.0, scalar2=None, op0=ALU.mult), r=["gmax"], w=["ngmax"])
                    P.op("act", lambda: nc.scalar.activation(out=pen[:], in_=lg[:, 0:4], func=AF.Exp, bias=sm[:, 1:2], accum_out=sm[:, 2:3]),
                         r=["lg", "ngmax", "goh"], w=["pen", "gsum"])
                    P.op("dve", lambda: V.reciprocal(out=sm[:, 3:4], in_=sm[:, 2:3]), r=["gsum"], w=["pg"])
                    P.op("dve", lambda: V.tensor_scalar(out=pen[:], in0=goh[:], scalar1=-1.0, scalar2=1e9, op0=ALU.add, op1=ALU.mult), r=["goh", "pen"], w=["pen2"])
                    P.op("dve", lambda: V.tensor_tensor(out=elm[:].rearrange("p (g e) -> p g e", g=4), in0=lg[:, 4:36].rearrange("p (g e) -> p g e", g=4),
                                                        in1=pen[:].unsqueeze(2).to_broadcast([128, 4, 8]), op=ALU.add), r=["lg", "pen2"], w=["elm"])
                    P.op("dve", lambda: V.reduce_max(out=sm[:, 4:5], in_=elm[:], axis=AX.X), r=["elm"], w=["m1"])
                    P.op("dve", lambda r_=r_: V.tensor_scalar(out=oh1[:, r_, :], in0=elm[:], scalar1=sm[:, 4:5], scalar2=None, op0=ALU.is_equal),
                         r=["elm", "m1"], w=[("oh1", r_)])
                    P.op("dve", lambda r_=r_: V.scalar_tensor_tensor(out=elm2[:], in0=oh1[:, r_, :], scalar=-1e9, in1=elm[:], op0=ALU.mult, op1=ALU.add),
                         r=[("oh1", r_), "elm"], w=["elm2"])
                    P.op("dve", lambda: V.reduce_max(out=sm[:, 5:6], in_=elm2[:], axis=AX.X), r=["elm2"], w=["m2"])
                    P.op("dve", lambda r_=r_: V.tensor_scalar(out=oh2[:, r_, :], in0=elm2[:], scalar1=sm[:, 5:6], scalar2=None, op0=ALU.is_equal),
                         r=["elm2", "m2"], w=[("oh2", r_)])
                    P.op("dve", lambda: V.tensor_tensor(out=sm[:, 6:7], in0=sm[:, 5:6], in1=sm[:, 4:5], op=ALU.subtract), r=["m1", "m2"], w=["dd"])
                    P.op("act", lambda: nc.scalar.activation(out=sm[:, 7:8], in_=sm[:, 6:7], func=AF.Exp), r=["dd"], w=["e2"])
                    P.op("dve", lambda: V.tensor_scalar(out=sm[:, 8:9], in0=sm[:, 7:8], scalar1=1.0, scalar2=None, op0=ALU.add), r=["e2"], w=["t1"])
                    P.op("dve", lambda: V.reciprocal(out=sm[:, 9:10], in_=sm[:, 8:9]), r=["t1"], w=["rt"])
                    P.op("dve", lambda r_=r_: V.tensor_tensor(out=wts[:, r_, 0:1], in0=sm[:, 9:10], in1=sm[:, 3:4], op=ALU.mult), r=["rt", "pg"], w=[("w1", r_)])
                    P.op("dve", lambda r_=r_: V.tensor_tensor(out=wts[:, r_, 1:2], in0=sm[:, 3:4], in1=wts[:, r_, 0:1], op=ALU.subtract),
                         r=["pg", ("w1", r_)], w=[("w2", r_)])
                    P.op("dve", lambda r_=r_: V.tensor_tensor(out=Mt[:], in0=oh1[:, r_, :], in1=oh2[:, r_, :], op=ALU.add), r=[("oh1", r_), ("oh2", r_)], w=["Mt"])
                    P.op("dve", lambda: V.tensor_copy(out=Mtb[:], in_=Mt[:]), r=["Mt"], w=["Mtb"])
                    P.op("dve", lambda: V.tensor_copy(out=Maccb[:], in_=Macc[:]), r=["Macc"], w=["Maccb"])

                    def mmRk():
                        nc.tensor.matmul(bank[7][:, 64:96], lhsT=ustr[:], rhs=Mtb[:], start=True, stop=False)
                        return nc.tensor.matmul(bank[7][:, 64:96], lhsT=ones_b[:], rhs=Maccb[:], start=False, stop=True)
                    P.op("pe", mmRk, r=["ustr", "Mtb", "ones_b", "Maccb", "lg"], w=["b7"])
                    P.op("dve", lambda r_=r_: V.tensor_copy(out=rank[:, r_, :], in_=bank[7][:, 64:96]), r=["b7"], w=[("rank", r_)])
                    P.op("dve", lambda: V.tensor_tensor(out=Macc[:], in0=Macc[:], in1=Mt[:], op=ALU.add), r=["Macc", "Mt", "Maccb"], w=["Macc"])
                P.flush()
                if STOP <= 3:
                    return nc
        with ExitStack() as e4:
            s4 = lambda shape, dt: P.sb(e4, shape, dt)
            V = nc.vector
            cnt = s4([128, 32], F32)
            cnti = s4([128, 32], I32)
            pcf = s4([128, 32], F32)
            incl = s4([128, 32], F32)
            offx = s4([128, 32], F32)
            pos = s4([128, NR, 32], F32)
            tmp = s4([128, NR, 32], F32)
            slf = s4([128, 2, NR], F32)
            sli = s4([128, 2, NR], I32)
            thr = s4([128, NSLOT], F32)
            cmpt = s4([128, NSLOT, 32], F32)
            eidf = s4([128, NSLOT], F32)
            pidx = s4([128, 1], F32)
            widx = s4([128, NSLOT], I32)
            Maccb = s4([128, 32], BF16)
            P.op("dve", lambda: V.tensor_copy(out=Maccb[:], in_=Macc[:]), w=["Maccb"])
            P.op("pe", lambda: nc.tensor.matmul(bank[0][:, 0:32], lhsT=ones_b[:], rhs=Maccb[:], start=True, stop=True), r=["Maccb"], w=["b0"])
            P.op("dve", lambda: V.tensor_copy(out=cnt[:], in_=bank[0][:, 0:32]), r=["b0"], w=["cnt"])
            P.op("dve", lambda: V.tensor_copy(out=cnti[:], in_=cnt[:]), r=["cnt"], w=["cnti"])
            P.op("dve", lambda: V.tensor_single_scalar(out=cnti[:], in_=cnti[:], scalar=127, op=ALU.add), r=["cnti"], w=["cnti"])
            P.op("dve", lambda: V.tensor_scalar(out=cnti[:], in0=cnti[:], scalar1=7, scalar2=7, op0=ALU.arith_shift_right, op1=ALU.logical_shift_left),
                 r=["cnti"], w=["cnti"])
            P.op("dve", lambda: V.tensor_copy(out=pcf[:], in_=cnti[:]), r=["cnti"], w=["pcf"])
            P.op("dve", lambda: V.tensor_tensor_scan(out=incl[:], data0=ones_f[:, 0:32], data1=pcf[:], initial=0.0, op0=ALU.mult, op1=ALU.add),
                 r=["pcf"], w=["incl"])
            P.op("dve", lambda: V.tensor_tensor(out=offx[:], in0=incl[:], in1=pcf[:], op=ALU.subtract), r=["incl", "pcf"], w=["offx"])
            P.op("dve", lambda: V.tensor_tensor(out=pos[:], in0=rank[:], in1=offx[:].unsqueeze(1).to_broadcast([128, NR, 32]), op=ALU.add), r=["offx"], w=["pos"])
            for k, ohk in enumerate((oh1, oh2)):
                P.op("dve", lambda ohk=ohk: V.tensor_tensor(out=tmp[:], in0=pos[:], in1=ohk[:], op=ALU.mult), r=["pos", "slf"], w=["tmp"])
                P.op("dve", lambda k=k: V.reduce_sum(out=slf[:, k, :], in_=tmp[:], axis=AX.X), r=["tmp"], w=["slf"])
            P.op("dve", lambda: V.tensor_copy(out=sli[:], in_=slf[:]), r=["slf"], w=["sli"])
            P.op("pool", lambda: nc.gpsimd.iota(thr[:], pattern=[[128, NSLOT]], base=0, channel_multiplier=0, allow_small_or_imprecise_dtypes=True), w=["thr"])
            P.op("pool", lambda: nc.gpsimd.iota(pidx[:], pattern=[[0, 1]], base=-128, channel_multiplier=1, allow_small_or_imprecise_dtypes=True), w=["pidx"])
            P.op("dve", lambda: V.tensor_tensor(out=cmpt[:], in0=offx[:].unsqueeze(1).to_broadcast([128, NSLOT, 32]),
                                                in1=thr[:].unsqueeze(2).to_broadcast([128, NSLOT, 32]), op=ALU.is_le), r=["offx", "thr"], w=["cmpt"])
            P.op("dve", lambda: V.reduce_sum(out=eidf[:], in_=cmpt[:], axis=AX.X), r=["cmpt"], w=["eidf"])
            P.op("dve", lambda: V.tensor_scalar(out=eidf[:], in0=eidf[:], scalar1=128.0, scalar2=pidx[:, 0:1], op0=ALU.mult, op1=ALU.add),
                 r=["eidf", "pidx"], w=["eidf"])
            P.op("dve", lambda: V.tensor_copy(out=widx[:], in_=eidf[:]), r=["eidf"], w=["widx"])
            vt = [s4([128, D], BF16) for _ in range(2)]
            for r_ in range(NR):
                t = r_ + 1
                b = r_ % 2
                P.dma("sp", lambda t=t, b=b: nc.sync.dma_start(out=vt[b][:], in_=sc_vh[t * 128:(t + 1) * 128, :]), w=[("vt", b)])
                for k in range(2):
                    P.dma("pool", lambda r_=r_, k=k, b=b: nc.gpsimd.indirect_dma_start(
                        out=sc_xs, out_offset=bass.IndirectOffsetOnAxis(ap=sli[:, k, r_:r_ + 1], axis=0), in_=vt[b][:, :], in_offset=None),
                        r=[("vt", b), "sli"], w=[("sc_xs", r_, k)])
            NWB = 3
            wcat = [s4([128, WC], BF16) for _ in range(NWB)]
            NXB = 3
            xsl = [s4([128, D], BF16) for _ in range(NXB)]
            xsT = [s4([128, KC, 128], BF16) for _ in range(2)]
            sgt = s4([128, 256], F32)
            hh = [s4([128, 256], BF16) for _ in range(2)]
            hT = s4([128, 2, 128], BF16)
            ysb = [s4([128, D], F32) for _ in range(2)]
            scat_keys = [("sc_xs", rr, kk) for rr in range(NR) for kk in range(2)]

            def load_x(i):
                xb = i % NXB
                P.dma("sp", lambda: nc.sync.dma_start(out=xsl[xb][:], in_=sc_xs[i * 128:(i + 1) * 128, :]), r=scat_keys, w=[("xsl", xb)])

            def stage4A(i):
                b2, xb, wb = i % 2, i % NXB, i % NWB
                bx = 0 if b2 == 0 else 5
                bg = 1 if b2 == 0 else 6
                P.dma("pool", lambda: nc.gpsimd.indirect_dma_start(
                    out=wcat[wb][:, :], out_offset=None, in_=sc_wc, in_offset=bass.IndirectOffsetOnAxis(ap=widx[:, i:i + 1], axis=0)),
                    r=["widx"], w=[("wcat", wb)])
                if i + 2 < NSLOT:
                    load_x(i + 2)

                def trX():
                    for c in range(KC):
                        ins = nc.tensor.transpose(out=bfv(bx)[:, c * 128:(c + 1) * 128], in_=xsl[xb][:, c * 128:(c + 1) * 128], identity=ident[:])
                    return ins
                P.op("pe", trX, r=[("xsl", xb), "ident"], w=["b%d" % bx])
                P.op("dve", lambda: V.tensor_copy(out=xsT[b2][:].rearrange("p c t -> p (c t)"), in_=bfv(bx)), r=["b%d" % bx], w=[("xsT", b2)])
                yield

                def mmGU():
                    for c in range(KC):
                        ins = nc.tensor.matmul(bank[bg][:], lhsT=xsT[b2][:, c, :], rhs=wcat[wb][:, c * 512:(c + 1) * 512], start=(c == 0), stop=(c == KC - 1))
                    return ins
                P.op("pe", mmGU, r=[("xsT", b2), ("wcat", wb)], w=["b%d" % bg])
                P.op("act", lambda: nc.scalar.activation(out=sgt[:], in_=bank[bg][:, 0:256], func=AF.Silu), r=["b%d" % bg], w=["sgt"])
                P.op("dve", lambda: V.tensor_tensor(out=hh[b2][:], in0=bank[bg][:, 256:512], in1=sgt[:], op=ALU.mult), r=["b%d" % bg, "sgt"], w=[("hh", b2)])
                yield

            def stage4B(i):
                b2, wb = i % 2, i % NWB
                bh = 2 if b2 == 0 else 7

                def trH():
                    for c in range(2):
                        ins = nc.tensor.transpose(out=bfv(bh)[:, c * 128:(c + 1) * 128], in_=hh[b2][:, c * 128:(c + 1) * 128], identity=ident[:])
                    return ins
                P.op("pe", trH, r=[("hh", b2), "ident"], w=["b%d" % bh])
                P.op("act", lambda: nc.scalar.copy(out=hT[:].rearrange("p c t -> p (c t)"), in_=bfv(bh)[:, 0:256]), r=["b%d" % bh], w=["hT"])
                yield

                def mmD():
                    for hf in range(2):
                        for c in range(2):
                            ins = nc.tensor.matmul(bank[3 + hf][:], lhsT=hT[:, c, :], rhs=wcat[wb][:, WGU_C + c * D + hf * 512: WGU_C + c * D + (hf + 1) * 512],
                                                   start=(c == 0), stop=(c == 1))
                    return ins
                P.op("pe", mmD, r=["hT", ("wcat", wb)], w=["b3", "b4"])
                P.op("act", lambda: nc.scalar.copy(out=ysb[b2][:, 0:512], in_=bank[3][:]), r=["b3"], w=[("ysbA", b2)])
                P.op("dve", lambda: V.tensor_copy(out=ysb[b2][:, 512:1024], in_=bank[4][:]), r=["b4"], w=[("ysbB", b2)])
                P.dma("sp", lambda: nc.sync.dma_start(out=sc_ys[i * 128:(i + 1) * 128, :], in_=ysb[b2][:]), r=[("ysbA", b2), ("ysbB", b2)], w=[("sc_ys", i)])
                yield

            load_x(0)
            load_x(1)
            for n in range(NSLOT + 1):
                gens = []
                if n - 1 >= 0:
                    gens.append(stage4B(n - 1))
                if n < NSLOT:
                    gens.append(stage4A(n))
                while gens:
                    for g_ in list(gens):
                        try:
                            next(g_)
                        except StopIteration:
                            gens.remove(g_)
            P.flush()
            if STOP <= 4:
                return nc
            gfin = s4([128, D], F32)
            P.dma("sp", lambda: nc.sync.dma_start(out=gfin[:], in_=g_fin), w=["gfin"])
            NB5 = 4
            h1t = [s4([128, D], F32) for _ in range(NB5)]
            y1 = [s4([128, D], F32) for _ in range(NB5)]
            y2 = [s4([128, D], F32) for _ in range(NB5)]
            ot = [s4([128, D], F32) for _ in range(NB5)]
            junk5 = s4([128, D], BF16)
            st5 = s4([128, 4], F32)
            for r_ in range(NR):
                t = r_ + 1
                b = r_ % NB5
                P.dma("sp", lambda t=t, b=b: nc.sync.dma_start(out=h1t[b][:], in_=sc_h1[t * 128:(t + 1) * 128, :]), w=[("h1t", b)])
                for k, yk in enumerate((y1, y2)):
                    P.dma("pool", lambda r_=r_, k=k, b=b, yk=yk: nc.gpsimd.indirect_dma_start(
                        out=yk[b][:, :], out_offset=None, in_=sc_ys, in_offset=bass.IndirectOffsetOnAxis(ap=sli[:, k, r_:r_ + 1], axis=0)),
                        w=[("y", k, b)])
                P.op("dve", lambda r_=r_, b=b: V.scalar_tensor_tensor(out=ot[b][:], in0=y1[b][:], scalar=wts[:, r_, 0:1], in1=h1t[b][:], op0=ALU.mult, op1=ALU.add),
                     r=[("y", 0, b), ("h1t", b)], w=[("ot", b)])
                P.op("dve", lambda r_=r_, b=b: V.scalar_tensor_tensor(out=ot[b][:], in0=y2[b][:], scalar=wts[:, r_, 1:2], in1=ot[b][:], op0=ALU.mult, op1=ALU.add),
                     r=[("y", 1, b), ("ot", b)], w=[("ot", b)])
                P.op("act", lambda b=b: nc.scalar.activation(out=junk5[:], in_=ot[b][:], func=AF.Square, accum_out=st5[:, 0:1]), r=[("ot", b)], w=["junk5", "ss5"])
                rstd_ops(st5[:, 0:1], st5[:, 1:2], D, "ss5", "rs5")
                P.op("dve", lambda b=b: V.scalar_tensor_tensor(out=ot[b][:], in0=ot[b][:], scalar=st5[:, 1:2], in1=gfin[:], op0=ALU.mult, op1=ALU.mult),
                     r=[("ot", b), "rs5", "gfin"], w=[("ot", b)])
                P.dma("sp", lambda r_=r_, b=b: nc.sync.dma_start(out=y_out[r_ * 128:(r_ + 1) * 128, :], in_=ot[b][:]), r=[("ot", b)])
            P.flush()
    return nc


def _kc(w):
    K, N = w.shape
    return np.ascontiguousarray(w.reshape(K // 128, 128, N).transpose(1, 0, 2))


def _bc(v, n=128):
    return np.ascontiguousarray(np.broadcast_to(np.asarray(v, np.float32).reshape(1, -1), (n, v.size)))


def _rope_tables(NT):
    TOK = NT * 128
    pos = np.maximum(np.arange(TOK) - 112, 0).astype(np.float32)
    inv = (np.float32(10000.0) ** (-np.arange(0, 32, 2, dtype=np.float32) / np.float32(32))).astype(np.float32)
    ang = (pos[:, None] * inv[None, :]).astype(np.float32)
    cos, sin = np.cos(ang).astype(np.float32), np.sin(ang).astype(np.float32)
    C2 = np.concatenate([cos, cos], axis=1)
    S2 = np.concatenate([-sin, sin], axis=1)
    c2tok = np.ascontiguousarray(C2.reshape(NT, 128, 32).transpose(1, 0, 2))
    s2tok = np.ascontiguousarray(S2.reshape(NT, 128, 32).transpose(1, 0, 2))
    return c2tok, s2tok, np.ascontiguousarray(C2.T), np.ascontiguousarray(S2.T)


def prep_shared(inp, NT):
    f = lambda k: np.asarray(inp[k], np.float32)
    w_in = f("w_in")[0]
    cols = np.concatenate([np.arange(0, 416), np.arange(400, 416), np.arange(384, 400), np.arange(2720, 2736),
                           np.arange(416, 1440), np.arange(1440, 2720), np.arange(2736, 3760), np.arange(3760, 4784)])
    assert cols.size == W_IN_COLS
    m = {"w_in": _kc(w_in[:, cols]), "meta": f("meta_tokens")}
    m["g_mix"] = _bc(f("norm_mix")[0]); m["g_q"] = _bc(f("mla_q_norm")[0]); m["g_kv"] = _bc(f("mla_kv_norm")[0])
    m["g_ssm"] = _bc(f("ssm_norm")[0]); m["g_ffn"] = _bc(f("norm_ffn")[0]); m["g_fin"] = _bc(f("norm_final"))
    wq = f("mla_w_uq")[0].reshape(256, 16, 96)
    wqa = np.zeros((256, 16, 128), np.float32)
    wqa[:, :, 0:32] = wq[:, :, 64:96]
    wqa[:, :, 64:128] = wq[:, :, 0:64]
    wqs = np.concatenate([wq[:, :, 80:96], wq[:, :, 64:80]], axis=2)
    m["w_uqa"] = np.ascontiguousarray(wqa.reshape(2, 128, 16, 128).transpose(1, 0, 2, 3))
    m["w_uqs"] = np.ascontiguousarray(wqs.reshape(2, 128, 16, 32).transpose(1, 0, 2, 3))
    wkv = f("mla_w_ukv")[0].reshape(128, 16, 128)
    m["w_uk"] = np.ascontiguousarray(wkv[:, :, 0:64]); m["w_uv"] = np.ascontiguousarray(wkv[:, :, 64:128])
    m["c2tok"], m["s2tok"], m["c2T"], m["s2T"] = _rope_tables(NT)
    cw = f("ssm_conv_w")[0]
    m["conv_w"] = np.ascontiguousarray(cw.reshape(4, 10, 128).transpose(2, 1, 0))
    m["conv_b"] = np.ascontiguousarray(f("ssm_conv_b")[0].reshape(10, 128).T)
    m["dt_bias"] = _bc(f("ssm_dt_bias")[0]); m["a_log"] = _bc(f("ssm_a_log")[0]); m["d_skip"] = _bc(f("ssm_d_skip")[0])
    m["w_ba"] = _kc(f("w_branch_attn")[0]); m["w_bs"] = _kc(f("w_branch_ssm")[0]); m["w_out"] = _kc(f("w_out")[0])
    m["w_r"] = _kc(np.concatenate([f("moe_w_group")[0], f("moe_w_expert")[0]], axis=1))
    m["b_r"] = _bc(np.concatenate([f("moe_b_group")[0], f("moe_b_expert")[0]]))
    wg, wu, wd = f("moe_w_gate")[0], f("moe_w_up")[0], f("moe_w_down")[0]
    gu = np.concatenate([wg, wu], axis=2).reshape(NEXP, KC, 128, 512).transpose(0, 2, 1, 3)
    m["w_gu"] = np.ascontiguousarray(gu).reshape(NEXP * 128, KC * 512)
    dn = wd.reshape(NEXP, 2, 128, D).transpose(0, 2, 1, 3)
    m["w_dn"] = np.ascontiguousarray(dn).reshape(NEXP * 128, 2 * D)
    return m


_CACHE = {}


def run(inputs, n_cores=None):
    x = np.asarray(inputs["x"], np.float32)
    B, S, _ = x.shape
    NT = S // 128 + 1
    if NT not in _CACHE:
        _CACHE[NT] = build(NT)
    nc = _CACHE[NT]
    shared = prep_shared(inputs, NT)
    in_maps = []
    for b in range(B):
        m = dict(shared)
        m["x"] = np.ascontiguousarray(x[b])
        in_maps.append(m)
    res = run_bass_kernel_spmd(nc, in_maps, core_ids=list(range(B)))
    return np.stack([np.asarray(r["y"], np.float32).reshape(S, D) for r in res.results], axis=0)


def kernel(**inputs):
    return run(inputs)
```
